# Optimizing a Trainium2 kernel written in Bass

```python
import math
import jax, jax.numpy as jnp
from jax import lax
import numpy as np

D_MODEL = 1024
BATCH = 16
SEQ = 2048
DEPTH = 2

CHUNK = 64
Q_BLOCK = 128
EPS = 1e-6

A_HEADS = 8
A_HEAD_DIM = 64
A_LATENT = 128
IDX_HEADS = 8
IDX_DIM = 64
TOPK_MAX = 256
REL_BUCKETS = 32
REL_MAX_DIST = 128
B_HEADS = 4
B_DK = 128
B_DV = 128
GM_CHUNK = 128
GM_WIDTH = D_MODEL
GM_GROUPS = 8
MEM_TOKENS = 256
X_HEADS = 4
X_HEAD_DIM = 128
FF_DIM = ((8 * D_MODEL // 3 + 255) // 256) * 256

kernel_name = 'hybrid_dsa_hgrn2_gmlp_stream_encoder'


def rmsnorm(x, g):
    xf = x.astype(jnp.float32)
    xf = xf * lax.rsqrt(jnp.mean(xf * xf, axis=-1, keepdims=True) + EPS)
    return xf.astype(x.dtype) * g


def layernorm(x, g, b):
    xf = x.astype(jnp.float32)
    mu = jnp.mean(xf, axis=-1, keepdims=True)
    var = jnp.mean(jnp.square(xf - mu), axis=-1, keepdims=True)
    return ((xf - mu) * lax.rsqrt(var + EPS)).astype(x.dtype) * g + b


def t5_bucket(rel):
    nb = REL_BUCKETS // 2
    max_exact = nb // 2
    ret = (rel > 0).astype(jnp.int32) * nb
    n = jnp.abs(rel)
    nf = jnp.maximum(n, 1).astype(jnp.float32)
    large = max_exact + (jnp.log(nf / max_exact) / math.log(REL_MAX_DIST / max_exact) * (nb - max_exact)).astype(jnp.int32)
    large = jnp.minimum(large, nb - 1)
    return ret + jnp.where(n < max_exact, n, large)


def dsa_sparse_attention(q_lat, c, qi, ki, wi, w_uv, rel_bias, topk):
    B, S, H, _ = q_lat.shape
    nblk = S // Q_BLOCK

    def blocks(a):
        return jnp.moveaxis(a.reshape((B, nblk, Q_BLOCK) + a.shape[2:]), 1, 0)

    ki32 = ki.astype(jnp.float32)
    kchunk = jnp.arange(S, dtype=jnp.int32) // CHUNK

    def one_block(args):
        ql, qib, wib, blk = args
        qpos = blk * Q_BLOCK + jnp.arange(Q_BLOCK, dtype=jnp.int32)
        qchunk = qpos // CHUNK
        admissible = kchunk[None, :] <= qchunk[:, None]
        dots = jnp.einsum('bthd,bsd->bths', qib.astype(jnp.float32), ki32) * (IDX_DIM ** -0.5)
        score = jnp.einsum('bth,bths->bts', wib.astype(jnp.float32), jax.nn.relu(dots))
        score = jnp.where(admissible[None], score, -jnp.inf)
        _, idx = lax.top_k(score, topk)
        sel = jax.vmap(lambda cb, ib: cb[ib])(c, idx)
        valid = (idx // CHUNK) <= qchunk[None, :, None]
        bias = rel_bias[t5_bucket(idx - qpos[None, :, None])]
        logits = jnp.einsum('bthc,btkc->bthk', ql, sel).astype(jnp.float32) * (A_HEAD_DIM ** -0.5)
        logits = logits + jnp.moveaxis(bias, -1, 2).astype(jnp.float32)
        logits = jnp.where(valid[:, :, None, :], logits, -jnp.inf)
        p = jax.nn.softmax(logits, axis=-1).astype(sel.dtype)
        o_lat = jnp.einsum('bthk,btkc->bthc', p, sel)
        return jnp.einsum('bthc,hcd->bthd', o_lat, w_uv).reshape(B, Q_BLOCK, H * A_HEAD_DIM)

    out = lax.map(one_block, (blocks(q_lat), blocks(qi), blocks(wi), jnp.arange(nblk, dtype=jnp.int32)))
    return jnp.moveaxis(out, 0, 1).reshape(B, S, H * A_HEAD_DIM)


def hgrn2_recurrence(q, f_logit, v, lb):
    B, S, H, DK = q.shape
    DV = v.shape[-1]
    f32 = jnp.float32
    f = lb + (1.0 - lb) * jax.nn.sigmoid(f_logit.astype(f32))
    g = jnp.log(f)
    k = 1.0 - f
    qf = jax.nn.silu(q.astype(f32))

    def to_chunks(a):
        return a.reshape(B, S // CHUNK, CHUNK, H, a.shape[-1]).transpose(1, 0, 3, 2, 4)

    tri = jnp.tril(jnp.ones((CHUNK, CHUNK), dtype=bool))

    def step(state, xs):
        qc, kc, gc, vc = xs
        b = jnp.cumsum(gc, axis=2)
        dec = jnp.where(tri[:, :, None], b[:, :, :, None, :] - b[:, :, None, :, :], -jnp.inf)
        scores = jnp.einsum('bhtd,bhsd,bhtsd->bhts', qc, kc, jnp.exp(dec))
        o = jnp.einsum('bhts,bhsv->bhtv', scores, vc) + jnp.einsum('bhtd,bhdv->bhtv', qc * jnp.exp(b), state)
        b_end = b[:, :, -1:, :]
        state = jnp.exp(b_end[:, :, 0, :])[..., None] * state + jnp.einsum('bhsd,bhsv->bhdv', kc * jnp.exp(b_end - b), vc)
        return state, o

    state0 = jnp.zeros((B, H, DK, DV), f32)
    _, o = lax.scan(step, state0, (to_chunks(qf), to_chunks(k), to_chunks(g), to_chunks(v.astype(f32))))
    return o.transpose(1, 0, 3, 2, 4).reshape(B, S, H, DV).astype(v.dtype)


def even_mixer(h, w_in, lat_g, w_uk, w_uv, o_g, w_out, rel_bias, lb, topk):
    B, S, _ = h.shape
    sizes = [A_HEADS * A_HEAD_DIM, A_LATENT, IDX_HEADS * IDX_DIM, IDX_DIM, IDX_HEADS,
             B_HEADS * B_DK, B_HEADS * B_DV, B_HEADS * B_DK, B_HEADS * B_DV]
    offsets = np.cumsum(sizes)[:-1].tolist()
    q_a, c, qi, ki, wi, f_b, i_b, q_b, g_b = jnp.split(h @ w_in, offsets, axis=-1)
    c = rmsnorm(c, lat_g)
    q_lat = jnp.einsum('bshd,hcd->bshc', q_a.reshape(B, S, A_HEADS, A_HEAD_DIM), w_uk)
    a_out = dsa_sparse_attention(q_lat, c, qi.reshape(B, S, IDX_HEADS, IDX_DIM), ki,
                                 wi * (IDX_HEADS ** -0.5), w_uv, rel_bias, topk)
    o = hgrn2_recurrence(q_b.reshape(B, S, B_HEADS, B_DK), f_b.reshape(B, S, B_HEADS, B_DK),
                         i_b.reshape(B, S, B_HEADS, B_DV), lb.reshape(B_HEADS, B_DK))
    b_out = (rmsnorm(o, o_g) * jax.nn.silu(g_b.reshape(B, S, B_HEADS, B_DV))).reshape(B, S, B_HEADS * B_DV)
    return jnp.concatenate([a_out, b_out], axis=-1) @ w_out


def gmlp_spatial_mixer(h, w_in, ln_g, ln_b, w_sp, b_sp, w_out):
    B, S, _ = h.shape
    u, v = jnp.split(jax.nn.gelu(h @ w_in, approximate=False), 2, axis=-1)
    v = layernorm(v, ln_g, ln_b)
    v = v.reshape(B, S // GM_CHUNK, GM_CHUNK, GM_GROUPS, GM_WIDTH // GM_GROUPS)
    w = jnp.where(jnp.tril(jnp.ones((GM_CHUNK, GM_CHUNK), dtype=bool)), w_sp, 0.0)
    mixed = jnp.einsum('gts,bnsgc->bntgc', w, v) + jnp.transpose(b_sp)[None, None, :, :, None]
    return (u * mixed.reshape(B, S, GM_WIDTH)) @ w_out


def memory_cross_attention(h, m, wq, wkv, wo):
    B, S, _ = h.shape
    M = m.shape[1]
    q = (h @ wq).reshape(B, S, X_HEADS, X_HEAD_DIM)
    k, v = jnp.split(m @ wkv, 2, axis=-1)
    k = k.reshape(B, M, X_HEADS, X_HEAD_DIM)
    v = v.reshape(B, M, X_HEADS, X_HEAD_DIM)
    logits = jnp.einsum('bshd,bmhd->bhsm', q, k).astype(jnp.float32) * (X_HEAD_DIM ** -0.5)
    p = jax.nn.softmax(logits, axis=-1).astype(v.dtype)
    return jnp.einsum('bhsm,bmhd->bshd', p, v).reshape(B, S, X_HEADS * X_HEAD_DIM) @ wo


def swiglu(h, w_gu, w_down):
    gate, up = jnp.split(h @ w_gu, 2, axis=-1)
    return (jax.nn.silu(gate) * up) @ w_down


def setup_inputs(seed: int = 0) -> dict:
    key = jax.random.key(seed)
    ks = list(jax.random.split(key, 40))
    cnt = [0]

    def nxt():
        k = ks[cnt[0]]
        cnt[0] += 1
        return k

    def nrm(shape, scale):
        return jax.random.normal(nxt(), shape, jnp.float32) * scale

    def gain(shape):
        return 1.0 + nrm(shape, 0.05)

    n_even = (DEPTH + 1) // 2
    n_odd = DEPTH // 2
    p_even = (A_HEADS * A_HEAD_DIM + A_LATENT + IDX_HEADS * IDX_DIM + IDX_DIM + IDX_HEADS
              + 2 * B_HEADS * B_DK + 2 * B_HEADS * B_DV)
    mix_even = A_HEADS * A_HEAD_DIM + B_HEADS * B_DV
    xw = X_HEADS * X_HEAD_DIM
    return {
        'x': nrm((BATCH, SEQ, D_MODEL), 1.0),
        'mem': nrm((BATCH, MEM_TOKENS, D_MODEL), 1.0),
        'rel_bias': nrm((REL_BUCKETS, A_HEADS), 0.5),
        'hgrn_lb': nrm((DEPTH + 1, B_HEADS * B_DK), 0.5),
        'mix_norm': gain((DEPTH, D_MODEL)),
        'e_w_in': nrm((n_even, D_MODEL, p_even), D_MODEL ** -0.5),
        'e_lat_norm': gain((n_even, A_LATENT)),
        'e_w_uk': nrm((n_even, A_HEADS, A_LATENT, A_HEAD_DIM), A_HEAD_DIM ** -0.5),
        'e_w_uv': nrm((n_even, A_HEADS, A_LATENT, A_HEAD_DIM), A_LATENT ** -0.5),
        'e_o_norm': gain((n_even, B_DV)),
        'e_w_out': nrm((n_even, mix_even, D_MODEL), mix_even ** -0.5),
        'o_w_in': nrm((n_odd, D_MODEL, 2 * GM_WIDTH), D_MODEL ** -0.5),
        'o_ln_g': gain((n_odd, GM_WIDTH)),
        'o_ln_b': nrm((n_odd, GM_WIDTH), 0.02),
        'o_w_sp': nrm((n_odd, GM_GROUPS, GM_CHUNK, GM_CHUNK), 0.5 * GM_CHUNK ** -0.5),
        'o_b_sp': 1.0 + nrm((n_odd, GM_GROUPS, GM_CHUNK), 0.1),
        'o_w_out': nrm((n_odd, GM_WIDTH, D_MODEL), GM_WIDTH ** -0.5),
        'x_norm': gain((DEPTH, D_MODEL)),
        'mem_norm': gain((DEPTH, D_MODEL)),
        'x_wq': nrm((DEPTH, D_MODEL, xw), D_MODEL ** -0.5),
        'x_wkv': nrm((DEPTH, D_MODEL, 2 * xw), D_MODEL ** -0.5),
        'x_wo': nrm((DEPTH, xw, D_MODEL), xw ** -0.5),
        'f_norm': gain((DEPTH, D_MODEL)),
        'f_w_gu': nrm((DEPTH, D_MODEL, 2 * FF_DIM), D_MODEL ** -0.5),
        'f_w_down': nrm((DEPTH, FF_DIM, D_MODEL), FF_DIM ** -0.5),
        'final_norm': gain((D_MODEL,)),
    }


def reference(x, mem, rel_bias, hgrn_lb, mix_norm, e_w_in, e_lat_norm, e_w_uk, e_w_uv, e_o_norm, e_w_out,
              o_w_in, o_ln_g, o_ln_b, o_w_sp, o_b_sp, o_w_out, x_norm, mem_norm, x_wq, x_wkv, x_wo,
              f_norm, f_w_gu, f_w_down, final_norm):
    topk = min(TOPK_MAX, x.shape[1] // 4)
    lb_all = jnp.cumsum(jax.nn.softmax(hgrn_lb.astype(jnp.float32), axis=0), axis=0)
    h = x
    for l in range(DEPTH):
        j = l // 2
        hn = rmsnorm(h, mix_norm[l])
        if l % 2 == 0:
            h = h + even_mixer(hn, e_w_in[j], e_lat_norm[j], e_w_uk[j], e_w_uv[j], e_o_norm[j], e_w_out[j],
                               rel_bias, lb_all[l], topk)
        else:
            h = h + gmlp_spatial_mixer(hn, o_w_in[j], o_ln_g[j], o_ln_b[j], o_w_sp[j], o_b_sp[j], o_w_out[j])
        h = h + memory_cross_attention(rmsnorm(h, x_norm[l]), rmsnorm(mem, mem_norm[l]), x_wq[l], x_wkv[l], x_wo[l])
        h = h + swiglu(rmsnorm(h, f_norm[l]), f_w_gu[l], f_w_down[l])
    return rmsnorm(h, final_norm)
```

```python
import math
import os
import numpy as np
import concourse.bass as bass
import concourse.mybir as mybir
from concourse.bass_utils import run_bass_kernel_spmd
from contextlib import ExitStack

F32 = mybir.dt.float32
BF16 = mybir.dt.bfloat16
AF = mybir.ActivationFunctionType
ALU = mybir.AluOpType
AX = mybir.AxisListType

D = 1024
NCH = 8
FF = 2816
NFC = 22
MEM = 256
EPS = 1e-6
NIT = int(os.environ.get('KNIT', '14'))
KSKIP = os.environ.get('KSKIP', '')
P_EVEN = 3272
NEG = -1.0e30

V_MIX0, V_MIX1, V_X0, V_X1, V_F0, V_F1, V_M0, V_M1, V_FIN, V_LAT, V_ON, V_LB = 0, 8, 16, 24, 32, 40, 48, 56, 64, 72, 73, 74
NV = 86
C_ID, C_ONE, C_TRIU, C_TRIL, C_POW = 0, 128, 256, 320, 448
NCST = 448 + NIT + 1


class Buf:
    __slots__ = ("name", "w", "r")

    def __init__(self, name=""):
        self.name = name
        self.w = None
        self.r = []


class _Rec:
    def __init__(self):
        self.call = None

    def __getattr__(self, name):
        def f(*a, **k):
            self.call = (name, a, k)
            return self
        return f


class Prog:
    CE = ("pe", "act", "dve", "pool")
    NDS = 40

    def __init__(self, nc, stack):
        self.nc = nc
        self.ops = {e: [] for e in ("pe", "act", "dve", "pool", "sp")}
        self.esem = {e: stack.enter_context(nc.semaphore("es_" + e)) for e in self.CE}
        self.cnt = {e: 0 for e in self.CE}
        self.rings = {}
        for q, n in (("sp", 24), ("pool", 32)):
            self.rings[q] = {"sems": [stack.enter_context(nc.semaphore("d%s%d" % (q, i))) for i in range(n)], "val": [0] * n, "next": 0}
        self.waited = {e: {} for e in self.ops}
        self.nops = 0

    def _need(self, eng, ev, waits):
        if ev is None:
            return
        sem, val, _ = ev
        k = id(sem)
        if self.waited[eng].get(k, 0) >= val:
            return
        cur = waits.get(k)
        if cur is None or cur[1] < val:
            waits[k] = (sem, val)

    def _deps(self, eng, reads, writes):
        waits = {}
        for b in reads:
            ev = b.w
            if ev is not None:
                if ev[2] == eng and eng == "pe":
                    continue
                self._need(eng, ev, waits)
        for b in writes:
            ev = b.w
            if ev is not None and not (ev[2] == eng and eng == "pe"):
                self._need(eng, ev, waits)
            for ev in b.r:
                if ev[2] == eng and eng == "pe":
                    continue
                self._need(eng, ev, waits)
        for k, (sem, val) in waits.items():
            self.waited[eng][k] = val
        return list(waits.values())

    def _commit(self, ev, reads, writes):
        for b in reads:
            lst = [e for e in b.r if e[0] is not ev[0]]
            lst.append(ev)
            b.r = lst
        for b in writes:
            b.w = ev
            b.r = []

    def op(self, eng, fn, reads=(), writes=()):
        rec = _Rec()
        fn(rec)
        name_, a_, k_ = rec.call
        fn = (lambda e, name_=name_, a_=a_, k_=k_: getattr(e, name_)(*a_, **k_))
        waits = self._deps(eng, reads, writes)
        self.cnt[eng] += 1
        ev = (self.esem[eng], self.cnt[eng], eng)
        self.ops[eng].append((waits, fn, ev[0], 1))
        self._commit(ev, reads, writes)
        self.nops += 1

    def dma(self, q, out, in_, reads=(), writes=()):
        waits = self._deps(q, reads, writes)
        ring = self.rings[q]
        i = ring["next"]
        ring["next"] = (i + 1) % len(ring["sems"])
        sem = ring["sems"][i]
        if ring["val"][i] > 0:
            k = id(sem)
            if self.waited[q].get(k, 0) < ring["val"][i]:
                waits.append((sem, ring["val"][i]))
                self.waited[q][k] = ring["val"][i]
        ring["val"][i] += 16
        ev = (sem, ring["val"][i], "dma")
        self.ops[q].append((waits, (lambda e, out=out, in_=in_: e.dma_start(out=out, in_=in_)), sem, 16))
        self._commit(ev, reads, writes)
        self.nops += 1

    def barrier(self):
        for e in self.ops:
            waits = []
            for x in self.CE:
                if x != e and self.cnt[x] > 0:
                    k = id(self.esem[x])
                    if self.waited[e].get(k, 0) < self.cnt[x]:
                        waits.append((self.esem[x], self.cnt[x]))
                        self.waited[e][k] = self.cnt[x]
            for ring in self.rings.values():
                for sem, val in zip(ring["sems"], ring["val"]):
                    if val > 0:
                        k = id(sem)
                        if self.waited[e].get(k, 0) < val:
                            waits.append((sem, val))
                            self.waited[e][k] = val
            if waits:
                self.ops[e].append((waits, None, None, 0))

    def emit(self):
        nc = self.nc
        with nc.Block() as block:
            def run(e, lst):
                for waits, fn, sem, inc in lst:
                    for (s, v) in waits:
                        e.wait_ge(s, v)
                    if fn is not None:
                        ins = fn(e)
                        if sem is not None:
                            ins.then_inc(sem, inc)

            @block.tensor
            def _(e):
                run(e, self.ops["pe"])

            @block.scalar
            def _(e):
                run(e, self.ops["act"])

            @block.vector
            def _(e):
                run(e, self.ops["dve"])

            @block.gpsimd
            def _(e):
                run(e, self.ops["pool"])

            @block.sync
            def _(e):
                run(e, self.ops["sp"])


class Arena:
    def __init__(self, ap):
        self.ap = ap
        self.n = ap.shape[1]
        self.off = 0

    def reset(self, off=0):
        self.off = off

    def bf(self, n):
        n16 = (n + 15) // 16 * 16
        assert self.off + n16 <= self.n, ("arena overflow", self.off, n16, self.n)
        a = self.ap[:, self.off:self.off + n]
        self.off += n16
        return a

    def f32(self, n):
        n16 = (2 * n + 15) // 16 * 16
        assert self.off + n16 <= self.n, ("arena overflow", self.off, n16, self.n)
        a = self.ap[:, self.off:self.off + 2 * n].bitcast(F32)
        self.off += n16
        return a


def build(T, NB, stages=("mix0", "xa0", "ffn0", "mix1", "xa1", "ffn1", "fin"), KTOP=None):
    NTG = T // 512
    NT = T // 128
    if KTOP is None:
        KTOP = min(256, T // 4)
    nc = bass.Bass("TRN2", target_bir_lowering=False)

    def din(name, shape):
        return nc.dram_tensor(name, list(shape), F32, kind="ExternalInput").ap()

    xT = din("xT", [NB, D, T])
    memT = din("memT", [NB, D, MEM])
    vecs_d = din("vecs", [128, NV])
    cst_d = din("cst", [128, NCST])
    bias_d = din("biasT", [128, 3 * 8 * 128])
    e_w_in = din("e_w_in", [D, P_EVEN])
    e_w_ukT = din("e_w_ukT", [128, 4 * 128])
    e_w_uv = din("e_w_uv", [128, 8 * 64])
    e_w_out = din("e_w_out", [D, D])
    o_w_in = din("o_w_in", [D, 2 * D])
    o_ln_g = din("o_ln_g", [1, D])
    o_ln_b = din("o_ln_b", [1, D])
    o_w_spT = din("o_w_spT", [128, 8 * 128])
    o_b_sp = din("o_b_sp", [1, 8 * 128])
    o_w_out = din("o_w_out", [D, D])
    x_wq = din("x_wq", [2, D, 512])
    x_wkv = din("x_wkv", [2, D, 1024])
    x_wo = din("x_wo", [2, 512, D])
    f_w_gu = din("f_w_gu", [2, D, 2 * FF])
    f_w_down = din("f_w_down", [2, FF, D])
    outT = nc.dram_tensor("outT", [NB, D, T], F32, kind="ExternalOutput").ap()
    w_in_bf = nc.dram_tensor("w_in_bf", [D, P_EVEN], BF16, kind="Internal").ap()

    with ExitStack() as st:
        P = Prog(nc, st)

        def sb(name, shape, dt):
            return st.enter_context(nc.sbuf_tensor(name, shape, dt))

        h_t = sb("h", [128, NCH * T], F32)
        h = h_t[:].rearrange("p (c t) -> p c t", c=NCH)
        hB = [[Buf("h%d_%d" % (c, g)) for g in range(NTG)] for c in range(NCH)]
        vecs = sb("vecs_sb", [128, NV], F32)
        cstf = sb("cstf", [128, NCST], F32)
        cstb = sb("cstb", [128, 448], BF16)
        lbv = sb("lbv", [128, 16], F32)
        vB, cB, cbB, lbB = Buf("vecs"), Buf("cstf"), Buf("cstb"), Buf("lbv")
        ARN = (200 * 1024 - NCH * T * 4 - 4 * (NV + NCST + 16) - 2 * 448) // 2
        ARN = ARN // 16 * 16
        arena_t = sb("arena", [128, ARN], BF16)
        ar = Arena(arena_t[:])
        PS = [st.enter_context(nc.psum_tensor("ps%d" % i, [128, 512], F32)) for i in range(8)]
        PB = [Buf("ps%d" % i) for i in range(8)]

        ident = cstb[:, C_ID:C_ID + 128]
        ones_b = cstb[:, C_ONE:C_ONE + 128]
        triu_f = cstf[:, C_TRIU:C_TRIU + 64]
        tril_f = cstf[:, C_TRIL:C_TRIL + 128]
        pow_f = cstf[:, C_POW:C_POW + NIT + 1]

        P.dma("sp", vecs[:], vecs_d, writes=[vB])
        P.dma("sp", cstf[:], cst_d, writes=[cB])
        P.dma("pool", cstb[:], cst_d[:, 0:448], writes=[cbB])
        wbfB = [Buf("wbf%d" % i) for i in range(8)]
        if "mix0" in stages:
            for i in range(8):
                P.dma("pool", w_in_bf[i * 128:(i + 1) * 128, :], e_w_in[i * 128:(i + 1) * 128, :], writes=[wbfB[i]])
        lbe = sb("lbe", [128, 12], F32)
        lbeB = Buf("lbe")
        P.op("act", lambda e: e.activation(out=lbe[:], in_=vecs[:, V_LB:V_LB + 12], func=AF.Exp), reads=[vB], writes=[lbeB])
        P.op("dve", lambda e: e.tensor_tensor(out=lbv[:, 8:12], in0=lbe[:, 0:4], in1=lbe[:, 4:8], op=ALU.add), reads=[lbeB], writes=[lbB])
        P.op("dve", lambda e: e.tensor_tensor(out=lbv[:, 8:12], in0=lbv[:, 8:12], in1=lbe[:, 8:12], op=ALU.add), reads=[lbeB, lbB], writes=[lbB])
        P.op("dve", lambda e: e.reciprocal(out=lbv[:, 12:16], in_=lbv[:, 8:12]), reads=[lbB], writes=[lbB])
        P.op("dve", lambda e: e.tensor_tensor(out=lbv[:, 0:4], in0=lbe[:, 0:4], in1=lbv[:, 12:16], op=ALU.mult), reads=[lbeB, lbB], writes=[lbB])
        P.op("dve", lambda e: e.tensor_scalar(out=lbv[:, 4:8], in0=lbv[:, 0:4], scalar1=-1.0, scalar2=1.0, op0=ALU.mult, op1=ALU.add),
             reads=[lbB], writes=[lbB])

        def tgs(g):
            return slice(g * 512, (g + 1) * 512)

        def mm(out, lhsT, rhs, start, stop, reads, writes):
            P.op("pe", lambda e: e.matmul(out, lhsT=lhsT, rhs=rhs, start=start, stop=stop), reads=reads, writes=writes)

        def rmsnorm_tg(g, gcol, dst, dstB, scr):
            sq, sqB, lnv, lnB, pb = scr
            P.op("act", lambda e: e.activation(out=sq, in_=h[:, :, tgs(g)], func=AF.Square),
                 reads=[hB[c][g] for c in range(NCH)], writes=[sqB])
            for c in range(NCH):
                mm(PS[pb][:], ones_b, sq[:, c, :], c == 0, c == NCH - 1, [cbB, sqB], [PB[pb]])
            P.op("act", lambda e: e.activation(out=lnv, in_=PS[pb][:], func=AF.Ln, scale=1.0 / D, bias=EPS), reads=[PB[pb]], writes=[lnB])
            P.op("act", lambda e: e.activation(out=lnv, in_=lnv, func=AF.Exp, scale=-0.5), reads=[lnB], writes=[lnB])
            for c in range(NCH):
                P.op("dve", lambda e, c=c: e.scalar_tensor_tensor(out=dst[:, c, :], in0=h[:, c, tgs(g)], scalar=vecs[:, gcol + c:gcol + c + 1],
                                                                  in1=lnv, op0=ALU.mult, op1=ALU.mult),
                     reads=[hB[c][g], vB, lnB], writes=[dstB])

        def norm_scratch(pb):
            sq = ar.bf(NCH * 512).rearrange("p (c t) -> p c t", c=NCH)
            lnv = ar.f32(512)
            return (sq, Buf("sq"), lnv, Buf("lnv"), pb)

        def wview(w2d, c0, c1):
            return w2d.rearrange("(c p) n -> p c n", p=128)[:, :, c0:c1]

        def ffn_stage(l):
            P.barrier()
            ar.reset()
            hn = ar.bf(NCH * T).rearrange("p (c t) -> p c t", c=NCH)
            hnB = [Buf("hn%d" % g) for g in range(NTG)]
            act = ar.bf(11 * T).rearrange("p (f t) -> p f t", f=11)
            actB = [[Buf("act") for g in range(NTG)] for f in range(11)]
            wd = ar.bf(11 * D).rearrange("p (f n) -> p f n", f=11)
            wdB = Buf("wd")
            wgu = [ar.bf(NCH * 512).rearrange("p (c u n) -> p c u n", c=NCH, u=2) for _ in range(2)]
            wguB = [Buf("wgu0"), Buf("wgu1")]
            sg = [ar.f32(512) for _ in range(2)]
            sgB = [Buf("sg0"), Buf("sg1")]
            scr = norm_scratch(6)
            gcol = V_F0 if l == 0 else V_F1
            for g in range(NTG):
                rmsnorm_tg(g, gcol, hn[:, :, tgs(g)], hnB[g], scr)
            wgu_d = f_w_gu[l]
            wd_d = f_w_down[l]
            k = 0
            kd = 0
            for half in range(2):
                pairs = [(0, 2), (2, 2), (4, 2), (6, 2), (8, 2), (10, 1)]

                def load(pi):
                    f0, nf = pairs[pi]
                    fc0 = half * 11 + f0
                    s = pi % 2
                    P.dma("pool", wgu[s][:, :, 0, 0:nf * 128], wview(wgu_d, fc0 * 128, (fc0 + nf) * 128), writes=[wguB[s]])
                    P.dma("pool", wgu[s][:, :, 1, 0:nf * 128], wview(wgu_d, FF + fc0 * 128, FF + (fc0 + nf) * 128), writes=[wguB[s]])

                load(0)
                P.dma("pool", wd, wd_d[half * 1408:(half + 1) * 1408, :].rearrange("(f p) n -> p f n", p=128), writes=[wdB])
                for pi in range(len(pairs)):
                    if pi + 1 < len(pairs):
                        load(pi + 1)
                    f0, nf = pairs[pi]
                    s = pi % 2
                    for j in range(nf):
                        fi = f0 + j
                        for g in range(NTG):
                            pg, pu = k % 2, 2 + k % 2
                            for c in range(NCH):
                                mm(PS[pg][:], wgu[s][:, c, 0, j * 128:(j + 1) * 128], hn[:, c, tgs(g)], c == 0, c == NCH - 1,
                                   [wguB[s], hnB[g]], [PB[pg]])
                            for c in range(NCH):
                                mm(PS[pu][:], wgu[s][:, c, 1, j * 128:(j + 1) * 128], hn[:, c, tgs(g)], c == 0, c == NCH - 1,
                                   [wguB[s], hnB[g]], [PB[pu]])
                            P.op("act", lambda e, pg=pg, q=k % 2: e.activation(out=sg[q], in_=PS[pg][:], func=AF.Silu),
                                 reads=[PB[pg]], writes=[sgB[k % 2]])
                            P.op("dve", lambda e, pu=pu, q=k % 2, fi=fi, g=g: e.tensor_tensor(out=act[:, fi, tgs(g)], in0=sg[q], in1=PS[pu][:], op=ALU.mult),
                                 reads=[sgB[k % 2], PB[pu]], writes=[actB[fi][g]])
                            k += 1
                for dc in range(NCH):
                    for g in range(NTG):
                        pb = 4 + kd % 2
                        for fi in range(11):
                            mm(PS[pb][:], wd[:, fi, dc * 128:(dc + 1) * 128], act[:, fi, tgs(g)], fi == 0, fi == 10,
                               [wdB, actB[fi][g]], [PB[pb]])
                        P.op("dve", lambda e, pb=pb, dc=dc, g=g: e.tensor_tensor(out=h[:, dc, tgs(g)], in0=h[:, dc, tgs(g)], in1=PS[pb][:], op=ALU.add),
                             reads=[PB[pb], hB[dc][g]], writes=[hB[dc][g]])
                        kd += 1

        def xattn_stage(l, b):
            P.barrier()
            ar.reset()
            wq = ar.bf(NCH * 512).rearrange("p (c n) -> p c n", c=NCH)
            wo = ar.bf(4 * D).rearrange("p (c n) -> p c n", c=4)
            wkv = ar.bf(NCH * D).rearrange("p (c n) -> p c n", c=NCH)
            mem_f = ar.f32(NCH * MEM).rearrange("p (c m) -> p c m", c=NCH)
            msq = ar.bf(NCH * MEM).rearrange("p (c m) -> p c m", c=NCH)
            memn = ar.bf(NCH * MEM).rearrange("p (c m) -> p c m", c=NCH)
            mrs = ar.f32(MEM)
            kT = ar.bf(4 * MEM).rearrange("p (h m) -> p h m", h=4)
            Vt = ar.bf(2 * 512).rearrange("p (m n) -> p m n", m=2)
            hn = [ar.bf(NCH * 512).rearrange("p (c t) -> p c t", c=NCH) for _ in range(2)]
            qT = [ar.bf(4 * 512).rearrange("p (h t) -> p h t", h=4) for _ in range(2)]
            E = [ar.bf(2 * 512).rearrange("p (m t) -> p m t", m=2) for _ in range(2)]
            rden = [ar.f32(512) for _ in range(2)]
            ao = [ar.bf(4 * 512).rearrange("p (h t) -> p h t", h=4) for _ in range(2)]
            scr = norm_scratch(0)
            wqB, woB, wkvB, memB, msqB, memnB, mrsB, kTB, VtB = [Buf(n) for n in "wq wo wkv mem msq memn mrs kT Vt".split()]
            hnB = [Buf("hn0"), Buf("hn1")]
            qTB = [Buf("q0"), Buf("q1")]
            EB = [Buf("E0"), Buf("E1")]
            rdB = [Buf("rd0"), Buf("rd1")]
            aoB = [Buf("ao0"), Buf("ao1")]
            wkvB2 = Buf("wkv_v")
            P.dma("pool", wkv[:, :, 0:512], wview(x_wkv[l], 0, 512), writes=[wkvB])
            P.dma("pool", wkv[:, :, 512:1024], wview(x_wkv[l], 512, 1024), writes=[wkvB2])
            P.dma("sp", mem_f, memT[b].rearrange("(c p) m -> p c m", p=128), writes=[memB])
            P.dma("pool", wq, wview(x_wq[l], 0, 512), writes=[wqB])
            P.dma("pool", wo, x_wo[l].rearrange("(c p) n -> p c n", p=128), writes=[woB])
            P.op("act", lambda e: e.activation(out=msq, in_=mem_f, func=AF.Square), reads=[memB], writes=[msqB])
            for c in range(NCH):
                mm(PS[0][:, 0:MEM], ones_b, msq[:, c, :], c == 0, c == NCH - 1, [cbB, msqB], [PB[0]])
            P.op("act", lambda e: e.activation(out=mrs, in_=PS[0][:, 0:MEM], func=AF.Ln, scale=1.0 / D, bias=EPS), reads=[PB[0]], writes=[mrsB])
            P.op("act", lambda e: e.activation(out=mrs, in_=mrs, func=AF.Exp, scale=-0.5), reads=[mrsB], writes=[mrsB])
            mcol = V_M0 if l == 0 else V_M1
            for c in range(NCH):
                P.op("dve", lambda e, c=c: e.scalar_tensor_tensor(out=memn[:, c, :], in0=mem_f[:, c, :], scalar=vecs[:, mcol + c:mcol + c + 1],
                                                                  in1=mrs, op0=ALU.mult, op1=ALU.mult), reads=[memB, vB, mrsB], writes=[memnB])
            for hd in range(4):
                pb = 1 + hd % 2
                for c in range(NCH):
                    mm(PS[pb][:, 0:MEM], wkv[:, c, hd * 128:(hd + 1) * 128], memn[:, c, :], c == 0, c == NCH - 1, [wkvB, memnB], [PB[pb]])
                P.op("act", lambda e, pb=pb, hd=hd: e.activation(out=kT[:, hd, :], in_=PS[pb][:, 0:MEM], func=AF.Copy), reads=[PB[pb]], writes=[kTB])
            for mt in range(2):
                pb = 1 + mt
                for c in range(NCH):
                    mm(PS[pb][:], memn[:, c, mt * 128:(mt + 1) * 128], wkv[:, c, 512:1024], c == 0, c == NCH - 1, [wkvB2, memnB], [PB[pb]])
                P.op("act", lambda e, pb=pb, mt=mt: e.activation(out=Vt[:, mt, :], in_=PS[pb][:], func=AF.Copy), reads=[PB[pb]], writes=[VtB])
            xcol = V_X0 if l == 0 else V_X1
            sc = 128.0 ** -0.5
            kk = 0
            for g in range(NTG):
                q = g % 2
                rmsnorm_tg(g, xcol, hn[q], hnB[q], scr)
                for hd in range(4):
                    pb = hd % 2
                    for c in range(NCH):
                        mm(PS[pb][:], wq[:, c, hd * 128:(hd + 1) * 128], hn[q][:, c, :], c == 0, c == NCH - 1, [wqB, hnB[q]], [PB[pb]])
                    P.op("act", lambda e, pb=pb, hd=hd, q=q: e.activation(out=qT[q][:, hd, :], in_=PS[pb][:], func=AF.Copy), reads=[PB[pb]], writes=[qTB[q]])
                for hd in range(4):
                    e2 = kk % 2
                    for mt in range(2):
                        pb = 2 + mt
                        mm(PS[pb][:], kT[:, hd, mt * 128:(mt + 1) * 128], qT[q][:, hd, :], True, True, [kTB, qTB[q]], [PB[pb]])
                        P.op("act", lambda e, pb=pb, mt=mt, e2=e2: e.activation(out=E[e2][:, mt, :], in_=PS[pb][:], func=AF.Exp, scale=sc),
                             reads=[PB[pb]], writes=[EB[e2]])
                    po, pd = 4 + 2 * (kk % 2), 5 + 2 * (kk % 2)
                    for mt in range(2):
                        mm(PS[po][:], Vt[:, mt, hd * 128:(hd + 1) * 128], E[e2][:, mt, :], mt == 0, mt == 1, [VtB, EB[e2]], [PB[po]])
                    for mt in range(2):
                        mm(PS[pd][:], ones_b, E[e2][:, mt, :], mt == 0, mt == 1, [cbB, EB[e2]], [PB[pd]])
                    P.op("act", lambda e: e.activation(out=rden[e2], in_=PS[pd][:], func=AF.Ln), reads=[PB[pd]], writes=[rdB[e2]])
                    P.op("act", lambda e: e.activation(out=rden[e2], in_=rden[e2], func=AF.Exp, scale=-1.0), reads=[rdB[e2]], writes=[rdB[e2]])
                    P.op("dve", lambda e: e.tensor_tensor(out=ao[q][:, hd, :], in0=PS[po][:], in1=rden[e2], op=ALU.mult),
                         reads=[PB[po], rdB[e2]], writes=[aoB[q]])
                    kk += 1
                for dc in range(NCH):
                    pb = 6 + dc % 2 if False else (dc % 2)
                    for hd in range(4):
                        mm(PS[pb][:], wo[:, hd, dc * 128:(dc + 1) * 128], ao[q][:, hd, :], hd == 0, hd == 3, [woB, aoB[q]], [PB[pb]])
                    P.op("dve", lambda e, pb=pb, dc=dc, g=g: e.tensor_tensor(out=h[:, dc, tgs(g)], in0=h[:, dc, tgs(g)], in1=PS[pb][:], op=ALU.add),
                         reads=[PB[pb], hB[dc][g]], writes=[hB[dc][g]])

        def gmlp_stage():
            P.barrier()
            ar.reset()
            w_in = ar.bf(NCH * 2048).rearrange("p (c n) -> p c n", c=NCH)
            w_out = ar.bf(NCH * D).rearrange("p (c n) -> p c n", c=NCH)
            wsp_f = ar.f32(8 * 128).rearrange("p (g t) -> p g t", g=8)
            wsp = ar.bf(8 * 128).rearrange("p (g t) -> p g t", g=8)
            bsp = ar.f32(8 * 128).rearrange("p (g t) -> p g t", g=8)
            lng = ar.f32(D)
            lnb = ar.f32(D)
            hn0_ = ar.bf(NCH * 512).rearrange("p (c t) -> p c t", c=NCH)
            hn = [hn0_, hn0_]
            uT = ar.bf(NCH * 512).rearrange("p (c t) -> p c t", c=NCH)
            vtok = ar.bf(4 * D).rearrange("p (n c) -> p n c", n=4)
            vg = [ar.f32(D) for _ in range(2)]
            vn = [ar.f32(D) for _ in range(2)]
            st6 = ar.f32(16)
            mv = ar.f32(8)
            tmpm = [ar.f32(512) for _ in range(2)]
            gated = ar.bf(NCH * 512).rearrange("p (c t) -> p c t", c=NCH)
            scr = norm_scratch(7)
            names = "w_in w_out wspf wsp bsp lng lnb uT vtok st6 mv gated".split()
            B = {n: Buf(n) for n in names}
            hnB0_ = Buf("hn0")
            hnB = [hnB0_, hnB0_]
            vgB = [Buf("vg0"), Buf("vg1")]
            vnB = [Buf("vn0"), Buf("vn1")]
            tmB = [Buf("tm0"), Buf("tm1")]
            winB = [Buf("w_in%d" % i) for i in range(4)]
            for i4 in range(4):
                P.dma("pool", w_in[:, :, i4 * 512:(i4 + 1) * 512], wview(o_w_in, i4 * 512, (i4 + 1) * 512), writes=[winB[i4]])
            P.dma("pool", w_out, wview(o_w_out, 0, 1024), writes=[B["w_out"]])
            P.dma("sp", wsp_f, o_w_spT.rearrange("p (g t) -> p g t", g=8), writes=[B["wspf"]])
            P.dma("sp", bsp, o_b_sp[0].partition_broadcast(128).rearrange("p (g t) -> p g t", g=8), writes=[B["bsp"]])
            P.dma("sp", lng, o_ln_g[0].partition_broadcast(128), writes=[B["lng"]])
            P.dma("sp", lnb, o_ln_b[0].partition_broadcast(128), writes=[B["lnb"]])
            P.op("dve", lambda e: e.tensor_tensor(out=wsp, in0=wsp_f, in1=tril_f.unsqueeze(1).to_broadcast([128, 8, 128]), op=ALU.mult),
                 reads=[B["wspf"], cB], writes=[B["wsp"]])
            kv = 0
            km = 0
            for g in range(NTG):
                q = g % 2
                rmsnorm_tg(g, V_MIX1, hn[q], hnB[q], scr)
                for gc in range(NCH):
                    pb = gc % 2
                    for c in range(NCH):
                        mm(PS[pb][:], w_in[:, c, gc * 128:(gc + 1) * 128], hn[q][:, c, :], c == 0, c == NCH - 1, [winB[gc // 4], hnB[q]], [PB[pb]])
                    P.op("act", lambda e, pb=pb, gc=gc: e.activation(out=uT[:, gc, :], in_=PS[pb][:], func=AF.Gelu), reads=[PB[pb]], writes=[B["uT"]])
                for n in range(4):
                    v2 = kv % 2
                    for hf in range(2):
                        pb = 2 + hf
                        for c in range(NCH):
                            mm(PS[pb][:], hn[q][:, c, n * 128:(n + 1) * 128], w_in[:, c, 1024 + hf * 512:1024 + (hf + 1) * 512], c == 0, c == NCH - 1,
                               [winB[2 + hf], hnB[q]], [PB[pb]])
                        P.op("act", lambda e, pb=pb, hf=hf, v2=v2: e.activation(out=vg[v2][:, hf * 512:(hf + 1) * 512], in_=PS[pb][:], func=AF.Gelu),
                             reads=[PB[pb]], writes=[vgB[v2]])
                    for hf in range(2):
                        P.op("dve", lambda e, hf=hf, v2=v2: e.bn_stats(out=st6[:, hf * 6:(hf + 1) * 6], in_=vg[v2][:, hf * 512:(hf + 1) * 512]),
                             reads=[vgB[v2]], writes=[B["st6"]])
                    P.op("dve", lambda e: e.bn_aggr(out=mv[:, 0:2], in_=st6[:, 0:12]), reads=[B["st6"]], writes=[B["mv"]])
                    P.op("act", lambda e: e.activation(out=mv[:, 2:3], in_=mv[:, 1:2], func=AF.Ln, bias=EPS), reads=[B["mv"]], writes=[B["mv"]])
                    P.op("act", lambda e: e.activation(out=mv[:, 2:3], in_=mv[:, 2:3], func=AF.Exp, scale=-0.5), reads=[B["mv"]], writes=[B["mv"]])
                    P.op("dve", lambda e, v2=v2: e.tensor_scalar(out=vn[v2], in0=vg[v2], scalar1=mv[:, 0:1], scalar2=mv[:, 2:3], op0=ALU.subtract, op1=ALU.mult),
                         reads=[vgB[v2], B["mv"]], writes=[vnB[v2]])
                    P.op("pool", lambda e, v2=v2: e.tensor_tensor(out=vn[v2], in0=vn[v2], in1=lng, op=ALU.mult), reads=[vnB[v2], B["lng"]], writes=[vnB[v2]])
                    P.op("pool", lambda e, v2=v2, n=n: e.tensor_tensor(out=vtok[:, n, :], in0=vn[v2], in1=lnb, op=ALU.add), reads=[vnB[v2], B["lnb"]], writes=[B["vtok"]])
                    kv += 1
                for gc in range(NCH):
                    pb = 4 + gc % 2
                    for n in range(4):
                        mm(PS[pb][:, n * 128:(n + 1) * 128], vtok[:, n, gc * 128:(gc + 1) * 128], wsp[:, gc, :], True, True, [B["vtok"], B["wsp"]], [PB[pb]])
                    t2 = km % 2
                    P.op("dve", lambda e, pb=pb, gc=gc, t2=t2: e.tensor_tensor(out=tmpm[t2].rearrange("p (n t) -> p n t", n=4),
                                                                            in0=PS[pb][:].rearrange("p (n t) -> p n t", n=4),
                                                                            in1=bsp[:, gc, :].unsqueeze(1).to_broadcast([128, 4, 128]), op=ALU.add),
                         reads=[PB[pb], B["bsp"]], writes=[tmB[t2]])
                    P.op("pool", lambda e, gc=gc, t2=t2: e.tensor_tensor(out=gated[:, gc, :], in0=tmpm[t2], in1=uT[:, gc, :], op=ALU.mult),
                         reads=[tmB[t2], B["uT"]], writes=[B["gated"]])
                    km += 1
                for dc in range(NCH):
                    pb = 6 + dc % 2
                    if pb == 7:
                        pb = 0
                    for c in range(NCH):
                        mm(PS[pb][:], w_out[:, c, dc * 128:(dc + 1) * 128], gated[:, c, :], c == 0, c == NCH - 1, [B["w_out"], B["gated"]], [PB[pb]])
                    P.op("dve", lambda e, pb=pb, dc=dc, g=g: e.tensor_tensor(out=h[:, dc, tgs(g)], in0=h[:, dc, tgs(g)], in1=PS[pb][:], op=ALU.add),
                         reads=[PB[pb], hB[dc][g]], writes=[hB[dc][g]])

        def mixer0_stage(b):
            P.barrier()
            ar.reset()
            cnT = ar.bf(T)
            Ctok = ar.bf(NT * 128).rearrange("p (i c) -> p i c", i=NT)
            kiT2 = ar.bf(T)
            Sf = ar.f32(512)
            Sb = ar.bf(512)
            expb = ar.bf(2 * 8 * 128).rearrange("p (k h t) -> p k h t", k=2, h=8)
            wukT = ar.bf(4 * 128).rearrange("p (q c) -> p q c", q=4)
            wuv = ar.bf(8 * 64).rearrange("p (h d) -> p h d", h=8)
            NAMES = "cnT Ctok kiT2 Sf Sb expb wukT wuv".split()
            B = {n: Buf(n) for n in NAMES}
            base_off = ar.off
            bias_f = ar.f32(3 * 8 * 128).rearrange("p (k h t) -> p k h t", k=3, h=8)
            bfB = Buf("bias_f")
            P.dma("sp", bias_f, bias_d.rearrange("p (k h t) -> p k h t", k=3, h=8), writes=[bfB])
            P.dma("pool", wukT, e_w_ukT.rearrange("p (q c) -> p q c", q=4), writes=[B["wukT"]])
            P.dma("pool", wuv, e_w_uv.rearrange("p (h d) -> p h d", h=8), writes=[B["wuv"]])
            for kd_ in range(2):
                P.op("dve", lambda e, kd_=kd_: e.tensor_tensor(out=bias_f[:, kd_], in0=bias_f[:, kd_], in1=bias_f[:, 2], op=ALU.subtract),
                     reads=[bfB], writes=[bfB])
            P.op("act", lambda e: e.activation(out=expb, in_=bias_f[:, 0:2], func=AF.Copy, scale=8.0), reads=[bfB], writes=[B["expb"]])
            P.op("pool", lambda e: e.memset(Sf, 0.0), writes=[B["Sf"]])
            P.op("pool", lambda e: e.memset(Sb, 0.0), writes=[B["Sb"]])
            P.barrier()

            wsB_persist = [Buf("ws%d" % i) for i in range(3)]
            prefetched = {"n": 0}
            for g in range(NTG):
                ar.reset(base_off)
                qaT = ar.bf(4 * 512).rearrange("p (q t) -> p q t", q=4)
                qlat = ar.bf(8 * 512).rearrange("p (h t) -> p h t", h=8)
                qiT = ar.bf(4 * 512).rearrange("p (q t) -> p q t", q=4)
                wtok = ar.f32(4 * 8).rearrange("p (j h) -> p j h", j=4)
                eb = ar.f32(4 * 512).rearrange("p (h t) -> p h t", h=4)
                Kt = ar.bf(4 * 512).rearrange("p (h t) -> p h t", h=4)
                Qt = ar.bf(4 * 512).rearrange("p (h t) -> p h t", h=4)
                Ktok = ar.bf(16 * 128).rearrange("p (j h d) -> p j h d", j=4, h=4)
                Vtok = ar.bf(4 * 512).rearrange("p (j n) -> p j n", j=4)
                sgT = ar.bf(4 * 512).rearrange("p (h t) -> p h t", h=4)
                boT = ar.bf(4 * 512).rearrange("p (h t) -> p h t", h=4)
                aoT = ar.bf(4 * 512).rearrange("p (q t) -> p q t", q=4)
                ph_off = ar.off
                hn = ar.bf(NCH * 512).rearrange("p (c t) -> p c t", c=NCH)
                wsl = [ar.bf(NCH * 512).rearrange("p (c n) -> p c n", c=NCH) for _ in range(3)]
                tA = [ar.f32(512) for _ in range(2)]
                tB = [ar.f32(512) for _ in range(2)]
                tC = [ar.f32(512) for _ in range(2)]
                craw = ar.f32(512)
                csq = ar.bf(512)
                scr = norm_scratch(7)
                L = {n: Buf(n) for n in "hn qaT qlat qiT wtok eb Kt Qt Ktok Vtok sgT boT aoT craw csq".split()}
                wsB = wsB_persist
                tAB = [Buf("tA0"), Buf("tA1")]
                tBB = [Buf("tB0"), Buf("tB1")]
                tCB = [Buf("tC0"), Buf("tC1")]
                rmsnorm_tg(g, V_MIX0, hn, L["hn"], scr)
                pieces = [("qa", 0, 512), ("small", None, None), ("qi", 640, 1152), ("f", 1224, 1736), ("q", 2248, 2760),
                          ("i", 1736, 2248), ("g", 2760, 3272)]

                def loadw(pi):
                    nm, c0, c1 = pieces[pi]
                    s = (pi + 1) % 3
                    if nm == "small":
                        P.dma("sp", wsl[s][:, :, 0:128], wview(w_in_bf, 512, 640), reads=wbfB, writes=[wsB[s]])
                        P.dma("sp", wsl[s][:, :, 128:192], wview(w_in_bf, 1152, 1216), reads=wbfB, writes=[wsB[s]])
                        P.dma("sp", wsl[s][:, :, 192:256], wview(w_in_bf, 1152, 1216), reads=wbfB, writes=[wsB[s]])
                        P.dma("sp", wsl[s][:, :, 256:264], wview(w_in_bf, 1216, 1224), reads=wbfB, writes=[wsB[s]])
                    else:
                        P.dma("sp", wsl[s], wview(w_in_bf, c0, c1), reads=wbfB, writes=[wsB[s]])

                if prefetched["n"] == 0:
                    loadw(0)
                    loadw(1)
                prefetched["n"] = 0
                kq = 0
                rot = {"i": 0}

                def nb():
                    rot["i"] = (rot["i"] + 1) % 6
                    return rot["i"]
                for pi, (nm, c0, c1) in enumerate(pieces):
                    if pi + 2 < len(pieces):
                        loadw(pi + 2)
                    s = (pi + 1) % 3
                    W = wsl[s]
                    WB = wsB[s]

                    def proj_fm(col0, ncol, pb):
                        for c in range(NCH):
                            mm(PS[pb][0:ncol, :], W[:, c, col0:col0 + ncol], hn[:, c, :], c == 0, c == NCH - 1, [WB, L["hn"]], [PB[pb]])

                    if nm == "qa":
                        for q4 in range(4):
                            pb = nb()
                            proj_fm(q4 * 128, 128, pb)
                            P.op("act", lambda e, pb=pb, q4=q4: e.activation(out=qaT[:, q4, :], in_=PS[pb][:], func=AF.Copy), reads=[PB[pb]], writes=[L["qaT"]])
                        for hh in range(8):
                            pb = nb()
                            i2, q4 = hh % 2, hh // 2
                            mm(PS[pb][:], wukT[64 * i2:64 * i2 + 64, q4, :], qaT[64 * i2:64 * i2 + 64, q4, :], True, True, [B["wukT"], L["qaT"]], [PB[pb]])
                            P.op("dve", lambda e, pb=pb, hh=hh: e.tensor_copy(out=qlat[:, hh, :], in_=PS[pb][:]), reads=[PB[pb]], writes=[L["qlat"]])
                    elif nm == "small":
                        proj_fm(0, 128, 0)
                        P.op("act", lambda e: e.activation(out=craw, in_=PS[0][:], func=AF.Copy), reads=[PB[0]], writes=[L["craw"]])
                        P.op("act", lambda e: e.activation(out=csq, in_=PS[0][:], func=AF.Square), reads=[PB[0]], writes=[L["csq"]])
                        mm(PS[1][:], ones_b, csq, True, True, [cbB, L["csq"]], [PB[1]])
                        P.op("act", lambda e: e.activation(out=tA[0], in_=PS[1][:], func=AF.Ln, scale=1.0 / 128, bias=EPS), reads=[PB[1]], writes=[tAB[0]])
                        P.op("act", lambda e: e.activation(out=tA[0], in_=tA[0], func=AF.Exp, scale=-0.5), reads=[tAB[0]], writes=[tAB[0]])
                        P.op("dve", lambda e: e.scalar_tensor_tensor(out=cnT[:, tgs(g)], in0=craw, scalar=vecs[:, V_LAT:V_LAT + 1], in1=tA[0],
                                                                     op0=ALU.mult, op1=ALU.mult), reads=[L["craw"], vB, tAB[0]], writes=[B["cnT"]])
                        psb = PS[1][:].bitcast(BF16)
                        for j in range(4):
                            P.op("pe", lambda e, j=j: e.transpose(psb[:, j * 128:(j + 1) * 128], cnT[:, g * 512 + j * 128:g * 512 + (j + 1) * 128], ident),
                                 reads=[B["cnT"], cbB], writes=[PB[1]])
                        P.op("act", lambda e: e.activation(out=Ctok[:, 4 * g:4 * g + 4, :], in_=psb[:, 0:512].rearrange("p (j c) -> p j c", j=4), func=AF.Copy),
                             reads=[PB[1]], writes=[B["Ctok"]])
                        proj_fm(128, 128, 2)
                        P.op("act", lambda e: e.activation(out=kiT2[:, tgs(g)], in_=PS[2][:], func=AF.Copy), reads=[PB[2]], writes=[B["kiT2"]])
                        for j in range(4):
                            for c in range(NCH):
                                mm(PS[3][:, j * 8:(j + 1) * 8], hn[:, c, j * 128:(j + 1) * 128], W[:, c, 256:264], c == 0, c == NCH - 1, [WB, L["hn"]], [PB[3]])
                        P.op("act", lambda e: e.activation(out=wtok, in_=PS[3][:, 0:32].rearrange("p (j h) -> p j h", j=4), func=AF.Copy,
                                                           scale=0.125 * (8.0 ** -0.5)), reads=[PB[3]], writes=[L["wtok"]])
                    elif nm == "qi":
                        for q4 in range(4):
                            pb = nb()
                            proj_fm(q4 * 128, 128, pb)
                            P.op("act", lambda e, pb=pb, q4=q4: e.activation(out=qiT[:, q4, :], in_=PS[pb][:], func=AF.Copy), reads=[PB[pb]], writes=[L["qiT"]])
                    elif nm == "f":
                        for hd in range(4):
                            pb = nb()
                            z = kq % 2
                            kq += 1
                            proj_fm(hd * 128, 128, pb)
                            P.op("act", lambda e, pb=pb, z=z: e.activation(out=tA[z], in_=PS[pb][:], func=AF.Sigmoid), reads=[PB[pb]], writes=[tAB[z]])
                            P.op("dve", lambda e, z=z, hd=hd: e.tensor_scalar(out=tA[z], in0=tA[z], scalar1=lbv[:, 4 + hd:5 + hd], scalar2=lbv[:, hd:hd + 1],
                                                                              op0=ALU.mult, op1=ALU.add), reads=[tAB[z], lbB], writes=[tAB[z]])
                            P.op("act", lambda e, z=z: e.activation(out=tB[z], in_=tA[z], func=AF.Ln), reads=[tAB[z]], writes=[tBB[z]])
                            for ch in range(8):
                                P.op("dve", lambda e, z=z, ch=ch: e.tensor_tensor_scan(out=tC[z][:, ch * 64:(ch + 1) * 64], data0=ones_f[:, 0:64],
                                                                                       data1=tB[z][:, ch * 64:(ch + 1) * 64], initial=0.0,
                                                                                       op0=ALU.mult, op1=ALU.add), reads=[tBB[z], cB], writes=[tCB[z]])
                            P.op("act", lambda e, z=z, hd=hd: e.activation(out=eb[:, hd, :], in_=tC[z], func=AF.Exp), reads=[tCB[z]], writes=[L["eb"]])
                            P.op("act", lambda e, z=z: e.activation(out=tB[z], in_=tC[z], func=AF.Exp, scale=-1.0), reads=[tCB[z], tBB[z]], writes=[tBB[z]])
                            P.op("dve", lambda e, z=z: e.tensor_scalar(out=tA[z], in0=tA[z], scalar1=-1.0, scalar2=1.0, op0=ALU.mult, op1=ALU.add),
                                 reads=[tAB[z]], writes=[tAB[z]])
                            P.op("dve", lambda e, z=z, hd=hd: e.tensor_tensor(out=Kt[:, hd, :], in0=tA[z], in1=tB[z], op=ALU.mult),
                                 reads=[tAB[z], tBB[z]], writes=[L["Kt"]])
                        for j in range(4):
                            pb = nb()
                            psb = PS[pb][:].bitcast(BF16)
                            for hd in range(4):
                                P.op("pe", lambda e, j=j, hd=hd, psb=psb: e.transpose(psb[:, hd * 128:(hd + 1) * 128], Kt[:, hd, j * 128:(j + 1) * 128], ident),
                                     reads=[L["Kt"], cbB], writes=[PB[pb]])
                            P.op("act", lambda e, j=j, psb=psb: e.activation(out=Ktok[:, j], in_=psb[:, 0:512].rearrange("p (h d) -> p h d", h=4), func=AF.Copy),
                                 reads=[PB[pb]], writes=[L["Ktok"]])
                    elif nm == "q":
                        for hd in range(4):
                            pb = nb()
                            z = kq % 2
                            kq += 1
                            proj_fm(hd * 128, 128, pb)
                            P.op("act", lambda e, pb=pb, z=z: e.activation(out=tA[z], in_=PS[pb][:], func=AF.Silu), reads=[PB[pb]], writes=[tAB[z]])
                            P.op("dve", lambda e, z=z, hd=hd: e.tensor_tensor(out=Qt[:, hd, :], in0=tA[z], in1=eb[:, hd, :], op=ALU.mult),
                                 reads=[tAB[z], L["eb"]], writes=[L["Qt"]])
                    elif nm == "i":
                        for j in range(4):
                            pb = nb()
                            for c in range(NCH):
                                mm(PS[pb][:], hn[:, c, j * 128:(j + 1) * 128], W[:, c, :], c == 0, c == NCH - 1, [WB, L["hn"]], [PB[pb]])
                            P.op("act", lambda e, pb=pb, j=j: e.activation(out=Vtok[:, j, :], in_=PS[pb][:], func=AF.Copy), reads=[PB[pb]], writes=[L["Vtok"]])
                    elif nm == "g":
                        for hd in range(4):
                            pb = nb()
                            proj_fm(hd * 128, 128, pb)
                            P.op("act", lambda e, pb=pb, hd=hd: e.activation(out=sgT[:, hd, :], in_=PS[pb][:], func=AF.Silu), reads=[PB[pb]], writes=[L["sgT"]])

                P.barrier()
                ar.reset(ph_off)
                Sm = [ar.bf(256) for _ in range(2)]
                SmB = [Buf("Sm0"), Buf("Sm1")]
                tS = ar.f32(512)
                tSB = Buf("tS")
                osq = ar.bf(512)
                osqB = Buf("osq")
                rst = ar.f32(512)
                rstB = Buf("rst")
                t1 = ar.f32(512)
                t1B = Buf("t1")
                first = (g == 0)
                for ch in (range(0) if 'hgrn' in KSKIP else range(8)):
                    j, p0 = ch // 2, 64 * (ch % 2)
                    cs = slice(ch * 64, (ch + 1) * 64)
                    sm = ch % 2
                    for hd in range(4):
                        mm(PS[4][p0:p0 + 64, hd * 64:(hd + 1) * 64], Kt[:, hd, cs], Qt[:, hd, cs], True, True, [L["Kt"], L["Qt"]], [PB[4]])
                    P.op("dve", lambda e, p0=p0, sm=sm: e.tensor_tensor(out=Sm[sm][p0:p0 + 64, :].rearrange("p (h t) -> p h t", h=4),
                                                                       in0=PS[4][p0:p0 + 64, 0:256].rearrange("p (h t) -> p h t", h=4),
                                                                       in1=triu_f[p0:p0 + 64, :].unsqueeze(1).to_broadcast([64, 4, 64]), op=ALU.mult),
                         reads=[PB[4], cB], writes=[SmB[sm]])
                    for hd in range(4):
                        noS = first and ch == 0
                        mm(PS[hd][:, cs], Vtok[p0:p0 + 64, j, hd * 128:(hd + 1) * 128], Sm[sm][p0:p0 + 64, hd * 64:(hd + 1) * 64], True, noS,
                           [L["Vtok"], SmB[sm]], [PB[hd]])
                        if not noS:
                            mm(PS[hd][:, cs], Sb[:, hd * 128:(hd + 1) * 128], Qt[:, hd, cs], False, True, [B["Sb"], L["Qt"]], [PB[hd]])
                    for hd in range(4):
                        mm(PS[5][:, hd * 128:(hd + 1) * 128], Ktok[p0:p0 + 64, j, hd, :], Vtok[p0:p0 + 64, j, hd * 128:(hd + 1) * 128], True, True,
                           [L["Ktok"], L["Vtok"]], [PB[5]])
                    P.op("dve", lambda e: e.tensor_tensor(out=tS, in0=Sf, in1=PS[5][:], op=ALU.add), reads=[B["Sf"], PB[5]], writes=[tSB])
                    aend = eb[:, :, ch * 64 + 63:ch * 64 + 64]
                    P.op("dve", lambda e, aend=aend: e.tensor_tensor(out=Sf.rearrange("p (h v) -> p h v", h=4), in0=tS.rearrange("p (h v) -> p h v", h=4),
                                                                     in1=aend.to_broadcast([128, 4, 128]), op=ALU.mult),
                         reads=[tSB, L["eb"]], writes=[B["Sf"]])
                    P.op("act", lambda e: e.activation(out=Sb, in_=Sf, func=AF.Copy), reads=[B["Sf"]], writes=[B["Sb"]])
                for hd in range(4):
                    P.op("act", lambda e, hd=hd: e.activation(out=osq, in_=PS[hd][:], func=AF.Square), reads=[PB[hd]], writes=[osqB])
                    mm(PS[6][:], ones_b, osq, True, True, [cbB, osqB], [PB[6]])
                    P.op("act", lambda e: e.activation(out=rst, in_=PS[6][:], func=AF.Ln, scale=1.0 / 128, bias=EPS), reads=[PB[6]], writes=[rstB])
                    P.op("act", lambda e: e.activation(out=rst, in_=rst, func=AF.Exp, scale=-0.5), reads=[rstB], writes=[rstB])
                    P.op("dve", lambda e, hd=hd: e.scalar_tensor_tensor(out=t1, in0=PS[hd][:], scalar=vecs[:, V_ON:V_ON + 1], in1=rst, op0=ALU.mult, op1=ALU.mult),
                         reads=[PB[hd], vB, rstB], writes=[t1B])
                    P.op("dve", lambda e, hd=hd: e.tensor_tensor(out=boT[:, hd, :], in0=t1, in1=sgT[:, hd, :], op=ALU.mult), reads=[t1B, L["sgT"]], writes=[L["boT"]])

                P.barrier()
                ar.reset(ph_off)
                score2 = [ar.f32(T) for _ in range(2)]
                Rr = [ar.bf(512) for _ in range(4)]
                rrow = ar.f32(512)
                msk = ar.bf(T)
                junk = msk
                maskT2 = [ar.bf(NT * 128).rearrange("p (i t) -> p i t", i=NT) for _ in range(2)]
                ET = [ar.bf(1024).rearrange("p (h t) -> p h t", h=8) for _ in range(3)]
                st_ = ar.f32(8 + 2 * (NIT + 1))
                rd = ar.f32(1024)
                olat = ar.bf(1024).rearrange("p (h t) -> p h t", h=8)
                wdiag = ar.bf(4 * 8 * 128).rearrange("p (j h t) -> p j h t", j=4, h=8)
                D3 = {n: Buf(n) for n in "msk st rd olat wdiag rrow".split()}
                D3["junk"] = D3["msk"]
                scB = [Buf("score0"), Buf("score1")]
                mTB = [Buf("maskT0"), Buf("maskT1")]
                RB = [Buf("R%d" % i) for i in range(4)]
                ETB = [Buf("ET0"), Buf("ET1"), Buf("ET2")]
                P7 = [Buf("ps7a"), Buf("ps7b")]
                P6 = [Buf("ps6a"), Buf("ps6b")]
                thr, cnt, tt, amax = st_[:, 0:1], st_[:, 1:2], st_[:, 2:3], st_[:, 3:4]
                dl = st_[:, 8:8 + NIT + 1]
                cnts = {"kr": 0, "ke": 0, "kf": 0}
                ident_f = cstf[:, C_ID:C_ID + 128]
                for jt in range(4):
                    P.op("dve", lambda e: e.tensor_tensor(out=wdiag[:, jt], in0=ident_f.unsqueeze(1).to_broadcast([128, 8, 128]),
                                                          in1=wtok[:, jt, :].unsqueeze(2).to_broadcast([128, 8, 128]), op=ALU.mult),
                         reads=[cB, L["wtok"]], writes=[D3["wdiag"]])

                def gen_I(jt):
                    J = 4 * g + jt
                    n2 = 128 * (J + 1)
                    tcol = slice(jt * 128, (jt + 1) * 128)
                    score = score2[jt % 2]
                    sB = scB[jt % 2]
                    nblk = (n2 + 511) // 512
                    steps = [(blk, hh) for blk in range(nblk) for hh in range(8)]
                    k0 = cnts["kr"]
                    cnts["kr"] += len(steps)
                    dbank = (5, 7)

                    def dots(k):
                        blk, hh = steps[k]
                        s0 = blk * 512
                        ns = min(512, n2 - s0)
                        pb = dbank[(k0 + k) % 2]
                        i2, q4 = hh % 2, hh // 2
                        mm(PS[pb][:, 0:ns], qiT[64 * i2:64 * i2 + 64, q4, tcol], kiT2[64 * i2:64 * i2 + 64, s0:s0 + ns], True, True,
                           [L["qiT"], B["kiT2"]], [PB[pb]])

                    dots(0)
                    for k, (blk, hh) in enumerate(steps):
                        s0 = blk * 512
                        ns = min(512, n2 - s0)
                        pb = dbank[(k0 + k) % 2]
                        r4 = (k0 + k) % 4
                        if k + 1 < len(steps):
                            dots(k + 1)
                        P.op("act", lambda e: e.activation(out=Rr[r4][:, 0:ns], in_=PS[pb][:, 0:ns], func=AF.Relu),
                             reads=[PB[pb]], writes=[RB[r4]])
                        mm(PS[6][:, 0:ns], wdiag[:, jt, hh, :], Rr[r4][:, 0:ns], hh == 0, hh == 7, [D3["wdiag"], RB[r4]], [PB[6]])
                        if hh == 7:
                            P.op("act", lambda e: e.activation(out=score[:, s0:s0 + ns], in_=PS[6][:, 0:ns], func=AF.Copy),
                                 reads=[PB[6]], writes=[sB])
                        yield "head"

                def gen_S(jt):
                    J = 4 * g + jt
                    n2 = 128 * (J + 1)
                    score = score2[jt % 2]
                    sB = scB[jt % 2]
                    maskT = maskT2[jt % 2]
                    mB = mTB[jt % 2]
                    if n2 > KTOP:
                        P.op("dve", lambda e: e.tensor_reduce(out=amax, in_=score[:, 0:n2], axis=AX.X, op=ALU.max, apply_absolute_value=True),
                             reads=[sB], writes=[D3["st"]])
                        P.op("dve", lambda e: e.tensor_scalar(out=dl, in0=pow_f, scalar1=amax, scalar2=None, op0=ALU.mult), reads=[D3["st"], cB], writes=[D3["st"]])
                        P.op("dve", lambda e: e.memset(thr, 0.0), reads=[D3["st"]], writes=[D3["st"]])
                    P.op("dve", lambda e: e.memset(score[0:64, n2 - 64:n2], NEG), reads=[sB], writes=[sB])
                    yield "it"
                    if n2 > KTOP:
                        for it in range(NIT):
                            P.op("dve", lambda e: e.tensor_scalar(out=junk[:, 0:n2], in0=score[:, 0:n2], scalar1=thr, scalar2=None, op0=ALU.is_ge, op1=ALU.add,
                                                                  accum_out=cnt), reads=[sB, D3["st"]], writes=[D3["junk"], D3["st"]])
                            P.op("dve", lambda e: e.tensor_scalar(out=tt, in0=cnt, scalar1=KTOP - 0.5, scalar2=dl[:, it:it + 1], op0=ALU.is_ge, op1=ALU.mult),
                                 reads=[D3["st"]], writes=[D3["st"]])
                            P.op("dve", lambda e: e.scalar_tensor_tensor(out=thr, in0=tt, scalar=dl[:, it + 1:it + 2], in1=thr, op0=ALU.subtract, op1=ALU.add),
                                 reads=[D3["st"]], writes=[D3["st"]])
                            yield "it"
                    else:
                        P.op("dve", lambda e: e.memset(thr, -1.0e29), reads=[D3["st"]], writes=[D3["st"]])
                    P.op("dve", lambda e: e.tensor_scalar(out=msk[:, 0:n2], in0=score[:, 0:n2], scalar1=thr, scalar2=-30000.0, op0=ALU.is_lt, op1=ALU.mult),
                         reads=[sB, D3["st"]], writes=[D3["msk"]])
                    yield "mask"
                    for i0 in range(0, J + 1, 8):
                        ni = min(8, J + 1 - i0)
                        psb = PS[7][:].bitcast(BF16)
                        for ii in range(ni):
                            P.op("pe", lambda e: e.transpose(psb[:, ii * 128:(ii + 1) * 128], msk[:, (i0 + ii) * 128:(i0 + ii + 1) * 128], ident),
                                 reads=[D3["msk"], cbB], writes=[PB[7]])
                        P.op("act", lambda e: e.activation(out=maskT[:, i0:i0 + ni, :], in_=psb[:, 0:ni * 128].rearrange("p (i t) -> p i t", i=ni),
                                                           func=AF.Copy), reads=[PB[7]], writes=[mB])
                        yield "tr"

                def gen_B(jt):
                    J = 4 * g + jt
                    tcol = slice(jt * 128, (jt + 1) * 128)
                    maskT = maskT2[jt % 2]
                    mB = mTB[jt % 2]
                    for i in range(J + 1):
                        e2 = cnts["ke"] % 3
                        cnts["ke"] += 1
                        near = i >= J - 1
                        kind = 0 if i == J else 1
                        for hf in range(2):
                            o3 = PS[hf][:].rearrange("p (h t) -> p h t", h=4)
                            mm(o3, cnT[:, i * 128:(i + 1) * 128], qlat[:, 4 * hf:4 * hf + 4, tcol], True, False, [B["cnT"], L["qlat"]], [PB[hf]])
                            mm(o3, ident, maskT[:, i:i + 1, :].to_broadcast([128, 4, 128]), False, not near, [cbB, mB], [PB[hf]])
                            if near:
                                mm(o3, ident, expb[:, kind, 4 * hf:4 * hf + 4, :], False, True, [cbB, B["expb"]], [PB[hf]])
                            P.op("act", lambda e: e.activation(out=ET[e2][:, 4 * hf:4 * hf + 4, :], in_=PS[hf][:].rearrange("p (h t) -> p h t", h=4),
                                                               func=AF.Exp, scale=0.125), reads=[PB[hf]], writes=[ETB[e2]])
                        for hf in range(2):
                            mm(PS[2 + hf][:].rearrange("p (h t) -> p h t", h=4), Ctok[:, i, :], ET[e2][:, 4 * hf:4 * hf + 4, :], i == 0, i == J,
                               [B["Ctok"], ETB[e2]], [PB[2 + hf]])
                        for hf in range(2):
                            mm(PS[4][32 * hf:32 * hf + 1, :].rearrange("p (h t) -> p h t", h=4), ones_b[:, 0:1], ET[e2][:, 4 * hf:4 * hf + 4, :], i == 0, i == J,
                               [cbB, ETB[e2]], [PB[4]])
                        yield "pair"
                    yield "pairs_done"
                    ones_f128 = cstf[:, C_ONE:C_ONE + 128]
                    for hf in range(2):
                        rr = rrow[32 * hf:32 * hf + 1, :]
                        P.op("act", lambda e: e.activation(out=rr, in_=PS[4][32 * hf:32 * hf + 1, :], func=AF.Ln), reads=[PB[4]], writes=[D3["rrow"]])
                        P.op("act", lambda e: e.activation(out=rr, in_=rr, func=AF.Exp, scale=-1.0), reads=[D3["rrow"]], writes=[D3["rrow"]])
                        mm(PS[hf][:], ones_f128[32 * hf:32 * hf + 1, :], rr, True, True, [cB, D3["rrow"]], [PB[hf]])
                        P.op("act", lambda e: e.activation(out=rd[:, hf * 512:(hf + 1) * 512], in_=PS[hf][:], func=AF.Copy), reads=[PB[hf]], writes=[D3["rd"]])
                        P.op("dve", lambda e: e.tensor_tensor(out=olat[:, 4 * hf:4 * hf + 4, :], in0=PS[2 + hf][:].rearrange("p (h t) -> p h t", h=4),
                                                              in1=rd[:, hf * 512:(hf + 1) * 512].rearrange("p (h t) -> p h t", h=4), op=ALU.mult),
                             reads=[PB[2 + hf], D3["rd"]], writes=[D3["olat"]])
                    yield
                    pb = 0
                    for hh in range(8):
                        i2, q4 = hh % 2, hh // 2
                        mm(PS[pb][64 * i2:64 * i2 + 64, q4 * 128:(q4 + 1) * 128], wuv[:, hh, :], olat[:, hh, :], True, True, [B["wuv"], D3["olat"]], [PB[pb]])
                    P.op("act", lambda e: e.activation(out=aoT[:, :, tcol], in_=PS[pb][:].rearrange("p (q t) -> p q t", q=4), func=AF.Copy),
                         reads=[PB[pb]], writes=[L["aoT"]])
                    yield

                if 'dsa' not in KSKIP:
                    def n_I(jt):
                        return ((128 * (4 * g + jt + 1) + 511) // 512) * 8
                    for step in range(6):
                        gi = gen_I(step) if step < 4 else None
                        gs = gen_S(step - 1) if 0 <= step - 1 < 4 else None
                        gb = gen_B(step - 2) if 0 <= step - 2 < 4 else None
                        nS = NIT + 2
                        rI = max(1, -(-n_I(step) // nS)) if gi is not None else 0
                        rB = max(1, -(-(4 * g + step - 2 + 2) // nS)) if gb is not None else 0
                        s_bisect_done = gs is None
                        b_parked = False
                        while gi is not None or gs is not None or (gb is not None):
                            if gs is not None:
                                try:
                                    if next(gs) == "mask":
                                        s_bisect_done = True
                                except StopIteration:
                                    gs = None
                                    s_bisect_done = True
                            if gb is not None:
                                for _ in range(rB):
                                    if b_parked and not s_bisect_done:
                                        break
                                    try:
                                        if next(gb) == "pairs_done":
                                            b_parked = True
                                    except StopIteration:
                                        gb = None
                                        break
                            if gi is not None:
                                for _ in range(rI):
                                    try:
                                        next(gi)
                                    except StopIteration:
                                        gi = None
                                        break
                P.barrier()
                ar.reset(ph_off)
                wo2 = ar.bf(NCH * D).rearrange("p (c n) -> p c n", c=NCH)
                wo2B = Buf("wo2")
                P.dma("pool", wo2, wview(e_w_out, 0, 1024), writes=[wo2B])
                if g + 1 < NTG:
                    loadw(0)
                    loadw(1)
                    prefetched["n"] = 2
                for dc in range(NCH):
                    pb = dc % 2
                    for c in range(NCH):
                        src = aoT[:, c, :] if c < 4 else boT[:, c - 4, :]
                        mm(PS[pb][:], wo2[:, c, dc * 128:(dc + 1) * 128], src, c == 0, c == NCH - 1, [wo2B, L["aoT"], L["boT"]], [PB[pb]])
                    P.op("dve", lambda e, pb=pb, dc=dc, g=g: e.tensor_tensor(out=h[:, dc, tgs(g)], in0=h[:, dc, tgs(g)], in1=PS[pb][:], op=ALU.add),
                         reads=[PB[pb], hB[dc][g]], writes=[hB[dc][g]])
                P.barrier()

        def final_stage(b, do_norm):
            P.barrier()
            ar.reset()
            of = [ar.f32(NCH * 512).rearrange("p (c t) -> p c t", c=NCH) for _ in range(2)]
            ofB = [Buf("of0"), Buf("of1")]
            sq = ar.bf(NCH * 512).rearrange("p (c t) -> p c t", c=NCH)
            lnv = ar.f32(512)
            sqB, lnB = Buf("sq"), Buf("lnv")
            for g in range(NTG):
                q = g % 2
                if do_norm:
                    P.op("act", lambda e, g=g: e.activation(out=sq, in_=h[:, :, tgs(g)], func=AF.Square), reads=[hB[c][g] for c in range(NCH)], writes=[sqB])
                    for c in range(NCH):
                        mm(PS[0][:], ones_b, sq[:, c, :], c == 0, c == NCH - 1, [cbB, sqB], [PB[0]])
                    P.op("act", lambda e: e.activation(out=lnv, in_=PS[0][:], func=AF.Ln, scale=1.0 / D, bias=EPS), reads=[PB[0]], writes=[lnB])
                    P.op("act", lambda e: e.activation(out=lnv, in_=lnv, func=AF.Exp, scale=-0.5), reads=[lnB], writes=[lnB])
                    for c in range(NCH):
                        P.op("dve", lambda e, c=c, g=g, q=q: e.scalar_tensor_tensor(out=of[q][:, c, :], in0=h[:, c, tgs(g)], scalar=vecs[:, V_FIN + c:V_FIN + c + 1],
                                                                                  in1=lnv, op0=ALU.mult, op1=ALU.mult),
                             reads=[hB[c][g], vB, lnB], writes=[ofB[q]])
                    P.dma("sp", outT[b].rearrange("(c p) t -> p c t", p=128)[:, :, tgs(g)], of[q], reads=[ofB[q]])
                else:
                    P.dma("sp", outT[b].rearrange("(c p) t -> p c t", p=128)[:, :, tgs(g)], h[:, :, tgs(g)], reads=[hB[c][g] for c in range(NCH)])

        ones_f_t = sb("ones_f", [128, 64], F32)
        ones_f = ones_f_t[:]
        P.op("pool", lambda e: e.memset(ones_f, 1.0), writes=[cB])
        for b in range(NB):
            P.barrier()
            for g in range(NTG):
                P.dma("sp", h[:, :, tgs(g)], xT[b].rearrange("(c p) t -> p c t", p=128)[:, :, tgs(g)], writes=[hB[c][g] for c in range(NCH)])
            for s in stages:
                if s == "mix0":
                    mixer0_stage(b)
                elif s == "xa0":
                    xattn_stage(0, b)
                elif s == "ffn0":
                    ffn_stage(0)
                elif s == "mix1":
                    gmlp_stage()
                elif s == "xa1":
                    xattn_stage(1, b)
                elif s == "ffn1":
                    ffn_stage(1)
            final_stage(b, "fin" in stages)
        P.barrier()
        P.emit()
        nops = P.nops
    return nc, nops


def _t5_bucket(rel):
    nb = 16
    max_exact = 8
    ret = (rel > 0).astype(np.int64) * nb
    n = np.abs(rel)
    nf = np.maximum(n, 1).astype(np.float32)
    large = max_exact + (np.log(nf / max_exact) / math.log(128 / max_exact) * (nb - max_exact)).astype(np.int32)
    large = np.minimum(large, nb - 1)
    return ret + np.where(n < max_exact, n, large)


def _consts():
    c = np.zeros((128, NCST), np.float32)
    c[:, C_ID:C_ID + 128] = np.eye(128, dtype=np.float32)
    c[:, C_ONE:C_ONE + 128] = 1.0
    p = np.arange(128)
    c[:, C_TRIU:C_TRIU + 64] = ((p[:, None] % 64) <= np.arange(64)[None, :]).astype(np.float32)
    c[:, C_TRIL:C_TRIL + 128] = (p[:, None] <= np.arange(128)[None, :]).astype(np.float32)
    c[:, C_POW:C_POW + NIT + 1] = (0.5 ** np.arange(NIT + 1))[None, :].astype(np.float32)
    return c


def _fm(v):
    return np.ascontiguousarray(np.asarray(v, np.float32).reshape(8, 128).T)


def make_common(inp):
    f = lambda k: np.asarray(inp[k], np.float32)
    vec = np.zeros((128, NV), np.float32)
    vec[:, V_MIX0:V_MIX0 + 8] = _fm(f("mix_norm")[0])
    vec[:, V_MIX1:V_MIX1 + 8] = _fm(f("mix_norm")[1])
    vec[:, V_X0:V_X0 + 8] = _fm(f("x_norm")[0])
    vec[:, V_X1:V_X1 + 8] = _fm(f("x_norm")[1])
    vec[:, V_F0:V_F0 + 8] = _fm(f("f_norm")[0])
    vec[:, V_F1:V_F1 + 8] = _fm(f("f_norm")[1])
    vec[:, V_M0:V_M0 + 8] = _fm(f("mem_norm")[0])
    vec[:, V_M1:V_M1 + 8] = _fm(f("mem_norm")[1])
    vec[:, V_FIN:V_FIN + 8] = _fm(f("final_norm"))
    vec[:, V_LAT] = f("e_lat_norm")[0]
    vec[:, V_ON] = f("e_o_norm")[0]
    vec[:, V_LB:V_LB + 12] = f("hgrn_lb").reshape(3, 4, 128).transpose(2, 0, 1).reshape(128, 12)
    ps = np.arange(128)[:, None]
    pt = np.arange(128)[None, :]
    rb = f("rel_bias")
    tiles = np.zeros((128, 3, 8, 128), np.float32)
    for kind, off in enumerate((0, -128)):
        bk = _t5_bucket(ps - pt + off)
        tiles[:, kind] = rb[bk].transpose(0, 2, 1)
    tiles[:, 2] = rb[15][None, :, None]
    wuk = f("e_w_uk")[0]
    wukT = wuk.reshape(4, 2, 128, 64).transpose(1, 3, 0, 2).reshape(128, 4 * 128)
    wuv = f("e_w_uv")[0].transpose(1, 0, 2).reshape(128, 8 * 64)
    wsp = f("o_w_sp")[0].transpose(2, 0, 1).reshape(128, 8 * 128)
    return {
        "vecs": vec, "cst": _consts(), "biasT": np.ascontiguousarray(tiles.reshape(128, -1)),
        "e_w_in": np.ascontiguousarray(f("e_w_in")[0]), "e_w_ukT": np.ascontiguousarray(wukT), "e_w_uv": np.ascontiguousarray(wuv),
        "e_w_out": np.ascontiguousarray(f("e_w_out")[0]), "o_w_in": np.ascontiguousarray(f("o_w_in")[0]),
        "o_ln_g": f("o_ln_g").reshape(1, D), "o_ln_b": f("o_ln_b").reshape(1, D), "o_w_spT": np.ascontiguousarray(wsp),
        "o_b_sp": f("o_b_sp").reshape(1, 8 * 128), "o_w_out": np.ascontiguousarray(f("o_w_out")[0]),
        "x_wq": f("x_wq"), "x_wkv": f("x_wkv"), "x_wo": f("x_wo"), "f_w_gu": f("f_w_gu"), "f_w_down": f("f_w_down"),
    }


_CACHE = {}


def kernel(**inputs):
    x = np.asarray(inputs["x"], np.float32)
    mem = np.asarray(inputs["mem"], np.float32)
    Bn, T, _ = x.shape
    ncores = 8
    NB = Bn // ncores
    stages = tuple(os.environ.get("KSTAGES", "mix0,xa0,ffn0,mix1,xa1,ffn1,fin").split(","))
    key = (T, NB, stages)
    if key not in _CACHE:
        _CACHE[key] = build(T, NB, stages=stages)[0]
    nc = _CACHE[key]
    common = make_common(inputs)
    xT = np.ascontiguousarray(x.transpose(0, 2, 1))
    mT = np.ascontiguousarray(mem.transpose(0, 2, 1))
    in_maps = []
    for i in range(ncores):
        m = dict(common)
        m["xT"] = xT[i * NB:(i + 1) * NB]
        m["memT"] = mT[i * NB:(i + 1) * NB]
        in_maps.append(m)
    res = run_bass_kernel_spmd(nc, in_maps, core_ids=list(range(ncores)))
    outs = [r["outT"] for r in res.results]
    o = np.concatenate(outs, axis=0)
    return np.ascontiguousarray(o.transpose(0, 2, 1)).astype(np.float32)
```

```python
import math
import os
import numpy as np
import concourse.bass as bass
import concourse.mybir as mybir
from concourse.bass_utils import run_bass_kernel_spmd
from contextlib import ExitStack

F32 = mybir.dt.float32
BF16 = mybir.dt.bfloat16
AF = mybir.ActivationFunctionType
ALU = mybir.AluOpType
AX = mybir.AxisListType

D = 1024
NCH = 8
FF = 2816
NFC = 22
MEM = 256
EPS = 1e-6
NIT = int(os.environ.get('KNIT', '14'))
KSKIP = os.environ.get('KSKIP', '')
P_EVEN = 3272
NEG = -1.0e30

V_MIX0, V_MIX1, V_X0, V_X1, V_F0, V_F1, V_M0, V_M1, V_FIN, V_LAT, V_ON, V_LB = 0, 8, 16, 24, 32, 40, 48, 56, 64, 72, 73, 74
NV = 86
C_ID, C_ONE, C_TRIU, C_TRIL, C_POW = 0, 128, 256, 320, 448
NCST = 448 + NIT + 1


class Buf:
    __slots__ = ("name", "w", "r")

    def __init__(self, name=""):
        self.name = name
        self.w = None
        self.r = []


class _Rec:
    def __init__(self):
        self.call = None

    def __getattr__(self, name):
        def f(*a, **k):
            self.call = (name, a, k)
            return self
        return f


class Prog:
    CE = ("pe", "act", "dve", "pool")
    NDS = 40

    def __init__(self, nc, stack):
        self.nc = nc
        self.ops = {e: [] for e in ("pe", "act", "dve", "pool", "sp")}
        self.esem = {e: stack.enter_context(nc.semaphore("es_" + e)) for e in self.CE}
        self.cnt = {e: 0 for e in self.CE}
        self.rings = {}
        for q, n in (("sp", 24), ("pool", 32)):
            self.rings[q] = {"sems": [stack.enter_context(nc.semaphore("d%s%d" % (q, i))) for i in range(n)], "val": [0] * n, "next": 0}
        self.waited = {e: {} for e in self.ops}
        self.nops = 0

    def _need(self, eng, ev, waits):
        if ev is None:
            return
        sem, val, _ = ev
        k = id(sem)
        if self.waited[eng].get(k, 0) >= val:
            return
        cur = waits.get(k)
        if cur is None or cur[1] < val:
            waits[k] = (sem, val)

    def _deps(self, eng, reads, writes):
        waits = {}
        for b in reads:
            ev = b.w
            if ev is not None:
                if ev[2] == eng and eng == "pe":
                    continue
                self._need(eng, ev, waits)
        for b in writes:
            ev = b.w
            if ev is not None and not (ev[2] == eng and eng == "pe"):
                self._need(eng, ev, waits)
            for ev in b.r:
                if ev[2] == eng and eng == "pe":
                    continue
                self._need(eng, ev, waits)
        for k, (sem, val) in waits.items():
            self.waited[eng][k] = val
        return list(waits.values())

    def _commit(self, ev, reads, writes):
        for b in reads:
            lst = [e for e in b.r if e[0] is not ev[0]]
            lst.append(ev)
            b.r = lst
        for b in writes:
            b.w = ev
            b.r = []

    def op(self, eng, fn, reads=(), writes=()):
        rec = _Rec()
        fn(rec)
        name_, a_, k_ = rec.call
        fn = (lambda e, name_=name_, a_=a_, k_=k_: getattr(e, name_)(*a_, **k_))
        waits = self._deps(eng, reads, writes)
        self.cnt[eng] += 1
        ev = (self.esem[eng], self.cnt[eng], eng)
        self.ops[eng].append((waits, fn, ev[0], 1))
        self._commit(ev, reads, writes)
        self.nops += 1

    def dma(self, q, out, in_, reads=(), writes=()):
        waits = self._deps(q, reads, writes)
        ring = self.rings[q]
        i = ring["next"]
        ring["next"] = (i + 1) % len(ring["sems"])
        sem = ring["sems"][i]
        if ring["val"][i] > 0:
            k = id(sem)
            if self.waited[q].get(k, 0) < ring["val"][i]:
                waits.append((sem, ring["val"][i]))
                self.waited[q][k] = ring["val"][i]
        ring["val"][i] += 16
        ev = (sem, ring["val"][i], "dma")
        self.ops[q].append((waits, (lambda e, out=out, in_=in_: e.dma_start(out=out, in_=in_)), sem, 16))
        self._commit(ev, reads, writes)
        self.nops += 1

    def barrier(self):
        for e in self.ops:
            waits = []
            for x in self.CE:
                if x != e and self.cnt[x] > 0:
                    k = id(self.esem[x])
                    if self.waited[e].get(k, 0) < self.cnt[x]:
                        waits.append((self.esem[x], self.cnt[x]))
                        self.waited[e][k] = self.cnt[x]
            for ring in self.rings.values():
                for sem, val in zip(ring["sems"], ring["val"]):
                    if val > 0:
                        k = id(sem)
                        if self.waited[e].get(k, 0) < val:
                            waits.append((sem, val))
                            self.waited[e][k] = val
            if waits:
                self.ops[e].append((waits, None, None, 0))

    def emit(self):
        nc = self.nc
        with nc.Block() as block:
            def run(e, lst):
                for waits, fn, sem, inc in lst:
                    for (s, v) in waits:
                        e.wait_ge(s, v)
                    if fn is not None:
                        ins = fn(e)
                        if sem is not None:
                            ins.then_inc(sem, inc)

            @block.tensor
            def _(e):
                run(e, self.ops["pe"])

            @block.scalar
            def _(e):
                run(e, self.ops["act"])

            @block.vector
            def _(e):
                run(e, self.ops["dve"])

            @block.gpsimd
            def _(e):
                run(e, self.ops["pool"])

            @block.sync
            def _(e):
                run(e, self.ops["sp"])


class Arena:
    def __init__(self, ap):
        self.ap = ap
        self.n = ap.shape[1]
        self.off = 0

    def reset(self, off=0):
        self.off = off

    def bf(self, n):
        n16 = (n + 15) // 16 * 16
        assert self.off + n16 <= self.n, ("arena overflow", self.off, n16, self.n)
        a = self.ap[:, self.off:self.off + n]
        self.off += n16
        return a

    def f32(self, n):
        n16 = (2 * n + 15) // 16 * 16
        assert self.off + n16 <= self.n, ("arena overflow", self.off, n16, self.n)
        a = self.ap[:, self.off:self.off + 2 * n].bitcast(F32)
        self.off += n16
        return a


def build(T, NB, stages=("mix0", "xa0", "ffn0", "mix1", "xa1", "ffn1", "fin"), KTOP=None):
    NTG = T // 512
    NT = T // 128
    if KTOP is None:
        KTOP = min(256, T // 4)
    nc = bass.Bass("TRN2", target_bir_lowering=False)

    def din(name, shape):
        return nc.dram_tensor(name, list(shape), F32, kind="ExternalInput").ap()

    xT = din("xT", [NB, D, T])
    memT = din("memT", [NB, D, MEM])
    vecs_d = din("vecs", [128, NV])
    cst_d = din("cst", [128, NCST])
    bias_d = din("biasT", [128, 3 * 8 * 128])
    e_w_in = din("e_w_in", [D, P_EVEN])
    e_w_ukT = din("e_w_ukT", [128, 4 * 128])
    e_w_uv = din("e_w_uv", [128, 8 * 64])
    e_w_out = din("e_w_out", [D, D])
    o_w_in = din("o_w_in", [D, 2 * D])
    o_ln_g = din("o_ln_g", [1, D])
    o_ln_b = din("o_ln_b", [1, D])
    o_w_spT = din("o_w_spT", [128, 8 * 128])
    o_b_sp = din("o_b_sp", [1, 8 * 128])
    o_w_out = din("o_w_out", [D, D])
    x_wq = din("x_wq", [2, D, 512])
    x_wkv = din("x_wkv", [2, D, 1024])
    x_wo = din("x_wo", [2, 512, D])
    f_w_gu = din("f_w_gu", [2, D, 2 * FF])
    f_w_down = din("f_w_down", [2, FF, D])
    outT = nc.dram_tensor("outT", [NB, D, T], F32, kind="ExternalOutput").ap()

    with ExitStack() as st:
        P = Prog(nc, st)

        def sb(name, shape, dt):
            return st.enter_context(nc.sbuf_tensor(name, shape, dt))

        h_t = sb("h", [128, NCH * T], F32)
        h = h_t[:].rearrange("p (c t) -> p c t", c=NCH)
        hB = [[Buf("h%d_%d" % (c, g)) for g in range(NTG)] for c in range(NCH)]
        vecs = sb("vecs_sb", [128, NV], F32)
        cstf = sb("cstf", [128, NCST], F32)
        cstb = sb("cstb", [128, 448], BF16)
        lbv = sb("lbv", [128, 16], F32)
        vB, cB, cbB, lbB = Buf("vecs"), Buf("cstf"), Buf("cstb"), Buf("lbv")
        ARN = (200 * 1024 - NCH * T * 4 - 4 * (NV + NCST + 16) - 2 * 448) // 2
        ARN = ARN // 16 * 16
        arena_t = sb("arena", [128, ARN], BF16)
        ar = Arena(arena_t[:])
        PS = [st.enter_context(nc.psum_tensor("ps%d" % i, [128, 512], F32)) for i in range(8)]
        PB = [Buf("ps%d" % i) for i in range(8)]

        ident = cstb[:, C_ID:C_ID + 128]
        ones_b = cstb[:, C_ONE:C_ONE + 128]
        triu_f = cstf[:, C_TRIU:C_TRIU + 64]
        tril_f = cstf[:, C_TRIL:C_TRIL + 128]
        pow_f = cstf[:, C_POW:C_POW + NIT + 1]

        P.dma("sp", vecs[:], vecs_d, writes=[vB])
        P.dma("sp", cstf[:], cst_d, writes=[cB])
        P.dma("pool", cstb[:], cst_d[:, 0:448], writes=[cbB])
        lbe = sb("lbe", [128, 12], F32)
        lbeB = Buf("lbe")
        P.op("act", lambda e: e.activation(out=lbe[:], in_=vecs[:, V_LB:V_LB + 12], func=AF.Exp), reads=[vB], writes=[lbeB])
        P.op("dve", lambda e: e.tensor_tensor(out=lbv[:, 8:12], in0=lbe[:, 0:4], in1=lbe[:, 4:8], op=ALU.add), reads=[lbeB], writes=[lbB])
        P.op("dve", lambda e: e.tensor_tensor(out=lbv[:, 8:12], in0=lbv[:, 8:12], in1=lbe[:, 8:12], op=ALU.add), reads=[lbeB, lbB], writes=[lbB])
        P.op("dve", lambda e: e.reciprocal(out=lbv[:, 12:16], in_=lbv[:, 8:12]), reads=[lbB], writes=[lbB])
        P.op("dve", lambda e: e.tensor_tensor(out=lbv[:, 0:4], in0=lbe[:, 0:4], in1=lbv[:, 12:16], op=ALU.mult), reads=[lbeB, lbB], writes=[lbB])
        P.op("dve", lambda e: e.tensor_scalar(out=lbv[:, 4:8], in0=lbv[:, 0:4], scalar1=-1.0, scalar2=1.0, op0=ALU.mult, op1=ALU.add),
             reads=[lbB], writes=[lbB])

        def tgs(g):
            return slice(g * 512, (g + 1) * 512)

        def mm(out, lhsT, rhs, start, stop, reads, writes):
            P.op("pe", lambda e: e.matmul(out, lhsT=lhsT, rhs=rhs, start=start, stop=stop), reads=reads, writes=writes)

        def rmsnorm_tg(g, gcol, dst, dstB, scr):
            sq, sqB, lnv, lnB, pb = scr
            P.op("act", lambda e: e.activation(out=sq, in_=h[:, :, tgs(g)], func=AF.Square),
                 reads=[hB[c][g] for c in range(NCH)], writes=[sqB])
            for c in range(NCH):
                mm(PS[pb][:], ones_b, sq[:, c, :], c == 0, c == NCH - 1, [cbB, sqB], [PB[pb]])
            P.op("act", lambda e: e.activation(out=lnv, in_=PS[pb][:], func=AF.Ln, scale=1.0 / D, bias=EPS), reads=[PB[pb]], writes=[lnB])
            P.op("act", lambda e: e.activation(out=lnv, in_=lnv, func=AF.Exp, scale=-0.5), reads=[lnB], writes=[lnB])
            for c in range(NCH):
                P.op("dve", lambda e, c=c: e.scalar_tensor_tensor(out=dst[:, c, :], in0=h[:, c, tgs(g)], scalar=vecs[:, gcol + c:gcol + c + 1],
                                                                  in1=lnv, op0=ALU.mult, op1=ALU.mult),
                     reads=[hB[c][g], vB, lnB], writes=[dstB])

        def norm_scratch(pb):
            sq = ar.bf(NCH * 512).rearrange("p (c t) -> p c t", c=NCH)
            lnv = ar.f32(512)
            return (sq, Buf("sq"), lnv, Buf("lnv"), pb)

        def wview(w2d, c0, c1):
            return w2d.rearrange("(c p) n -> p c n", p=128)[:, :, c0:c1]

        def ffn_stage(l):
            P.barrier()
            ar.reset()
            hn = ar.bf(NCH * T).rearrange("p (c t) -> p c t", c=NCH)
            hnB = [Buf("hn%d" % g) for g in range(NTG)]
            act = ar.bf(11 * T).rearrange("p (f t) -> p f t", f=11)
            actB = [[Buf("act") for g in range(NTG)] for f in range(11)]
            wd = ar.bf(11 * D).rearrange("p (f n) -> p f n", f=11)
            wdB = Buf("wd")
            wgu = [ar.bf(NCH * 512).rearrange("p (c u n) -> p c u n", c=NCH, u=2) for _ in range(2)]
            wguB = [Buf("wgu0"), Buf("wgu1")]
            sg = [ar.f32(512) for _ in range(2)]
            sgB = [Buf("sg0"), Buf("sg1")]
            scr = norm_scratch(6)
            gcol = V_F0 if l == 0 else V_F1
            for g in range(NTG):
                rmsnorm_tg(g, gcol, hn[:, :, tgs(g)], hnB[g], scr)
            wgu_d = f_w_gu[l]
            wd_d = f_w_down[l]
            k = 0
            kd = 0
            for half in range(2):
                pairs = [(0, 2), (2, 2), (4, 2), (6, 2), (8, 2), (10, 1)]

                def load(pi):
                    f0, nf = pairs[pi]
                    fc0 = half * 11 + f0
                    s = pi % 2
                    P.dma("pool", wgu[s][:, :, 0, 0:nf * 128], wview(wgu_d, fc0 * 128, (fc0 + nf) * 128), writes=[wguB[s]])
                    P.dma("pool", wgu[s][:, :, 1, 0:nf * 128], wview(wgu_d, FF + fc0 * 128, FF + (fc0 + nf) * 128), writes=[wguB[s]])

                load(0)
                P.dma("pool", wd, wd_d[half * 1408:(half + 1) * 1408, :].rearrange("(f p) n -> p f n", p=128), writes=[wdB])
                for pi in range(len(pairs)):
                    if pi + 1 < len(pairs):
                        load(pi + 1)
                    f0, nf = pairs[pi]
                    s = pi % 2
                    for j in range(nf):
                        fi = f0 + j
                        for g in range(NTG):
                            pg, pu = k % 2, 2 + k % 2
                            for c in range(NCH):
                                mm(PS[pg][:], wgu[s][:, c, 0, j * 128:(j + 1) * 128], hn[:, c, tgs(g)], c == 0, c == NCH - 1,
                                   [wguB[s], hnB[g]], [PB[pg]])
                            for c in range(NCH):
                                mm(PS[pu][:], wgu[s][:, c, 1, j * 128:(j + 1) * 128], hn[:, c, tgs(g)], c == 0, c == NCH - 1,
                                   [wguB[s], hnB[g]], [PB[pu]])
                            P.op("act", lambda e, pg=pg, q=k % 2: e.activation(out=sg[q], in_=PS[pg][:], func=AF.Silu),
                                 reads=[PB[pg]], writes=[sgB[k % 2]])
                            P.op("dve", lambda e, pu=pu, q=k % 2, fi=fi, g=g: e.tensor_tensor(out=act[:, fi, tgs(g)], in0=sg[q], in1=PS[pu][:], op=ALU.mult),
                                 reads=[sgB[k % 2], PB[pu]], writes=[actB[fi][g]])
                            k += 1
                for dc in range(NCH):
                    for g in range(NTG):
                        pb = 4 + kd % 2
                        for fi in range(11):
                            mm(PS[pb][:], wd[:, fi, dc * 128:(dc + 1) * 128], act[:, fi, tgs(g)], fi == 0, fi == 10,
                               [wdB, actB[fi][g]], [PB[pb]])
                        P.op("dve", lambda e, pb=pb, dc=dc, g=g: e.tensor_tensor(out=h[:, dc, tgs(g)], in0=h[:, dc, tgs(g)], in1=PS[pb][:], op=ALU.add),
                             reads=[PB[pb], hB[dc][g]], writes=[hB[dc][g]])
                        kd += 1

        def xattn_stage(l, b):
            P.barrier()
            ar.reset()
            wq = ar.bf(NCH * 512).rearrange("p (c n) -> p c n", c=NCH)
            wo = ar.bf(4 * D).rearrange("p (c n) -> p c n", c=4)
            wkv = ar.bf(NCH * D).rearrange("p (c n) -> p c n", c=NCH)
            mem_f = ar.f32(NCH * MEM).rearrange("p (c m) -> p c m", c=NCH)
            msq = ar.bf(NCH * MEM).rearrange("p (c m) -> p c m", c=NCH)
            memn = ar.bf(NCH * MEM).rearrange("p (c m) -> p c m", c=NCH)
            mrs = ar.f32(MEM)
            kT = ar.bf(4 * MEM).rearrange("p (h m) -> p h m", h=4)
            Vt = ar.bf(2 * 512).rearrange("p (m n) -> p m n", m=2)
            hn = [ar.bf(NCH * 512).rearrange("p (c t) -> p c t", c=NCH) for _ in range(2)]
            qT = [ar.bf(4 * 512).rearrange("p (h t) -> p h t", h=4) for _ in range(2)]
            E = [ar.bf(2 * 512).rearrange("p (m t) -> p m t", m=2) for _ in range(2)]
            rden = [ar.f32(512) for _ in range(2)]
            ao = [ar.bf(4 * 512).rearrange("p (h t) -> p h t", h=4) for _ in range(2)]
            scr = norm_scratch(0)
            wqB, woB, wkvB, memB, msqB, memnB, mrsB, kTB, VtB = [Buf(n) for n in "wq wo wkv mem msq memn mrs kT Vt".split()]
            hnB = [Buf("hn0"), Buf("hn1")]
            qTB = [Buf("q0"), Buf("q1")]
            EB = [Buf("E0"), Buf("E1")]
            rdB = [Buf("rd0"), Buf("rd1")]
            aoB = [Buf("ao0"), Buf("ao1")]
            P.dma("pool", wkv, wview(x_wkv[l], 0, 1024), writes=[wkvB])
            P.dma("sp", mem_f, memT[b].rearrange("(c p) m -> p c m", p=128), writes=[memB])
            P.dma("pool", wq, wview(x_wq[l], 0, 512), writes=[wqB])
            P.dma("pool", wo, x_wo[l].rearrange("(c p) n -> p c n", p=128), writes=[woB])
            P.op("act", lambda e: e.activation(out=msq, in_=mem_f, func=AF.Square), reads=[memB], writes=[msqB])
            for c in range(NCH):
                mm(PS[0][:, 0:MEM], ones_b, msq[:, c, :], c == 0, c == NCH - 1, [cbB, msqB], [PB[0]])
            P.op("act", lambda e: e.activation(out=mrs, in_=PS[0][:, 0:MEM], func=AF.Ln, scale=1.0 / D, bias=EPS), reads=[PB[0]], writes=[mrsB])
            P.op("act", lambda e: e.activation(out=mrs, in_=mrs, func=AF.Exp, scale=-0.5), reads=[mrsB], writes=[mrsB])
            mcol = V_M0 if l == 0 else V_M1
            for c in range(NCH):
                P.op("dve", lambda e, c=c: e.scalar_tensor_tensor(out=memn[:, c, :], in0=mem_f[:, c, :], scalar=vecs[:, mcol + c:mcol + c + 1],
                                                                  in1=mrs, op0=ALU.mult, op1=ALU.mult), reads=[memB, vB, mrsB], writes=[memnB])
            for hd in range(4):
                pb = 1 + hd % 2
                for c in range(NCH):
                    mm(PS[pb][:, 0:MEM], wkv[:, c, hd * 128:(hd + 1) * 128], memn[:, c, :], c == 0, c == NCH - 1, [wkvB, memnB], [PB[pb]])
                P.op("act", lambda e, pb=pb, hd=hd: e.activation(out=kT[:, hd, :], in_=PS[pb][:, 0:MEM], func=AF.Copy), reads=[PB[pb]], writes=[kTB])
            for mt in range(2):
                pb = 1 + mt
                for c in range(NCH):
                    mm(PS[pb][:], memn[:, c, mt * 128:(mt + 1) * 128], wkv[:, c, 512:1024], c == 0, c == NCH - 1, [wkvB, memnB], [PB[pb]])
                P.op("act", lambda e, pb=pb, mt=mt: e.activation(out=Vt[:, mt, :], in_=PS[pb][:], func=AF.Copy), reads=[PB[pb]], writes=[VtB])
            xcol = V_X0 if l == 0 else V_X1
            sc = 128.0 ** -0.5
            kk = 0
            for g in range(NTG):
                q = g % 2
                rmsnorm_tg(g, xcol, hn[q], hnB[q], scr)
                for hd in range(4):
                    pb = hd % 2
                    for c in range(NCH):
                        mm(PS[pb][:], wq[:, c, hd * 128:(hd + 1) * 128], hn[q][:, c, :], c == 0, c == NCH - 1, [wqB, hnB[q]], [PB[pb]])
                    P.op("act", lambda e, pb=pb, hd=hd, q=q: e.activation(out=qT[q][:, hd, :], in_=PS[pb][:], func=AF.Copy), reads=[PB[pb]], writes=[qTB[q]])
                for hd in range(4):
                    e2 = kk % 2
                    for mt in range(2):
                        pb = 2 + mt
                        mm(PS[pb][:], kT[:, hd, mt * 128:(mt + 1) * 128], qT[q][:, hd, :], True, True, [kTB, qTB[q]], [PB[pb]])
                        P.op("act", lambda e, pb=pb, mt=mt, e2=e2: e.activation(out=E[e2][:, mt, :], in_=PS[pb][:], func=AF.Exp, scale=sc),
                             reads=[PB[pb]], writes=[EB[e2]])
                    po, pd = 4 + 2 * (kk % 2), 5 + 2 * (kk % 2)
                    for mt in range(2):
                        mm(PS[po][:], Vt[:, mt, hd * 128:(hd + 1) * 128], E[e2][:, mt, :], mt == 0, mt == 1, [VtB, EB[e2]], [PB[po]])
                    for mt in range(2):
                        mm(PS[pd][:], ones_b, E[e2][:, mt, :], mt == 0, mt == 1, [cbB, EB[e2]], [PB[pd]])
                    P.op("act", lambda e: e.activation(out=rden[e2], in_=PS[pd][:], func=AF.Ln), reads=[PB[pd]], writes=[rdB[e2]])
                    P.op("act", lambda e: e.activation(out=rden[e2], in_=rden[e2], func=AF.Exp, scale=-1.0), reads=[rdB[e2]], writes=[rdB[e2]])
                    P.op("dve", lambda e: e.tensor_tensor(out=ao[q][:, hd, :], in0=PS[po][:], in1=rden[e2], op=ALU.mult),
                         reads=[PB[po], rdB[e2]], writes=[aoB[q]])
                    kk += 1
                for dc in range(NCH):
                    pb = 6 + dc % 2 if False else (dc % 2)
                    for hd in range(4):
                        mm(PS[pb][:], wo[:, hd, dc * 128:(dc + 1) * 128], ao[q][:, hd, :], hd == 0, hd == 3, [woB, aoB[q]], [PB[pb]])
                    P.op("dve", lambda e, pb=pb, dc=dc, g=g: e.tensor_tensor(out=h[:, dc, tgs(g)], in0=h[:, dc, tgs(g)], in1=PS[pb][:], op=ALU.add),
                         reads=[PB[pb], hB[dc][g]], writes=[hB[dc][g]])

        def gmlp_stage():
            P.barrier()
            ar.reset()
            w_in = ar.bf(NCH * 2048).rearrange("p (c n) -> p c n", c=NCH)
            w_out = ar.bf(NCH * D).rearrange("p (c n) -> p c n", c=NCH)
            wsp_f = ar.f32(8 * 128).rearrange("p (g t) -> p g t", g=8)
            wsp = ar.bf(8 * 128).rearrange("p (g t) -> p g t", g=8)
            bsp = ar.f32(8 * 128).rearrange("p (g t) -> p g t", g=8)
            lng = ar.f32(D)
            lnb = ar.f32(D)
            hn0_ = ar.bf(NCH * 512).rearrange("p (c t) -> p c t", c=NCH)
            hn = [hn0_, hn0_]
            uT = ar.bf(NCH * 512).rearrange("p (c t) -> p c t", c=NCH)
            vtok = ar.bf(4 * D).rearrange("p (n c) -> p n c", n=4)
            vg = [ar.f32(D) for _ in range(2)]
            vn = [ar.f32(D) for _ in range(2)]
            st6 = ar.f32(16)
            mv = ar.f32(8)
            tmpm = [ar.f32(512) for _ in range(2)]
            gated = ar.bf(NCH * 512).rearrange("p (c t) -> p c t", c=NCH)
            scr = norm_scratch(7)
            names = "w_in w_out wspf wsp bsp lng lnb uT vtok st6 mv gated".split()
            B = {n: Buf(n) for n in names}
            hnB0_ = Buf("hn0")
            hnB = [hnB0_, hnB0_]
            vgB = [Buf("vg0"), Buf("vg1")]
            vnB = [Buf("vn0"), Buf("vn1")]
            tmB = [Buf("tm0"), Buf("tm1")]
            P.dma("pool", w_in, wview(o_w_in, 0, 2048), writes=[B["w_in"]])
            P.dma("pool", w_out, wview(o_w_out, 0, 1024), writes=[B["w_out"]])
            P.dma("sp", wsp_f, o_w_spT.rearrange("p (g t) -> p g t", g=8), writes=[B["wspf"]])
            P.dma("sp", bsp, o_b_sp[0].partition_broadcast(128).rearrange("p (g t) -> p g t", g=8), writes=[B["bsp"]])
            P.dma("sp", lng, o_ln_g[0].partition_broadcast(128), writes=[B["lng"]])
            P.dma("sp", lnb, o_ln_b[0].partition_broadcast(128), writes=[B["lnb"]])
            P.op("dve", lambda e: e.tensor_tensor(out=wsp, in0=wsp_f, in1=tril_f.unsqueeze(1).to_broadcast([128, 8, 128]), op=ALU.mult),
                 reads=[B["wspf"], cB], writes=[B["wsp"]])
            kv = 0
            km = 0
            for g in range(NTG):
                q = g % 2
                rmsnorm_tg(g, V_MIX1, hn[q], hnB[q], scr)
                for gc in range(NCH):
                    pb = gc % 2
                    for c in range(NCH):
                        mm(PS[pb][:], w_in[:, c, gc * 128:(gc + 1) * 128], hn[q][:, c, :], c == 0, c == NCH - 1, [B["w_in"], hnB[q]], [PB[pb]])
                    P.op("act", lambda e, pb=pb, gc=gc: e.activation(out=uT[:, gc, :], in_=PS[pb][:], func=AF.Gelu), reads=[PB[pb]], writes=[B["uT"]])
                for n in range(4):
                    v2 = kv % 2
                    for hf in range(2):
                        pb = 2 + hf
                        for c in range(NCH):
                            mm(PS[pb][:], hn[q][:, c, n * 128:(n + 1) * 128], w_in[:, c, 1024 + hf * 512:1024 + (hf + 1) * 512], c == 0, c == NCH - 1,
                               [B["w_in"], hnB[q]], [PB[pb]])
                        P.op("act", lambda e, pb=pb, hf=hf, v2=v2: e.activation(out=vg[v2][:, hf * 512:(hf + 1) * 512], in_=PS[pb][:], func=AF.Gelu),
                             reads=[PB[pb]], writes=[vgB[v2]])
                    for hf in range(2):
                        P.op("dve", lambda e, hf=hf, v2=v2: e.bn_stats(out=st6[:, hf * 6:(hf + 1) * 6], in_=vg[v2][:, hf * 512:(hf + 1) * 512]),
                             reads=[vgB[v2]], writes=[B["st6"]])
                    P.op("dve", lambda e: e.bn_aggr(out=mv[:, 0:2], in_=st6[:, 0:12]), reads=[B["st6"]], writes=[B["mv"]])
                    P.op("act", lambda e: e.activation(out=mv[:, 2:3], in_=mv[:, 1:2], func=AF.Ln, bias=EPS), reads=[B["mv"]], writes=[B["mv"]])
                    P.op("act", lambda e: e.activation(out=mv[:, 2:3], in_=mv[:, 2:3], func=AF.Exp, scale=-0.5), reads=[B["mv"]], writes=[B["mv"]])
                    P.op("dve", lambda e, v2=v2: e.tensor_scalar(out=vn[v2], in0=vg[v2], scalar1=mv[:, 0:1], scalar2=mv[:, 2:3], op0=ALU.subtract, op1=ALU.mult),
                         reads=[vgB[v2], B["mv"]], writes=[vnB[v2]])
                    P.op("pool", lambda e, v2=v2: e.tensor_tensor(out=vn[v2], in0=vn[v2], in1=lng, op=ALU.mult), reads=[vnB[v2], B["lng"]], writes=[vnB[v2]])
                    P.op("pool", lambda e, v2=v2, n=n: e.tensor_tensor(out=vtok[:, n, :], in0=vn[v2], in1=lnb, op=ALU.add), reads=[vnB[v2], B["lnb"]], writes=[B["vtok"]])
                    kv += 1
                for gc in range(NCH):
                    pb = 4 + gc % 2
                    for n in range(4):
                        mm(PS[pb][:, n * 128:(n + 1) * 128], vtok[:, n, gc * 128:(gc + 1) * 128], wsp[:, gc, :], True, True, [B["vtok"], B["wsp"]], [PB[pb]])
                    t2 = km % 2
                    P.op("dve", lambda e, pb=pb, gc=gc, t2=t2: e.tensor_tensor(out=tmpm[t2].rearrange("p (n t) -> p n t", n=4),
                                                                            in0=PS[pb][:].rearrange("p (n t) -> p n t", n=4),
                                                                            in1=bsp[:, gc, :].unsqueeze(1).to_broadcast([128, 4, 128]), op=ALU.add),
                         reads=[PB[pb], B["bsp"]], writes=[tmB[t2]])
                    P.op("pool", lambda e, gc=gc, t2=t2: e.tensor_tensor(out=gated[:, gc, :], in0=tmpm[t2], in1=uT[:, gc, :], op=ALU.mult),
                         reads=[tmB[t2], B["uT"]], writes=[B["gated"]])
                    km += 1
                for dc in range(NCH):
                    pb = 6 + dc % 2
                    if pb == 7:
                        pb = 0
                    for c in range(NCH):
                        mm(PS[pb][:], w_out[:, c, dc * 128:(dc + 1) * 128], gated[:, c, :], c == 0, c == NCH - 1, [B["w_out"], B["gated"]], [PB[pb]])
                    P.op("dve", lambda e, pb=pb, dc=dc, g=g: e.tensor_tensor(out=h[:, dc, tgs(g)], in0=h[:, dc, tgs(g)], in1=PS[pb][:], op=ALU.add),
                         reads=[PB[pb], hB[dc][g]], writes=[hB[dc][g]])

        def mixer0_stage(b):
            P.barrier()
            ar.reset()
            cnT = ar.bf(T)
            Ctok = ar.bf(NT * 128).rearrange("p (i c) -> p i c", i=NT)
            kiT2 = ar.bf(T)
            Sf = ar.f32(512)
            Sb = ar.bf(512)
            expb = ar.bf(2 * 8 * 128).rearrange("p (k h t) -> p k h t", k=2, h=8)
            wukT = ar.bf(4 * 128).rearrange("p (q c) -> p q c", q=4)
            wuv = ar.bf(8 * 64).rearrange("p (h d) -> p h d", h=8)
            NAMES = "cnT Ctok kiT2 Sf Sb expb wukT wuv".split()
            B = {n: Buf(n) for n in NAMES}
            base_off = ar.off
            bias_f = ar.f32(3 * 8 * 128).rearrange("p (k h t) -> p k h t", k=3, h=8)
            bfB = Buf("bias_f")
            P.dma("sp", bias_f, bias_d.rearrange("p (k h t) -> p k h t", k=3, h=8), writes=[bfB])
            P.dma("pool", wukT, e_w_ukT.rearrange("p (q c) -> p q c", q=4), writes=[B["wukT"]])
            P.dma("pool", wuv, e_w_uv.rearrange("p (h d) -> p h d", h=8), writes=[B["wuv"]])
            for kd_ in range(2):
                P.op("dve", lambda e, kd_=kd_: e.tensor_tensor(out=bias_f[:, kd_], in0=bias_f[:, kd_], in1=bias_f[:, 2], op=ALU.subtract),
                     reads=[bfB], writes=[bfB])
            P.op("act", lambda e: e.activation(out=expb, in_=bias_f[:, 0:2], func=AF.Copy, scale=8.0), reads=[bfB], writes=[B["expb"]])
            P.op("pool", lambda e: e.memset(Sf, 0.0), writes=[B["Sf"]])
            P.op("pool", lambda e: e.memset(Sb, 0.0), writes=[B["Sb"]])
            P.barrier()

            for g in range(NTG):
                ar.reset(base_off)
                qaT = ar.bf(4 * 512).rearrange("p (q t) -> p q t", q=4)
                qlat = ar.bf(8 * 512).rearrange("p (h t) -> p h t", h=8)
                qiT = ar.bf(4 * 512).rearrange("p (q t) -> p q t", q=4)
                wtok = ar.f32(4 * 8).rearrange("p (j h) -> p j h", j=4)
                eb = ar.f32(4 * 512).rearrange("p (h t) -> p h t", h=4)
                Kt = ar.bf(4 * 512).rearrange("p (h t) -> p h t", h=4)
                Qt = ar.bf(4 * 512).rearrange("p (h t) -> p h t", h=4)
                Ktok = ar.bf(16 * 128).rearrange("p (j h d) -> p j h d", j=4, h=4)
                Vtok = ar.bf(4 * 512).rearrange("p (j n) -> p j n", j=4)
                sgT = ar.bf(4 * 512).rearrange("p (h t) -> p h t", h=4)
                boT = ar.bf(4 * 512).rearrange("p (h t) -> p h t", h=4)
                aoT = ar.bf(4 * 512).rearrange("p (q t) -> p q t", q=4)
                ph_off = ar.off
                hn = ar.bf(NCH * 512).rearrange("p (c t) -> p c t", c=NCH)
                wsl = [ar.bf(NCH * 512).rearrange("p (c n) -> p c n", c=NCH) for _ in range(3)]
                tA = [ar.f32(512) for _ in range(2)]
                tB = [ar.f32(512) for _ in range(2)]
                tC = [ar.f32(512) for _ in range(2)]
                craw = ar.f32(512)
                csq = ar.bf(512)
                scr = norm_scratch(7)
                L = {n: Buf(n) for n in "hn qaT qlat qiT wtok eb Kt Qt Ktok Vtok sgT boT aoT craw csq".split()}
                wsB = [Buf("ws%d" % i) for i in range(3)]
                tAB = [Buf("tA0"), Buf("tA1")]
                tBB = [Buf("tB0"), Buf("tB1")]
                tCB = [Buf("tC0"), Buf("tC1")]
                rmsnorm_tg(g, V_MIX0, hn, L["hn"], scr)
                pieces = [("qa", 0, 512), ("small", None, None), ("qi", 640, 1152), ("f", 1224, 1736), ("q", 2248, 2760),
                          ("i", 1736, 2248), ("g", 2760, 3272)]

                def loadw(pi):
                    nm, c0, c1 = pieces[pi]
                    s = pi % 3
                    if nm == "small":
                        P.dma("pool", wsl[s][:, :, 0:128], wview(e_w_in, 512, 640), writes=[wsB[s]])
                        P.dma("pool", wsl[s][:, :, 128:192], wview(e_w_in, 1152, 1216), writes=[wsB[s]])
                        P.dma("pool", wsl[s][:, :, 192:256], wview(e_w_in, 1152, 1216), writes=[wsB[s]])
                        P.dma("pool", wsl[s][:, :, 256:264], wview(e_w_in, 1216, 1224), writes=[wsB[s]])
                    else:
                        P.dma("pool", wsl[s], wview(e_w_in, c0, c1), writes=[wsB[s]])

                loadw(0)
                loadw(1)
                kq = 0
                for pi, (nm, c0, c1) in enumerate(pieces):
                    if pi + 2 < len(pieces):
                        loadw(pi + 2)
                    s = pi % 3
                    W = wsl[s]
                    WB = wsB[s]

                    def proj_fm(col0, ncol, pb):
                        for c in range(NCH):
                            mm(PS[pb][0:ncol, :], W[:, c, col0:col0 + ncol], hn[:, c, :], c == 0, c == NCH - 1, [WB, L["hn"]], [PB[pb]])

                    if nm == "qa":
                        for q4 in range(4):
                            pb = q4 % 2
                            proj_fm(q4 * 128, 128, pb)
                            P.op("act", lambda e, pb=pb, q4=q4: e.activation(out=qaT[:, q4, :], in_=PS[pb][:], func=AF.Copy), reads=[PB[pb]], writes=[L["qaT"]])
                        for hh in range(8):
                            pb = 2 + hh % 2
                            i2, q4 = hh % 2, hh // 2
                            mm(PS[pb][:], wukT[64 * i2:64 * i2 + 64, q4, :], qaT[64 * i2:64 * i2 + 64, q4, :], True, True, [B["wukT"], L["qaT"]], [PB[pb]])
                            P.op("dve", lambda e, pb=pb, hh=hh: e.tensor_copy(out=qlat[:, hh, :], in_=PS[pb][:]), reads=[PB[pb]], writes=[L["qlat"]])
                    elif nm == "small":
                        proj_fm(0, 128, 0)
                        P.op("act", lambda e: e.activation(out=craw, in_=PS[0][:], func=AF.Copy), reads=[PB[0]], writes=[L["craw"]])
                        P.op("act", lambda e: e.activation(out=csq, in_=PS[0][:], func=AF.Square), reads=[PB[0]], writes=[L["csq"]])
                        mm(PS[1][:], ones_b, csq, True, True, [cbB, L["csq"]], [PB[1]])
                        P.op("act", lambda e: e.activation(out=tA[0], in_=PS[1][:], func=AF.Ln, scale=1.0 / 128, bias=EPS), reads=[PB[1]], writes=[tAB[0]])
                        P.op("act", lambda e: e.activation(out=tA[0], in_=tA[0], func=AF.Exp, scale=-0.5), reads=[tAB[0]], writes=[tAB[0]])
                        P.op("dve", lambda e: e.scalar_tensor_tensor(out=cnT[:, tgs(g)], in0=craw, scalar=vecs[:, V_LAT:V_LAT + 1], in1=tA[0],
                                                                     op0=ALU.mult, op1=ALU.mult), reads=[L["craw"], vB, tAB[0]], writes=[B["cnT"]])
                        psb = PS[1][:].bitcast(BF16)
                        for j in range(4):
                            P.op("pe", lambda e, j=j: e.transpose(psb[:, j * 128:(j + 1) * 128], cnT[:, g * 512 + j * 128:g * 512 + (j + 1) * 128], ident),
                                 reads=[B["cnT"], cbB], writes=[PB[1]])
                        P.op("act", lambda e: e.activation(out=Ctok[:, 4 * g:4 * g + 4, :], in_=psb[:, 0:512].rearrange("p (j c) -> p j c", j=4), func=AF.Copy),
                             reads=[PB[1]], writes=[B["Ctok"]])
                        proj_fm(128, 128, 2)
                        P.op("act", lambda e: e.activation(out=kiT2[:, tgs(g)], in_=PS[2][:], func=AF.Copy), reads=[PB[2]], writes=[B["kiT2"]])
                        for j in range(4):
                            for c in range(NCH):
                                mm(PS[3][:, j * 8:(j + 1) * 8], hn[:, c, j * 128:(j + 1) * 128], W[:, c, 256:264], c == 0, c == NCH - 1, [WB, L["hn"]], [PB[3]])
                        P.op("act", lambda e: e.activation(out=wtok, in_=PS[3][:, 0:32].rearrange("p (j h) -> p j h", j=4), func=AF.Copy,
                                                           scale=0.125 * (8.0 ** -0.5)), reads=[PB[3]], writes=[L["wtok"]])
                    elif nm == "qi":
                        for q4 in range(4):
                            pb = q4 % 2
                            proj_fm(q4 * 128, 128, pb)
                            P.op("act", lambda e, pb=pb, q4=q4: e.activation(out=qiT[:, q4, :], in_=PS[pb][:], func=AF.Copy), reads=[PB[pb]], writes=[L["qiT"]])
                    elif nm == "f":
                        for hd in range(4):
                            pb = hd % 2
                            z = kq % 2
                            kq += 1
                            proj_fm(hd * 128, 128, pb)
                            P.op("act", lambda e, pb=pb, z=z: e.activation(out=tA[z], in_=PS[pb][:], func=AF.Sigmoid), reads=[PB[pb]], writes=[tAB[z]])
                            P.op("dve", lambda e, z=z, hd=hd: e.tensor_scalar(out=tA[z], in0=tA[z], scalar1=lbv[:, 4 + hd:5 + hd], scalar2=lbv[:, hd:hd + 1],
                                                                              op0=ALU.mult, op1=ALU.add), reads=[tAB[z], lbB], writes=[tAB[z]])
                            P.op("act", lambda e, z=z: e.activation(out=tB[z], in_=tA[z], func=AF.Ln), reads=[tAB[z]], writes=[tBB[z]])
                            for ch in range(8):
                                P.op("dve", lambda e, z=z, ch=ch: e.tensor_tensor_scan(out=tC[z][:, ch * 64:(ch + 1) * 64], data0=ones_f[:, 0:64],
                                                                                       data1=tB[z][:, ch * 64:(ch + 1) * 64], initial=0.0,
                                                                                       op0=ALU.mult, op1=ALU.add), reads=[tBB[z], cB], writes=[tCB[z]])
                            P.op("act", lambda e, z=z, hd=hd: e.activation(out=eb[:, hd, :], in_=tC[z], func=AF.Exp), reads=[tCB[z]], writes=[L["eb"]])
                            P.op("act", lambda e, z=z: e.activation(out=tB[z], in_=tC[z], func=AF.Exp, scale=-1.0), reads=[tCB[z], tBB[z]], writes=[tBB[z]])
                            P.op("dve", lambda e, z=z: e.tensor_scalar(out=tA[z], in0=tA[z], scalar1=-1.0, scalar2=1.0, op0=ALU.mult, op1=ALU.add),
                                 reads=[tAB[z]], writes=[tAB[z]])
                            P.op("dve", lambda e, z=z, hd=hd: e.tensor_tensor(out=Kt[:, hd, :], in0=tA[z], in1=tB[z], op=ALU.mult),
                                 reads=[tAB[z], tBB[z]], writes=[L["Kt"]])
                        for j in range(4):
                            pb = 2 + j % 2
                            psb = PS[pb][:].bitcast(BF16)
                            for hd in range(4):
                                P.op("pe", lambda e, j=j, hd=hd, psb=psb: e.transpose(psb[:, hd * 128:(hd + 1) * 128], Kt[:, hd, j * 128:(j + 1) * 128], ident),
                                     reads=[L["Kt"], cbB], writes=[PB[pb]])
                            P.op("act", lambda e, j=j, psb=psb: e.activation(out=Ktok[:, j], in_=psb[:, 0:512].rearrange("p (h d) -> p h d", h=4), func=AF.Copy),
                                 reads=[PB[pb]], writes=[L["Ktok"]])
                    elif nm == "q":
                        for hd in range(4):
                            pb = hd % 2
                            z = kq % 2
                            kq += 1
                            proj_fm(hd * 128, 128, pb)
                            P.op("act", lambda e, pb=pb, z=z: e.activation(out=tA[z], in_=PS[pb][:], func=AF.Silu), reads=[PB[pb]], writes=[tAB[z]])
                            P.op("dve", lambda e, z=z, hd=hd: e.tensor_tensor(out=Qt[:, hd, :], in0=tA[z], in1=eb[:, hd, :], op=ALU.mult),
                                 reads=[tAB[z], L["eb"]], writes=[L["Qt"]])
                    elif nm == "i":
                        for j in range(4):
                            pb = j % 2
                            for c in range(NCH):
                                mm(PS[pb][:], hn[:, c, j * 128:(j + 1) * 128], W[:, c, :], c == 0, c == NCH - 1, [WB, L["hn"]], [PB[pb]])
                            P.op("act", lambda e, pb=pb, j=j: e.activation(out=Vtok[:, j, :], in_=PS[pb][:], func=AF.Copy), reads=[PB[pb]], writes=[L["Vtok"]])
                    elif nm == "g":
                        for hd in range(4):
                            pb = hd % 2
                            proj_fm(hd * 128, 128, pb)
                            P.op("act", lambda e, pb=pb, hd=hd: e.activation(out=sgT[:, hd, :], in_=PS[pb][:], func=AF.Silu), reads=[PB[pb]], writes=[L["sgT"]])

                P.barrier()
                ar.reset(ph_off)
                Sm = [ar.bf(256) for _ in range(2)]
                SmB = [Buf("Sm0"), Buf("Sm1")]
                tS = ar.f32(512)
                tSB = Buf("tS")
                osq = ar.bf(512)
                osqB = Buf("osq")
                rst = ar.f32(512)
                rstB = Buf("rst")
                t1 = ar.f32(512)
                t1B = Buf("t1")
                first = (g == 0)
                for ch in (range(0) if 'hgrn' in KSKIP else range(8)):
                    j, p0 = ch // 2, 64 * (ch % 2)
                    cs = slice(ch * 64, (ch + 1) * 64)
                    sm = ch % 2
                    for hd in range(4):
                        mm(PS[4][p0:p0 + 64, hd * 64:(hd + 1) * 64], Kt[:, hd, cs], Qt[:, hd, cs], True, True, [L["Kt"], L["Qt"]], [PB[4]])
                    P.op("dve", lambda e, p0=p0, sm=sm: e.tensor_tensor(out=Sm[sm][p0:p0 + 64, :].rearrange("p (h t) -> p h t", h=4),
                                                                       in0=PS[4][p0:p0 + 64, 0:256].rearrange("p (h t) -> p h t", h=4),
                                                                       in1=triu_f[p0:p0 + 64, :].unsqueeze(1).to_broadcast([64, 4, 64]), op=ALU.mult),
                         reads=[PB[4], cB], writes=[SmB[sm]])
                    for hd in range(4):
                        noS = first and ch == 0
                        mm(PS[hd][:, cs], Vtok[p0:p0 + 64, j, hd * 128:(hd + 1) * 128], Sm[sm][p0:p0 + 64, hd * 64:(hd + 1) * 64], True, noS,
                           [L["Vtok"], SmB[sm]], [PB[hd]])
                        if not noS:
                            mm(PS[hd][:, cs], Sb[:, hd * 128:(hd + 1) * 128], Qt[:, hd, cs], False, True, [B["Sb"], L["Qt"]], [PB[hd]])
                    for hd in range(4):
                        mm(PS[5][:, hd * 128:(hd + 1) * 128], Ktok[p0:p0 + 64, j, hd, :], Vtok[p0:p0 + 64, j, hd * 128:(hd + 1) * 128], True, True,
                           [L["Ktok"], L["Vtok"]], [PB[5]])
                    P.op("dve", lambda e: e.tensor_tensor(out=tS, in0=Sf, in1=PS[5][:], op=ALU.add), reads=[B["Sf"], PB[5]], writes=[tSB])
                    aend = eb[:, :, ch * 64 + 63:ch * 64 + 64]
                    P.op("dve", lambda e, aend=aend: e.tensor_tensor(out=Sf.rearrange("p (h v) -> p h v", h=4), in0=tS.rearrange("p (h v) -> p h v", h=4),
                                                                     in1=aend.to_broadcast([128, 4, 128]), op=ALU.mult),
                         reads=[tSB, L["eb"]], writes=[B["Sf"]])
                    P.op("act", lambda e: e.activation(out=Sb, in_=Sf, func=AF.Copy), reads=[B["Sf"]], writes=[B["Sb"]])
                for hd in range(4):
                    P.op("act", lambda e, hd=hd: e.activation(out=osq, in_=PS[hd][:], func=AF.Square), reads=[PB[hd]], writes=[osqB])
                    mm(PS[6][:], ones_b, osq, True, True, [cbB, osqB], [PB[6]])
                    P.op("act", lambda e: e.activation(out=rst, in_=PS[6][:], func=AF.Ln, scale=1.0 / 128, bias=EPS), reads=[PB[6]], writes=[rstB])
                    P.op("act", lambda e: e.activation(out=rst, in_=rst, func=AF.Exp, scale=-0.5), reads=[rstB], writes=[rstB])
                    P.op("dve", lambda e, hd=hd: e.scalar_tensor_tensor(out=t1, in0=PS[hd][:], scalar=vecs[:, V_ON:V_ON + 1], in1=rst, op0=ALU.mult, op1=ALU.mult),
                         reads=[PB[hd], vB, rstB], writes=[t1B])
                    P.op("dve", lambda e, hd=hd: e.tensor_tensor(out=boT[:, hd, :], in0=t1, in1=sgT[:, hd, :], op=ALU.mult), reads=[t1B, L["sgT"]], writes=[L["boT"]])

                P.barrier()
                ar.reset(ph_off)
                score2 = [ar.f32(T) for _ in range(2)]
                Rr = [ar.bf(512) for _ in range(4)]
                rrow = ar.f32(512)
                msk = ar.bf(T)
                junk = msk
                maskT2 = [ar.bf(NT * 128).rearrange("p (i t) -> p i t", i=NT) for _ in range(2)]
                ET = [ar.bf(1024).rearrange("p (h t) -> p h t", h=8) for _ in range(3)]
                st_ = ar.f32(8 + 2 * (NIT + 1))
                rd = ar.f32(1024)
                olat = ar.bf(1024).rearrange("p (h t) -> p h t", h=8)
                wdiag = ar.bf(4 * 8 * 128).rearrange("p (j h t) -> p j h t", j=4, h=8)
                D3 = {n: Buf(n) for n in "msk st rd olat wdiag rrow".split()}
                D3["junk"] = D3["msk"]
                scB = [Buf("score0"), Buf("score1")]
                mTB = [Buf("maskT0"), Buf("maskT1")]
                RB = [Buf("R%d" % i) for i in range(4)]
                ETB = [Buf("ET0"), Buf("ET1"), Buf("ET2")]
                P7 = [Buf("ps7a"), Buf("ps7b")]
                P6 = [Buf("ps6a"), Buf("ps6b")]
                thr, cnt, tt, amax = st_[:, 0:1], st_[:, 1:2], st_[:, 2:3], st_[:, 3:4]
                dl = st_[:, 8:8 + NIT + 1]
                cnts = {"kr": 0, "ke": 0, "kf": 0}
                ident_f = cstf[:, C_ID:C_ID + 128]
                for jt in range(4):
                    P.op("dve", lambda e: e.tensor_tensor(out=wdiag[:, jt], in0=ident_f.unsqueeze(1).to_broadcast([128, 8, 128]),
                                                          in1=wtok[:, jt, :].unsqueeze(2).to_broadcast([128, 8, 128]), op=ALU.mult),
                         reads=[cB, L["wtok"]], writes=[D3["wdiag"]])

                def gen_I(jt):
                    J = 4 * g + jt
                    n2 = 128 * (J + 1)
                    tcol = slice(jt * 128, (jt + 1) * 128)
                    score = score2[jt % 2]
                    sB = scB[jt % 2]
                    nblk = (n2 + 511) // 512
                    steps = [(blk, hh) for blk in range(nblk) for hh in range(8)]
                    k0 = cnts["kr"]
                    cnts["kr"] += len(steps)
                    dbank = (5, 7)

                    def dots(k):
                        blk, hh = steps[k]
                        s0 = blk * 512
                        ns = min(512, n2 - s0)
                        pb = dbank[(k0 + k) % 2]
                        i2, q4 = hh % 2, hh // 2
                        mm(PS[pb][:, 0:ns], qiT[64 * i2:64 * i2 + 64, q4, tcol], kiT2[64 * i2:64 * i2 + 64, s0:s0 + ns], True, True,
                           [L["qiT"], B["kiT2"]], [PB[pb]])

                    dots(0)
                    for k, (blk, hh) in enumerate(steps):
                        s0 = blk * 512
                        ns = min(512, n2 - s0)
                        pb = dbank[(k0 + k) % 2]
                        r4 = (k0 + k) % 4
                        if k + 1 < len(steps):
                            dots(k + 1)
                        P.op("act", lambda e: e.activation(out=Rr[r4][:, 0:ns], in_=PS[pb][:, 0:ns], func=AF.Relu),
                             reads=[PB[pb]], writes=[RB[r4]])
                        mm(PS[6][:, 0:ns], wdiag[:, jt, hh, :], Rr[r4][:, 0:ns], hh == 0, hh == 7, [D3["wdiag"], RB[r4]], [PB[6]])
                        if hh == 7:
                            P.op("act", lambda e: e.activation(out=score[:, s0:s0 + ns], in_=PS[6][:, 0:ns], func=AF.Copy),
                                 reads=[PB[6]], writes=[sB])
                        yield "head"

                def gen_S(jt):
                    J = 4 * g + jt
                    n2 = 128 * (J + 1)
                    score = score2[jt % 2]
                    sB = scB[jt % 2]
                    maskT = maskT2[jt % 2]
                    mB = mTB[jt % 2]
                    if n2 > KTOP:
                        P.op("dve", lambda e: e.tensor_reduce(out=amax, in_=score[:, 0:n2], axis=AX.X, op=ALU.max, apply_absolute_value=True),
                             reads=[sB], writes=[D3["st"]])
                        P.op("dve", lambda e: e.tensor_scalar(out=dl, in0=pow_f, scalar1=amax, scalar2=None, op0=ALU.mult), reads=[D3["st"], cB], writes=[D3["st"]])
                        P.op("dve", lambda e: e.memset(thr, 0.0), reads=[D3["st"]], writes=[D3["st"]])
                    P.op("dve", lambda e: e.memset(score[0:64, n2 - 64:n2], NEG), reads=[sB], writes=[sB])
                    yield "it"
                    if n2 > KTOP:
                        for it in range(NIT):
                            P.op("dve", lambda e: e.tensor_scalar(out=junk[:, 0:n2], in0=score[:, 0:n2], scalar1=thr, scalar2=None, op0=ALU.is_ge, op1=ALU.add,
                                                                  accum_out=cnt), reads=[sB, D3["st"]], writes=[D3["junk"], D3["st"]])
                            P.op("dve", lambda e: e.tensor_scalar(out=tt, in0=cnt, scalar1=KTOP - 0.5, scalar2=dl[:, it:it + 1], op0=ALU.is_ge, op1=ALU.mult),
                                 reads=[D3["st"]], writes=[D3["st"]])
                            P.op("dve", lambda e: e.scalar_tensor_tensor(out=thr, in0=tt, scalar=dl[:, it + 1:it + 2], in1=thr, op0=ALU.subtract, op1=ALU.add),
                                 reads=[D3["st"]], writes=[D3["st"]])
                            yield "it"
                    else:
                        P.op("dve", lambda e: e.memset(thr, -1.0e29), reads=[D3["st"]], writes=[D3["st"]])
                    P.op("dve", lambda e: e.tensor_scalar(out=msk[:, 0:n2], in0=score[:, 0:n2], scalar1=thr, scalar2=-30000.0, op0=ALU.is_lt, op1=ALU.mult),
                         reads=[sB, D3["st"]], writes=[D3["msk"]])
                    yield "mask"
                    for i0 in range(0, J + 1, 8):
                        ni = min(8, J + 1 - i0)
                        psb = PS[7][:].bitcast(BF16)
                        for ii in range(ni):
                            P.op("pe", lambda e: e.transpose(psb[:, ii * 128:(ii + 1) * 128], msk[:, (i0 + ii) * 128:(i0 + ii + 1) * 128], ident),
                                 reads=[D3["msk"], cbB], writes=[PB[7]])
                        P.op("act", lambda e: e.activation(out=maskT[:, i0:i0 + ni, :], in_=psb[:, 0:ni * 128].rearrange("p (i t) -> p i t", i=ni),
                                                           func=AF.Copy), reads=[PB[7]], writes=[mB])
                        yield "tr"

                def gen_B(jt):
                    J = 4 * g + jt
                    tcol = slice(jt * 128, (jt + 1) * 128)
                    maskT = maskT2[jt % 2]
                    mB = mTB[jt % 2]
                    for i in range(J + 1):
                        e2 = cnts["ke"] % 3
                        cnts["ke"] += 1
                        near = i >= J - 1
                        kind = 0 if i == J else 1
                        for hf in range(2):
                            o3 = PS[hf][:].rearrange("p (h t) -> p h t", h=4)
                            mm(o3, cnT[:, i * 128:(i + 1) * 128], qlat[:, 4 * hf:4 * hf + 4, tcol], True, False, [B["cnT"], L["qlat"]], [PB[hf]])
                            mm(o3, ident, maskT[:, i:i + 1, :].to_broadcast([128, 4, 128]), False, not near, [cbB, mB], [PB[hf]])
                            if near:
                                mm(o3, ident, expb[:, kind, 4 * hf:4 * hf + 4, :], False, True, [cbB, B["expb"]], [PB[hf]])
                            P.op("act", lambda e: e.activation(out=ET[e2][:, 4 * hf:4 * hf + 4, :], in_=PS[hf][:].rearrange("p (h t) -> p h t", h=4),
                                                               func=AF.Exp, scale=0.125), reads=[PB[hf]], writes=[ETB[e2]])
                        for hf in range(2):
                            mm(PS[2 + hf][:].rearrange("p (h t) -> p h t", h=4), Ctok[:, i, :], ET[e2][:, 4 * hf:4 * hf + 4, :], i == 0, i == J,
                               [B["Ctok"], ETB[e2]], [PB[2 + hf]])
                        for hf in range(2):
                            mm(PS[4][32 * hf:32 * hf + 1, :].rearrange("p (h t) -> p h t", h=4), ones_b[:, 0:1], ET[e2][:, 4 * hf:4 * hf + 4, :], i == 0, i == J,
                               [cbB, ETB[e2]], [PB[4]])
                        yield "pair"
                    yield "pairs_done"
                    ones_f128 = cstf[:, C_ONE:C_ONE + 128]
                    for hf in range(2):
                        rr = rrow[32 * hf:32 * hf + 1, :]
                        P.op("act", lambda e: e.activation(out=rr, in_=PS[4][32 * hf:32 * hf + 1, :], func=AF.Ln), reads=[PB[4]], writes=[D3["rrow"]])
                        P.op("act", lambda e: e.activation(out=rr, in_=rr, func=AF.Exp, scale=-1.0), reads=[D3["rrow"]], writes=[D3["rrow"]])
                        mm(PS[hf][:], ones_f128[32 * hf:32 * hf + 1, :], rr, True, True, [cB, D3["rrow"]], [PB[hf]])
                        P.op("act", lambda e: e.activation(out=rd[:, hf * 512:(hf + 1) * 512], in_=PS[hf][:], func=AF.Copy), reads=[PB[hf]], writes=[D3["rd"]])
                        P.op("dve", lambda e: e.tensor_tensor(out=olat[:, 4 * hf:4 * hf + 4, :], in0=PS[2 + hf][:].rearrange("p (h t) -> p h t", h=4),
                                                              in1=rd[:, hf * 512:(hf + 1) * 512].rearrange("p (h t) -> p h t", h=4), op=ALU.mult),
                             reads=[PB[2 + hf], D3["rd"]], writes=[D3["olat"]])
                    yield
                    pb = 0
                    for hh in range(8):
                        i2, q4 = hh % 2, hh // 2
                        mm(PS[pb][64 * i2:64 * i2 + 64, q4 * 128:(q4 + 1) * 128], wuv[:, hh, :], olat[:, hh, :], True, True, [B["wuv"], D3["olat"]], [PB[pb]])
                    P.op("act", lambda e: e.activation(out=aoT[:, :, tcol], in_=PS[pb][:].rearrange("p (q t) -> p q t", q=4), func=AF.Copy),
                         reads=[PB[pb]], writes=[L["aoT"]])
                    yield

                if 'dsa' not in KSKIP:
                    def n_I(jt):
                        return ((128 * (4 * g + jt + 1) + 511) // 512) * 8
                    for step in range(6):
                        gi = gen_I(step) if step < 4 else None
                        gs = gen_S(step - 1) if 0 <= step - 1 < 4 else None
                        gb = gen_B(step - 2) if 0 <= step - 2 < 4 else None
                        nS = NIT + 2
                        rI = max(1, -(-n_I(step) // nS)) if gi is not None else 0
                        rB = max(1, -(-(4 * g + step - 2 + 2) // nS)) if gb is not None else 0
                        s_bisect_done = gs is None
                        b_parked = False
                        while gi is not None or gs is not None or (gb is not None):
                            if gs is not None:
                                try:
                                    if next(gs) == "mask":
                                        s_bisect_done = True
                                except StopIteration:
                                    gs = None
                                    s_bisect_done = True
                            if gb is not None:
                                for _ in range(rB):
                                    if b_parked and not s_bisect_done:
                                        break
                                    try:
                                        if next(gb) == "pairs_done":
                                            b_parked = True
                                    except StopIteration:
                                        gb = None
                                        break
                            if gi is not None:
                                for _ in range(rI):
                                    try:
                                        next(gi)
                                    except StopIteration:
                                        gi = None
                                        break
                P.barrier()
                ar.reset(ph_off)
                wo2 = ar.bf(NCH * D).rearrange("p (c n) -> p c n", c=NCH)
                wo2B = [Buf("wo2_%d" % i) for i in range(4)]
                for i4 in range(4):
                    P.dma("pool", wo2[:, :, i4 * 256:(i4 + 1) * 256], wview(e_w_out, i4 * 256, (i4 + 1) * 256), writes=[wo2B[i4]])
                for dc in range(NCH):
                    pb = dc % 2
                    for c in range(NCH):
                        src = aoT[:, c, :] if c < 4 else boT[:, c - 4, :]
                        mm(PS[pb][:], wo2[:, c, dc * 128:(dc + 1) * 128], src, c == 0, c == NCH - 1, [wo2B[dc // 2], L["aoT"], L["boT"]], [PB[pb]])
                    P.op("dve", lambda e, pb=pb, dc=dc, g=g: e.tensor_tensor(out=h[:, dc, tgs(g)], in0=h[:, dc, tgs(g)], in1=PS[pb][:], op=ALU.add),
                         reads=[PB[pb], hB[dc][g]], writes=[hB[dc][g]])
                P.barrier()

        def final_stage(b, do_norm):
            P.barrier()
            ar.reset()
            of = [ar.f32(NCH * 512).rearrange("p (c t) -> p c t", c=NCH) for _ in range(2)]
            ofB = [Buf("of0"), Buf("of1")]
            sq = ar.bf(NCH * 512).rearrange("p (c t) -> p c t", c=NCH)
            lnv = ar.f32(512)
            sqB, lnB = Buf("sq"), Buf("lnv")
            for g in range(NTG):
                q = g % 2
                if do_norm:
                    P.op("act", lambda e, g=g: e.activation(out=sq, in_=h[:, :, tgs(g)], func=AF.Square), reads=[hB[c][g] for c in range(NCH)], writes=[sqB])
                    for c in range(NCH):
                        mm(PS[0][:], ones_b, sq[:, c, :], c == 0, c == NCH - 1, [cbB, sqB], [PB[0]])
                    P.op("act", lambda e: e.activation(out=lnv, in_=PS[0][:], func=AF.Ln, scale=1.0 / D, bias=EPS), reads=[PB[0]], writes=[lnB])
                    P.op("act", lambda e: e.activation(out=lnv, in_=lnv, func=AF.Exp, scale=-0.5), reads=[lnB], writes=[lnB])
                    for c in range(NCH):
                        P.op("dve", lambda e, c=c, g=g, q=q: e.scalar_tensor_tensor(out=of[q][:, c, :], in0=h[:, c, tgs(g)], scalar=vecs[:, V_FIN + c:V_FIN + c + 1],
                                                                                  in1=lnv, op0=ALU.mult, op1=ALU.mult),
                             reads=[hB[c][g], vB, lnB], writes=[ofB[q]])
                    P.dma("sp", outT[b].rearrange("(c p) t -> p c t", p=128)[:, :, tgs(g)], of[q], reads=[ofB[q]])
                else:
                    P.dma("sp", outT[b].rearrange("(c p) t -> p c t", p=128)[:, :, tgs(g)], h[:, :, tgs(g)], reads=[hB[c][g] for c in range(NCH)])

        ones_f_t = sb("ones_f", [128, 64], F32)
        ones_f = ones_f_t[:]
        P.op("pool", lambda e: e.memset(ones_f, 1.0), writes=[cB])
        for b in range(NB):
            P.barrier()
            for g in range(NTG):
                P.dma("sp", h[:, :, tgs(g)], xT[b].rearrange("(c p) t -> p c t", p=128)[:, :, tgs(g)], writes=[hB[c][g] for c in range(NCH)])
            for s in stages:
                if s == "mix0":
                    mixer0_stage(b)
                elif s == "xa0":
                    xattn_stage(0, b)
                elif s == "ffn0":
                    ffn_stage(0)
                elif s == "mix1":
                    gmlp_stage()
                elif s == "xa1":
                    xattn_stage(1, b)
                elif s == "ffn1":
                    ffn_stage(1)
            final_stage(b, "fin" in stages)
        P.barrier()
        P.emit()
        nops = P.nops
    return nc, nops


def _t5_bucket(rel):
    nb = 16
    max_exact = 8
    ret = (rel > 0).astype(np.int64) * nb
    n = np.abs(rel)
    nf = np.maximum(n, 1).astype(np.float32)
    large = max_exact + (np.log(nf / max_exact) / math.log(128 / max_exact) * (nb - max_exact)).astype(np.int32)
    large = np.minimum(large, nb - 1)
    return ret + np.where(n < max_exact, n, large)


def _consts():
    c = np.zeros((128, NCST), np.float32)
    c[:, C_ID:C_ID + 128] = np.eye(128, dtype=np.float32)
    c[:, C_ONE:C_ONE + 128] = 1.0
    p = np.arange(128)
    c[:, C_TRIU:C_TRIU + 64] = ((p[:, None] % 64) <= np.arange(64)[None, :]).astype(np.float32)
    c[:, C_TRIL:C_TRIL + 128] = (p[:, None] <= np.arange(128)[None, :]).astype(np.float32)
    c[:, C_POW:C_POW + NIT + 1] = (0.5 ** np.arange(NIT + 1))[None, :].astype(np.float32)
    return c


def _fm(v):
    return np.ascontiguousarray(np.asarray(v, np.float32).reshape(8, 128).T)


def make_common(inp):
    f = lambda k: np.asarray(inp[k], np.float32)
    vec = np.zeros((128, NV), np.float32)
    vec[:, V_MIX0:V_MIX0 + 8] = _fm(f("mix_norm")[0])
    vec[:, V_MIX1:V_MIX1 + 8] = _fm(f("mix_norm")[1])
    vec[:, V_X0:V_X0 + 8] = _fm(f("x_norm")[0])
    vec[:, V_X1:V_X1 + 8] = _fm(f("x_norm")[1])
    vec[:, V_F0:V_F0 + 8] = _fm(f("f_norm")[0])
    vec[:, V_F1:V_F1 + 8] = _fm(f("f_norm")[1])
    vec[:, V_M0:V_M0 + 8] = _fm(f("mem_norm")[0])
    vec[:, V_M1:V_M1 + 8] = _fm(f("mem_norm")[1])
    vec[:, V_FIN:V_FIN + 8] = _fm(f("final_norm"))
    vec[:, V_LAT] = f("e_lat_norm")[0]
    vec[:, V_ON] = f("e_o_norm")[0]
    vec[:, V_LB:V_LB + 12] = f("hgrn_lb").reshape(3, 4, 128).transpose(2, 0, 1).reshape(128, 12)
    ps = np.arange(128)[:, None]
    pt = np.arange(128)[None, :]
    rb = f("rel_bias")
    tiles = np.zeros((128, 3, 8, 128), np.float32)
    for kind, off in enumerate((0, -128)):
        bk = _t5_bucket(ps - pt + off)
        tiles[:, kind] = rb[bk].transpose(0, 2, 1)
    tiles[:, 2] = rb[15][None, :, None]
    wuk = f("e_w_uk")[0]
    wukT = wuk.reshape(4, 2, 128, 64).transpose(1, 3, 0, 2).reshape(128, 4 * 128)
    wuv = f("e_w_uv")[0].transpose(1, 0, 2).reshape(128, 8 * 64)
    wsp = f("o_w_sp")[0].transpose(2, 0, 1).reshape(128, 8 * 128)
    return {
        "vecs": vec, "cst": _consts(), "biasT": np.ascontiguousarray(tiles.reshape(128, -1)),
        "e_w_in": np.ascontiguousarray(f("e_w_in")[0]), "e_w_ukT": np.ascontiguousarray(wukT), "e_w_uv": np.ascontiguousarray(wuv),
        "e_w_out": np.ascontiguousarray(f("e_w_out")[0]), "o_w_in": np.ascontiguousarray(f("o_w_in")[0]),
        "o_ln_g": f("o_ln_g").reshape(1, D), "o_ln_b": f("o_ln_b").reshape(1, D), "o_w_spT": np.ascontiguousarray(wsp),
        "o_b_sp": f("o_b_sp").reshape(1, 8 * 128), "o_w_out": np.ascontiguousarray(f("o_w_out")[0]),
        "x_wq": f("x_wq"), "x_wkv": f("x_wkv"), "x_wo": f("x_wo"), "f_w_gu": f("f_w_gu"), "f_w_down": f("f_w_down"),
    }


_CACHE = {}


def kernel(**inputs):
    x = np.asarray(inputs["x"], np.float32)
    mem = np.asarray(inputs["mem"], np.float32)
    Bn, T, _ = x.shape
    ncores = 8
    NB = Bn // ncores
    stages = tuple(os.environ.get("KSTAGES", "mix0,xa0,ffn0,mix1,xa1,ffn1,fin").split(","))
    key = (T, NB, stages)
    if key not in _CACHE:
        _CACHE[key] = build(T, NB, stages=stages)[0]
    nc = _CACHE[key]
    common = make_common(inputs)
    xT = np.ascontiguousarray(x.transpose(0, 2, 1))
    mT = np.ascontiguousarray(mem.transpose(0, 2, 1))
    in_maps = []
    for i in range(ncores):
        m = dict(common)
        m["xT"] = xT[i * NB:(i + 1) * NB]
        m["memT"] = mT[i * NB:(i + 1) * NB]
        in_maps.append(m)
    res = run_bass_kernel_spmd(nc, in_maps, core_ids=list(range(ncores)))
    outs = [r["outT"] for r in res.results]
    o = np.concatenate(outs, axis=0)
    return np.ascontiguousarray(o.transpose(0, 2, 1)).astype(np.float32)
```

```python
import math
import os
import numpy as np
import concourse.bass as bass
import concourse.mybir as mybir
from concourse.bass_utils import run_bass_kernel_spmd
from contextlib import ExitStack

F32 = mybir.dt.float32
BF16 = mybir.dt.bfloat16
AF = mybir.ActivationFunctionType
ALU = mybir.AluOpType
AX = mybir.AxisListType

D = 1024
NCH = 8
FF = 2816
NFC = 22
MEM = 256
EPS = 1e-6
NIT = int(os.environ.get('KNIT', '14'))
KSKIP = os.environ.get('KSKIP', '')
P_EVEN = 3272
NEG = -1.0e30

V_MIX0, V_MIX1, V_X0, V_X1, V_F0, V_F1, V_M0, V_M1, V_FIN, V_LAT, V_ON, V_LB = 0, 8, 16, 24, 32, 40, 48, 56, 64, 72, 73, 74
NV = 86
C_ID, C_ONE, C_TRIU, C_TRIL, C_POW = 0, 128, 256, 320, 448
NCST = 448 + NIT + 1


class Buf:
    __slots__ = ("name", "w", "r")

    def __init__(self, name=""):
        self.name = name
        self.w = None
        self.r = []


class _Rec:
    def __init__(self):
        self.call = None

    def __getattr__(self, name):
        def f(*a, **k):
            self.call = (name, a, k)
            return self
        return f


class Prog:
    CE = ("pe", "act", "dve", "pool")
    NDS = 40

    def __init__(self, nc, stack):
        self.nc = nc
        self.ops = {e: [] for e in ("pe", "act", "dve", "pool", "sp")}
        self.esem = {e: stack.enter_context(nc.semaphore("es_" + e)) for e in self.CE}
        self.cnt = {e: 0 for e in self.CE}
        self.rings = {}
        for q, n in (("sp", 24), ("pool", 32)):
            self.rings[q] = {"sems": [stack.enter_context(nc.semaphore("d%s%d" % (q, i))) for i in range(n)], "val": [0] * n, "next": 0}
        self.waited = {e: {} for e in self.ops}
        self.nops = 0

    def _need(self, eng, ev, waits):
        if ev is None:
            return
        sem, val, _ = ev
        k = id(sem)
        if self.waited[eng].get(k, 0) >= val:
            return
        cur = waits.get(k)
        if cur is None or cur[1] < val:
            waits[k] = (sem, val)

    def _deps(self, eng, reads, writes):
        waits = {}
        for b in reads:
            ev = b.w
            if ev is not None:
                if ev[2] == eng and eng == "pe":
                    continue
                self._need(eng, ev, waits)
        for b in writes:
            ev = b.w
            if ev is not None and not (ev[2] == eng and eng == "pe"):
                self._need(eng, ev, waits)
            for ev in b.r:
                if ev[2] == eng and eng == "pe":
                    continue
                self._need(eng, ev, waits)
        for k, (sem, val) in waits.items():
            self.waited[eng][k] = val
        return list(waits.values())

    def _commit(self, ev, reads, writes):
        for b in reads:
            lst = [e for e in b.r if e[0] is not ev[0]]
            lst.append(ev)
            b.r = lst
        for b in writes:
            b.w = ev
            b.r = []

    def op(self, eng, fn, reads=(), writes=()):
        rec = _Rec()
        fn(rec)
        name_, a_, k_ = rec.call
        fn = (lambda e, name_=name_, a_=a_, k_=k_: getattr(e, name_)(*a_, **k_))
        waits = self._deps(eng, reads, writes)
        self.cnt[eng] += 1
        ev = (self.esem[eng], self.cnt[eng], eng)
        self.ops[eng].append((waits, fn, ev[0], 1))
        self._commit(ev, reads, writes)
        self.nops += 1

    def dma(self, q, out, in_, reads=(), writes=()):
        waits = self._deps(q, reads, writes)
        ring = self.rings[q]
        i = ring["next"]
        ring["next"] = (i + 1) % len(ring["sems"])
        sem = ring["sems"][i]
        if ring["val"][i] > 0:
            k = id(sem)
            if self.waited[q].get(k, 0) < ring["val"][i]:
                waits.append((sem, ring["val"][i]))
                self.waited[q][k] = ring["val"][i]
        ring["val"][i] += 16
        ev = (sem, ring["val"][i], "dma")
        self.ops[q].append((waits, (lambda e, out=out, in_=in_: e.dma_start(out=out, in_=in_)), sem, 16))
        self._commit(ev, reads, writes)
        self.nops += 1

    def barrier(self):
        for e in self.ops:
            waits = []
            for x in self.CE:
                if x != e and self.cnt[x] > 0:
                    k = id(self.esem[x])
                    if self.waited[e].get(k, 0) < self.cnt[x]:
                        waits.append((self.esem[x], self.cnt[x]))
                        self.waited[e][k] = self.cnt[x]
            for ring in self.rings.values():
                for sem, val in zip(ring["sems"], ring["val"]):
                    if val > 0:
                        k = id(sem)
                        if self.waited[e].get(k, 0) < val:
                            waits.append((sem, val))
                            self.waited[e][k] = val
            if waits:
                self.ops[e].append((waits, None, None, 0))

    def emit(self):
        nc = self.nc
        with nc.Block() as block:
            def run(e, lst):
                for waits, fn, sem, inc in lst:
                    for (s, v) in waits:
                        e.wait_ge(s, v)
                    if fn is not None:
                        ins = fn(e)
                        if sem is not None:
                            ins.then_inc(sem, inc)

            @block.tensor
            def _(e):
                run(e, self.ops["pe"])

            @block.scalar
            def _(e):
                run(e, self.ops["act"])

            @block.vector
            def _(e):
                run(e, self.ops["dve"])

            @block.gpsimd
            def _(e):
                run(e, self.ops["pool"])

            @block.sync
            def _(e):
                run(e, self.ops["sp"])


class Arena:
    def __init__(self, ap):
        self.ap = ap
        self.n = ap.shape[1]
        self.off = 0

    def reset(self, off=0):
        self.off = off

    def bf(self, n):
        n16 = (n + 15) // 16 * 16
        assert self.off + n16 <= self.n, ("arena overflow", self.off, n16, self.n)
        a = self.ap[:, self.off:self.off + n]
        self.off += n16
        return a

    def f32(self, n):
        n16 = (2 * n + 15) // 16 * 16
        assert self.off + n16 <= self.n, ("arena overflow", self.off, n16, self.n)
        a = self.ap[:, self.off:self.off + 2 * n].bitcast(F32)
        self.off += n16
        return a


def build(T, NB, stages=("mix0", "xa0", "ffn0", "mix1", "xa1", "ffn1", "fin"), KTOP=None):
    NTG = T // 512
    NT = T // 128
    if KTOP is None:
        KTOP = min(256, T // 4)
    nc = bass.Bass("TRN2", target_bir_lowering=False)

    def din(name, shape):
        return nc.dram_tensor(name, list(shape), F32, kind="ExternalInput").ap()

    xT = din("xT", [NB, D, T])
    memT = din("memT", [NB, D, MEM])
    vecs_d = din("vecs", [128, NV])
    cst_d = din("cst", [128, NCST])
    bias_d = din("biasT", [128, 3 * 8 * 128])
    e_w_in = din("e_w_in", [D, P_EVEN])
    e_w_ukT = din("e_w_ukT", [128, 4 * 128])
    e_w_uv = din("e_w_uv", [128, 8 * 64])
    e_w_out = din("e_w_out", [D, D])
    o_w_in = din("o_w_in", [D, 2 * D])
    o_ln_g = din("o_ln_g", [1, D])
    o_ln_b = din("o_ln_b", [1, D])
    o_w_spT = din("o_w_spT", [128, 8 * 128])
    o_b_sp = din("o_b_sp", [1, 8 * 128])
    o_w_out = din("o_w_out", [D, D])
    x_wq = din("x_wq", [2, D, 512])
    x_wkv = din("x_wkv", [2, D, 1024])
    x_wo = din("x_wo", [2, 512, D])
    f_w_gu = din("f_w_gu", [2, D, 2 * FF])
    f_w_down = din("f_w_down", [2, FF, D])
    outT = nc.dram_tensor("outT", [NB, D, T], F32, kind="ExternalOutput").ap()

    with ExitStack() as st:
        P = Prog(nc, st)

        def sb(name, shape, dt):
            return st.enter_context(nc.sbuf_tensor(name, shape, dt))

        h_t = sb("h", [128, NCH * T], F32)
        h = h_t[:].rearrange("p (c t) -> p c t", c=NCH)
        hB = [[Buf("h%d_%d" % (c, g)) for g in range(NTG)] for c in range(NCH)]
        vecs = sb("vecs_sb", [128, NV], F32)
        cstf = sb("cstf", [128, NCST], F32)
        cstb = sb("cstb", [128, 448], BF16)
        lbv = sb("lbv", [128, 16], F32)
        vB, cB, cbB, lbB = Buf("vecs"), Buf("cstf"), Buf("cstb"), Buf("lbv")
        ARN = (200 * 1024 - NCH * T * 4 - 4 * (NV + NCST + 16) - 2 * 448) // 2
        ARN = ARN // 16 * 16
        arena_t = sb("arena", [128, ARN], BF16)
        ar = Arena(arena_t[:])
        PS = [st.enter_context(nc.psum_tensor("ps%d" % i, [128, 512], F32)) for i in range(8)]
        PB = [Buf("ps%d" % i) for i in range(8)]

        ident = cstb[:, C_ID:C_ID + 128]
        ones_b = cstb[:, C_ONE:C_ONE + 128]
        triu_f = cstf[:, C_TRIU:C_TRIU + 64]
        tril_f = cstf[:, C_TRIL:C_TRIL + 128]
        pow_f = cstf[:, C_POW:C_POW + NIT + 1]

        P.dma("sp", vecs[:], vecs_d, writes=[vB])
        P.dma("sp", cstf[:], cst_d, writes=[cB])
        P.dma("pool", cstb[:], cst_d[:, 0:448], writes=[cbB])
        lbe = sb("lbe", [128, 12], F32)
        lbeB = Buf("lbe")
        P.op("act", lambda e: e.activation(out=lbe[:], in_=vecs[:, V_LB:V_LB + 12], func=AF.Exp), reads=[vB], writes=[lbeB])
        P.op("dve", lambda e: e.tensor_tensor(out=lbv[:, 8:12], in0=lbe[:, 0:4], in1=lbe[:, 4:8], op=ALU.add), reads=[lbeB], writes=[lbB])
        P.op("dve", lambda e: e.tensor_tensor(out=lbv[:, 8:12], in0=lbv[:, 8:12], in1=lbe[:, 8:12], op=ALU.add), reads=[lbeB, lbB], writes=[lbB])
        P.op("dve", lambda e: e.reciprocal(out=lbv[:, 12:16], in_=lbv[:, 8:12]), reads=[lbB], writes=[lbB])
        P.op("dve", lambda e: e.tensor_tensor(out=lbv[:, 0:4], in0=lbe[:, 0:4], in1=lbv[:, 12:16], op=ALU.mult), reads=[lbeB, lbB], writes=[lbB])
        P.op("dve", lambda e: e.tensor_scalar(out=lbv[:, 4:8], in0=lbv[:, 0:4], scalar1=-1.0, scalar2=1.0, op0=ALU.mult, op1=ALU.add),
             reads=[lbB], writes=[lbB])

        def tgs(g):
            return slice(g * 512, (g + 1) * 512)

        def mm(out, lhsT, rhs, start, stop, reads, writes):
            P.op("pe", lambda e: e.matmul(out, lhsT=lhsT, rhs=rhs, start=start, stop=stop), reads=reads, writes=writes)

        def rmsnorm_tg(g, gcol, dst, dstB, scr):
            sq, sqB, lnv, lnB, pb = scr
            P.op("act", lambda e: e.activation(out=sq, in_=h[:, :, tgs(g)], func=AF.Square),
                 reads=[hB[c][g] for c in range(NCH)], writes=[sqB])
            for c in range(NCH):
                mm(PS[pb][:], ones_b, sq[:, c, :], c == 0, c == NCH - 1, [cbB, sqB], [PB[pb]])
            P.op("act", lambda e: e.activation(out=lnv, in_=PS[pb][:], func=AF.Ln, scale=1.0 / D, bias=EPS), reads=[PB[pb]], writes=[lnB])
            P.op("act", lambda e: e.activation(out=lnv, in_=lnv, func=AF.Exp, scale=-0.5), reads=[lnB], writes=[lnB])
            for c in range(NCH):
                P.op("dve", lambda e, c=c: e.scalar_tensor_tensor(out=dst[:, c, :], in0=h[:, c, tgs(g)], scalar=vecs[:, gcol + c:gcol + c + 1],
                                                                  in1=lnv, op0=ALU.mult, op1=ALU.mult),
                     reads=[hB[c][g], vB, lnB], writes=[dstB])

        def norm_scratch(pb):
            sq = ar.bf(NCH * 512).rearrange("p (c t) -> p c t", c=NCH)
            lnv = ar.f32(512)
            return (sq, Buf("sq"), lnv, Buf("lnv"), pb)

        def wview(w2d, c0, c1):
            return w2d.rearrange("(c p) n -> p c n", p=128)[:, :, c0:c1]

        def ffn_stage(l):
            P.barrier()
            ar.reset()
            hn = ar.bf(NCH * T).rearrange("p (c t) -> p c t", c=NCH)
            hnB = [Buf("hn%d" % g) for g in range(NTG)]
            act = ar.bf(11 * T).rearrange("p (f t) -> p f t", f=11)
            actB = [[Buf("act") for g in range(NTG)] for f in range(11)]
            wd = ar.bf(11 * D).rearrange("p (f n) -> p f n", f=11)
            wdB = Buf("wd")
            wgu = [ar.bf(NCH * 512).rearrange("p (c u n) -> p c u n", c=NCH, u=2) for _ in range(2)]
            wguB = [Buf("wgu0"), Buf("wgu1")]
            sg = [ar.f32(512) for _ in range(2)]
            sgB = [Buf("sg0"), Buf("sg1")]
            scr = norm_scratch(6)
            gcol = V_F0 if l == 0 else V_F1
            for g in range(NTG):
                rmsnorm_tg(g, gcol, hn[:, :, tgs(g)], hnB[g], scr)
            wgu_d = f_w_gu[l]
            wd_d = f_w_down[l]
            k = 0
            kd = 0
            for half in range(2):
                pairs = [(0, 2), (2, 2), (4, 2), (6, 2), (8, 2), (10, 1)]

                def load(pi):
                    f0, nf = pairs[pi]
                    fc0 = half * 11 + f0
                    s = pi % 2
                    P.dma("pool", wgu[s][:, :, 0, 0:nf * 128], wview(wgu_d, fc0 * 128, (fc0 + nf) * 128), writes=[wguB[s]])
                    P.dma("pool", wgu[s][:, :, 1, 0:nf * 128], wview(wgu_d, FF + fc0 * 128, FF + (fc0 + nf) * 128), writes=[wguB[s]])

                load(0)
                P.dma("pool", wd, wd_d[half * 1408:(half + 1) * 1408, :].rearrange("(f p) n -> p f n", p=128), writes=[wdB])
                for pi in range(len(pairs)):
                    if pi + 1 < len(pairs):
                        load(pi + 1)
                    f0, nf = pairs[pi]
                    s = pi % 2
                    for j in range(nf):
                        fi = f0 + j
                        for g in range(NTG):
                            pg, pu = k % 2, 2 + k % 2
                            for c in range(NCH):
                                mm(PS[pg][:], wgu[s][:, c, 0, j * 128:(j + 1) * 128], hn[:, c, tgs(g)], c == 0, c == NCH - 1,
                                   [wguB[s], hnB[g]], [PB[pg]])
                            for c in range(NCH):
                                mm(PS[pu][:], wgu[s][:, c, 1, j * 128:(j + 1) * 128], hn[:, c, tgs(g)], c == 0, c == NCH - 1,
                                   [wguB[s], hnB[g]], [PB[pu]])
                            P.op("act", lambda e, pg=pg, q=k % 2: e.activation(out=sg[q], in_=PS[pg][:], func=AF.Silu),
                                 reads=[PB[pg]], writes=[sgB[k % 2]])
                            P.op("dve", lambda e, pu=pu, q=k % 2, fi=fi, g=g: e.tensor_tensor(out=act[:, fi, tgs(g)], in0=sg[q], in1=PS[pu][:], op=ALU.mult),
                                 reads=[sgB[k % 2], PB[pu]], writes=[actB[fi][g]])
                            k += 1
                for dc in range(NCH):
                    for g in range(NTG):
                        pb = 4 + kd % 2
                        for fi in range(11):
                            mm(PS[pb][:], wd[:, fi, dc * 128:(dc + 1) * 128], act[:, fi, tgs(g)], fi == 0, fi == 10,
                               [wdB, actB[fi][g]], [PB[pb]])
                        P.op("dve", lambda e, pb=pb, dc=dc, g=g: e.tensor_tensor(out=h[:, dc, tgs(g)], in0=h[:, dc, tgs(g)], in1=PS[pb][:], op=ALU.add),
                             reads=[PB[pb], hB[dc][g]], writes=[hB[dc][g]])
                        kd += 1

        def xattn_stage(l, b):
            P.barrier()
            ar.reset()
            wq = ar.bf(NCH * 512).rearrange("p (c n) -> p c n", c=NCH)
            wo = ar.bf(4 * D).rearrange("p (c n) -> p c n", c=4)
            wkv = ar.bf(NCH * D).rearrange("p (c n) -> p c n", c=NCH)
            mem_f = ar.f32(NCH * MEM).rearrange("p (c m) -> p c m", c=NCH)
            msq = ar.bf(NCH * MEM).rearrange("p (c m) -> p c m", c=NCH)
            memn = ar.bf(NCH * MEM).rearrange("p (c m) -> p c m", c=NCH)
            mrs = ar.f32(MEM)
            kT = ar.bf(4 * MEM).rearrange("p (h m) -> p h m", h=4)
            Vt = ar.bf(2 * 512).rearrange("p (m n) -> p m n", m=2)
            hn = [ar.bf(NCH * 512).rearrange("p (c t) -> p c t", c=NCH) for _ in range(2)]
            qT = [ar.bf(4 * 512).rearrange("p (h t) -> p h t", h=4) for _ in range(2)]
            E = [ar.bf(2 * 512).rearrange("p (m t) -> p m t", m=2) for _ in range(2)]
            rden = [ar.f32(512) for _ in range(2)]
            ao = [ar.bf(4 * 512).rearrange("p (h t) -> p h t", h=4) for _ in range(2)]
            scr = norm_scratch(0)
            wqB, woB, wkvB, memB, msqB, memnB, mrsB, kTB, VtB = [Buf(n) for n in "wq wo wkv mem msq memn mrs kT Vt".split()]
            hnB = [Buf("hn0"), Buf("hn1")]
            qTB = [Buf("q0"), Buf("q1")]
            EB = [Buf("E0"), Buf("E1")]
            rdB = [Buf("rd0"), Buf("rd1")]
            aoB = [Buf("ao0"), Buf("ao1")]
            P.dma("pool", wkv, wview(x_wkv[l], 0, 1024), writes=[wkvB])
            P.dma("sp", mem_f, memT[b].rearrange("(c p) m -> p c m", p=128), writes=[memB])
            P.dma("pool", wq, wview(x_wq[l], 0, 512), writes=[wqB])
            P.dma("pool", wo, x_wo[l].rearrange("(c p) n -> p c n", p=128), writes=[woB])
            P.op("act", lambda e: e.activation(out=msq, in_=mem_f, func=AF.Square), reads=[memB], writes=[msqB])
            for c in range(NCH):
                mm(PS[0][:, 0:MEM], ones_b, msq[:, c, :], c == 0, c == NCH - 1, [cbB, msqB], [PB[0]])
            P.op("act", lambda e: e.activation(out=mrs, in_=PS[0][:, 0:MEM], func=AF.Ln, scale=1.0 / D, bias=EPS), reads=[PB[0]], writes=[mrsB])
            P.op("act", lambda e: e.activation(out=mrs, in_=mrs, func=AF.Exp, scale=-0.5), reads=[mrsB], writes=[mrsB])
            mcol = V_M0 if l == 0 else V_M1
            for c in range(NCH):
                P.op("dve", lambda e, c=c: e.scalar_tensor_tensor(out=memn[:, c, :], in0=mem_f[:, c, :], scalar=vecs[:, mcol + c:mcol + c + 1],
                                                                  in1=mrs, op0=ALU.mult, op1=ALU.mult), reads=[memB, vB, mrsB], writes=[memnB])
            for hd in range(4):
                pb = 1 + hd % 2
                for c in range(NCH):
                    mm(PS[pb][:, 0:MEM], wkv[:, c, hd * 128:(hd + 1) * 128], memn[:, c, :], c == 0, c == NCH - 1, [wkvB, memnB], [PB[pb]])
                P.op("act", lambda e, pb=pb, hd=hd: e.activation(out=kT[:, hd, :], in_=PS[pb][:, 0:MEM], func=AF.Copy), reads=[PB[pb]], writes=[kTB])
            for mt in range(2):
                pb = 1 + mt
                for c in range(NCH):
                    mm(PS[pb][:], memn[:, c, mt * 128:(mt + 1) * 128], wkv[:, c, 512:1024], c == 0, c == NCH - 1, [wkvB, memnB], [PB[pb]])
                P.op("act", lambda e, pb=pb, mt=mt: e.activation(out=Vt[:, mt, :], in_=PS[pb][:], func=AF.Copy), reads=[PB[pb]], writes=[VtB])
            xcol = V_X0 if l == 0 else V_X1
            sc = 128.0 ** -0.5
            kk = 0
            rmsnorm_tg(0, xcol, hn[0], hnB[0], scr)
            for g in range(NTG):
                q = g % 2
                for hd in range(4):
                    pb = hd % 2
                    for c in range(NCH):
                        mm(PS[pb][:], wq[:, c, hd * 128:(hd + 1) * 128], hn[q][:, c, :], c == 0, c == NCH - 1, [wqB, hnB[q]], [PB[pb]])
                    P.op("act", lambda e, pb=pb, hd=hd, q=q: e.activation(out=qT[q][:, hd, :], in_=PS[pb][:], func=AF.Copy), reads=[PB[pb]], writes=[qTB[q]])
                if g + 1 < NTG:
                    rmsnorm_tg(g + 1, xcol, hn[1 - q], hnB[1 - q], scr)
                for hd in range(4):
                    e2 = kk % 2
                    for mt in range(2):
                        pb = 2 + mt
                        mm(PS[pb][:], kT[:, hd, mt * 128:(mt + 1) * 128], qT[q][:, hd, :], True, True, [kTB, qTB[q]], [PB[pb]])
                        P.op("act", lambda e, pb=pb, mt=mt, e2=e2: e.activation(out=E[e2][:, mt, :], in_=PS[pb][:], func=AF.Exp, scale=sc),
                             reads=[PB[pb]], writes=[EB[e2]])
                    po, pd = 4 + 2 * (kk % 2), 5 + 2 * (kk % 2)
                    for mt in range(2):
                        mm(PS[po][:], Vt[:, mt, hd * 128:(hd + 1) * 128], E[e2][:, mt, :], mt == 0, mt == 1, [VtB, EB[e2]], [PB[po]])
                    for mt in range(2):
                        mm(PS[pd][:], ones_b, E[e2][:, mt, :], mt == 0, mt == 1, [cbB, EB[e2]], [PB[pd]])
                    P.op("act", lambda e: e.activation(out=rden[e2], in_=PS[pd][:], func=AF.Ln), reads=[PB[pd]], writes=[rdB[e2]])
                    P.op("act", lambda e: e.activation(out=rden[e2], in_=rden[e2], func=AF.Exp, scale=-1.0), reads=[rdB[e2]], writes=[rdB[e2]])
                    P.op("dve", lambda e: e.tensor_tensor(out=ao[q][:, hd, :], in0=PS[po][:], in1=rden[e2], op=ALU.mult),
                         reads=[PB[po], rdB[e2]], writes=[aoB[q]])
                    kk += 1
                for dc in range(NCH):
                    pb = 6 + dc % 2 if False else (dc % 2)
                    for hd in range(4):
                        mm(PS[pb][:], wo[:, hd, dc * 128:(dc + 1) * 128], ao[q][:, hd, :], hd == 0, hd == 3, [woB, aoB[q]], [PB[pb]])
                    P.op("dve", lambda e, pb=pb, dc=dc, g=g: e.tensor_tensor(out=h[:, dc, tgs(g)], in0=h[:, dc, tgs(g)], in1=PS[pb][:], op=ALU.add),
                         reads=[PB[pb], hB[dc][g]], writes=[hB[dc][g]])

        def gmlp_stage():
            P.barrier()
            ar.reset()
            w_in = ar.bf(NCH * 2048).rearrange("p (c n) -> p c n", c=NCH)
            w_out = ar.bf(NCH * D).rearrange("p (c n) -> p c n", c=NCH)
            wsp_f = ar.f32(8 * 128).rearrange("p (g t) -> p g t", g=8)
            wsp = ar.bf(8 * 128).rearrange("p (g t) -> p g t", g=8)
            bsp = ar.f32(8 * 128).rearrange("p (g t) -> p g t", g=8)
            lng = ar.f32(D)
            lnb = ar.f32(D)
            hn0_ = ar.bf(NCH * 512).rearrange("p (c t) -> p c t", c=NCH)
            hn = [hn0_, hn0_]
            uT = ar.bf(NCH * 512).rearrange("p (c t) -> p c t", c=NCH)
            vtok = ar.bf(4 * D).rearrange("p (n c) -> p n c", n=4)
            vg = [ar.f32(D) for _ in range(2)]
            vn = [ar.f32(D) for _ in range(2)]
            st6 = ar.f32(16)
            mv = ar.f32(8)
            tmpm = [ar.f32(512) for _ in range(2)]
            gated = ar.bf(NCH * 512).rearrange("p (c t) -> p c t", c=NCH)
            scr = norm_scratch(7)
            names = "w_in w_out wspf wsp bsp lng lnb uT vtok st6 mv gated".split()
            B = {n: Buf(n) for n in names}
            hnB0_ = Buf("hn0")
            hnB = [hnB0_, hnB0_]
            vgB = [Buf("vg0"), Buf("vg1")]
            vnB = [Buf("vn0"), Buf("vn1")]
            tmB = [Buf("tm0"), Buf("tm1")]
            P.dma("pool", w_in, wview(o_w_in, 0, 2048), writes=[B["w_in"]])
            P.dma("pool", w_out, wview(o_w_out, 0, 1024), writes=[B["w_out"]])
            P.dma("sp", wsp_f, o_w_spT.rearrange("p (g t) -> p g t", g=8), writes=[B["wspf"]])
            P.dma("sp", bsp, o_b_sp[0].partition_broadcast(128).rearrange("p (g t) -> p g t", g=8), writes=[B["bsp"]])
            P.dma("sp", lng, o_ln_g[0].partition_broadcast(128), writes=[B["lng"]])
            P.dma("sp", lnb, o_ln_b[0].partition_broadcast(128), writes=[B["lnb"]])
            P.op("dve", lambda e: e.tensor_tensor(out=wsp, in0=wsp_f, in1=tril_f.unsqueeze(1).to_broadcast([128, 8, 128]), op=ALU.mult),
                 reads=[B["wspf"], cB], writes=[B["wsp"]])
            kv = 0
            km = 0
            for g in range(NTG):
                q = g % 2
                rmsnorm_tg(g, V_MIX1, hn[q], hnB[q], scr)
                for gc in range(NCH):
                    pb = gc % 2
                    for c in range(NCH):
                        mm(PS[pb][:], w_in[:, c, gc * 128:(gc + 1) * 128], hn[q][:, c, :], c == 0, c == NCH - 1, [B["w_in"], hnB[q]], [PB[pb]])
                    P.op("act", lambda e, pb=pb, gc=gc: e.activation(out=uT[:, gc, :], in_=PS[pb][:], func=AF.Gelu), reads=[PB[pb]], writes=[B["uT"]])
                for n in range(4):
                    v2 = kv % 2
                    for hf in range(2):
                        pb = 2 + hf
                        for c in range(NCH):
                            mm(PS[pb][:], hn[q][:, c, n * 128:(n + 1) * 128], w_in[:, c, 1024 + hf * 512:1024 + (hf + 1) * 512], c == 0, c == NCH - 1,
                               [B["w_in"], hnB[q]], [PB[pb]])
                        P.op("act", lambda e, pb=pb, hf=hf, v2=v2: e.activation(out=vg[v2][:, hf * 512:(hf + 1) * 512], in_=PS[pb][:], func=AF.Gelu),
                             reads=[PB[pb]], writes=[vgB[v2]])
                    for hf in range(2):
                        P.op("dve", lambda e, hf=hf, v2=v2: e.bn_stats(out=st6[:, hf * 6:(hf + 1) * 6], in_=vg[v2][:, hf * 512:(hf + 1) * 512]),
                             reads=[vgB[v2]], writes=[B["st6"]])
                    P.op("dve", lambda e: e.bn_aggr(out=mv[:, 0:2], in_=st6[:, 0:12]), reads=[B["st6"]], writes=[B["mv"]])
                    P.op("act", lambda e: e.activation(out=mv[:, 2:3], in_=mv[:, 1:2], func=AF.Ln, bias=EPS), reads=[B["mv"]], writes=[B["mv"]])
                    P.op("act", lambda e: e.activation(out=mv[:, 2:3], in_=mv[:, 2:3], func=AF.Exp, scale=-0.5), reads=[B["mv"]], writes=[B["mv"]])
                    P.op("dve", lambda e, v2=v2: e.tensor_scalar(out=vn[v2], in0=vg[v2], scalar1=mv[:, 0:1], scalar2=mv[:, 2:3], op0=ALU.subtract, op1=ALU.mult),
                         reads=[vgB[v2], B["mv"]], writes=[vnB[v2]])
                    P.op("pool", lambda e, v2=v2: e.tensor_tensor(out=vn[v2], in0=vn[v2], in1=lng, op=ALU.mult), reads=[vnB[v2], B["lng"]], writes=[vnB[v2]])
                    P.op("pool", lambda e, v2=v2, n=n: e.tensor_tensor(out=vtok[:, n, :], in0=vn[v2], in1=lnb, op=ALU.add), reads=[vnB[v2], B["lnb"]], writes=[B["vtok"]])
                    kv += 1
                for gc in range(NCH):
                    pb = 4 + gc % 2
                    for n in range(4):
                        mm(PS[pb][:, n * 128:(n + 1) * 128], vtok[:, n, gc * 128:(gc + 1) * 128], wsp[:, gc, :], True, True, [B["vtok"], B["wsp"]], [PB[pb]])
                    t2 = km % 2
                    P.op("dve", lambda e, pb=pb, gc=gc, t2=t2: e.tensor_tensor(out=tmpm[t2].rearrange("p (n t) -> p n t", n=4),
                                                                            in0=PS[pb][:].rearrange("p (n t) -> p n t", n=4),
                                                                            in1=bsp[:, gc, :].unsqueeze(1).to_broadcast([128, 4, 128]), op=ALU.add),
                         reads=[PB[pb], B["bsp"]], writes=[tmB[t2]])
                    P.op("pool", lambda e, gc=gc, t2=t2: e.tensor_tensor(out=gated[:, gc, :], in0=tmpm[t2], in1=uT[:, gc, :], op=ALU.mult),
                         reads=[tmB[t2], B["uT"]], writes=[B["gated"]])
                    km += 1
                for dc in range(NCH):
                    pb = 6 + dc % 2
                    if pb == 7:
                        pb = 0
                    for c in range(NCH):
                        mm(PS[pb][:], w_out[:, c, dc * 128:(dc + 1) * 128], gated[:, c, :], c == 0, c == NCH - 1, [B["w_out"], B["gated"]], [PB[pb]])
                    P.op("dve", lambda e, pb=pb, dc=dc, g=g: e.tensor_tensor(out=h[:, dc, tgs(g)], in0=h[:, dc, tgs(g)], in1=PS[pb][:], op=ALU.add),
                         reads=[PB[pb], hB[dc][g]], writes=[hB[dc][g]])

        def mixer0_stage(b):
            P.barrier()
            ar.reset()
            cnT = ar.bf(T)
            Ctok = ar.bf(NT * 128).rearrange("p (i c) -> p i c", i=NT)
            kiT2 = ar.bf(T)
            Sf = ar.f32(512)
            Sb = ar.bf(512)
            expb = ar.bf(2 * 8 * 128).rearrange("p (k h t) -> p k h t", k=2, h=8)
            wukT = ar.bf(4 * 128).rearrange("p (q c) -> p q c", q=4)
            wuv = ar.bf(8 * 64).rearrange("p (h d) -> p h d", h=8)
            NAMES = "cnT Ctok kiT2 Sf Sb expb wukT wuv".split()
            B = {n: Buf(n) for n in NAMES}
            base_off = ar.off
            bias_f = ar.f32(3 * 8 * 128).rearrange("p (k h t) -> p k h t", k=3, h=8)
            bfB = Buf("bias_f")
            P.dma("sp", bias_f, bias_d.rearrange("p (k h t) -> p k h t", k=3, h=8), writes=[bfB])
            P.dma("pool", wukT, e_w_ukT.rearrange("p (q c) -> p q c", q=4), writes=[B["wukT"]])
            P.dma("pool", wuv, e_w_uv.rearrange("p (h d) -> p h d", h=8), writes=[B["wuv"]])
            for kd_ in range(2):
                P.op("dve", lambda e, kd_=kd_: e.tensor_tensor(out=bias_f[:, kd_], in0=bias_f[:, kd_], in1=bias_f[:, 2], op=ALU.subtract),
                     reads=[bfB], writes=[bfB])
            P.op("act", lambda e: e.activation(out=expb, in_=bias_f[:, 0:2], func=AF.Copy, scale=8.0), reads=[bfB], writes=[B["expb"]])
            P.op("pool", lambda e: e.memset(Sf, 0.0), writes=[B["Sf"]])
            P.op("pool", lambda e: e.memset(Sb, 0.0), writes=[B["Sb"]])
            P.barrier()

            for g in range(NTG):
                ar.reset(base_off)
                qaT = ar.bf(4 * 512).rearrange("p (q t) -> p q t", q=4)
                qlat = ar.bf(8 * 512).rearrange("p (h t) -> p h t", h=8)
                qiT = ar.bf(4 * 512).rearrange("p (q t) -> p q t", q=4)
                wtok = ar.f32(4 * 8).rearrange("p (j h) -> p j h", j=4)
                eb = ar.f32(4 * 512).rearrange("p (h t) -> p h t", h=4)
                Kt = ar.bf(4 * 512).rearrange("p (h t) -> p h t", h=4)
                Qt = ar.bf(4 * 512).rearrange("p (h t) -> p h t", h=4)
                Ktok = ar.bf(16 * 128).rearrange("p (j h d) -> p j h d", j=4, h=4)
                Vtok = ar.bf(4 * 512).rearrange("p (j n) -> p j n", j=4)
                sgT = ar.bf(4 * 512).rearrange("p (h t) -> p h t", h=4)
                boT = ar.bf(4 * 512).rearrange("p (h t) -> p h t", h=4)
                aoT = ar.bf(4 * 512).rearrange("p (q t) -> p q t", q=4)
                ph_off = ar.off
                hn = ar.bf(NCH * 512).rearrange("p (c t) -> p c t", c=NCH)
                wsl = [ar.bf(NCH * 512).rearrange("p (c n) -> p c n", c=NCH) for _ in range(3)]
                tA = [ar.f32(512) for _ in range(2)]
                tB = [ar.f32(512) for _ in range(2)]
                tC = [ar.f32(512) for _ in range(2)]
                craw = ar.f32(512)
                csq = ar.bf(512)
                scr = norm_scratch(7)
                L = {n: Buf(n) for n in "hn qaT qlat qiT wtok eb Kt Qt Ktok Vtok sgT boT aoT craw csq".split()}
                wsB = [Buf("ws%d" % i) for i in range(3)]
                tAB = [Buf("tA0"), Buf("tA1")]
                tBB = [Buf("tB0"), Buf("tB1")]
                tCB = [Buf("tC0"), Buf("tC1")]
                rmsnorm_tg(g, V_MIX0, hn, L["hn"], scr)
                pieces = [("qa", 0, 512), ("small", None, None), ("qi", 640, 1152), ("f", 1224, 1736), ("q", 2248, 2760),
                          ("i", 1736, 2248), ("g", 2760, 3272)]

                def loadw(pi):
                    nm, c0, c1 = pieces[pi]
                    s = pi % 3
                    if nm == "small":
                        P.dma("pool", wsl[s][:, :, 0:128], wview(e_w_in, 512, 640), writes=[wsB[s]])
                        P.dma("pool", wsl[s][:, :, 128:192], wview(e_w_in, 1152, 1216), writes=[wsB[s]])
                        P.dma("pool", wsl[s][:, :, 192:256], wview(e_w_in, 1152, 1216), writes=[wsB[s]])
                        P.dma("pool", wsl[s][:, :, 256:264], wview(e_w_in, 1216, 1224), writes=[wsB[s]])
                    else:
                        P.dma("pool", wsl[s], wview(e_w_in, c0, c1), writes=[wsB[s]])

                loadw(0)
                loadw(1)
                kq = 0
                for pi, (nm, c0, c1) in enumerate(pieces):
                    if pi + 2 < len(pieces):
                        loadw(pi + 2)
                    s = pi % 3
                    W = wsl[s]
                    WB = wsB[s]

                    def proj_fm(col0, ncol, pb):
                        for c in range(NCH):
                            mm(PS[pb][0:ncol, :], W[:, c, col0:col0 + ncol], hn[:, c, :], c == 0, c == NCH - 1, [WB, L["hn"]], [PB[pb]])

                    if nm == "qa":
                        for q4 in range(4):
                            pb = q4 % 2
                            proj_fm(q4 * 128, 128, pb)
                            P.op("act", lambda e, pb=pb, q4=q4: e.activation(out=qaT[:, q4, :], in_=PS[pb][:], func=AF.Copy), reads=[PB[pb]], writes=[L["qaT"]])
                        for hh in range(8):
                            pb = 2 + hh % 2
                            i2, q4 = hh % 2, hh // 2
                            mm(PS[pb][:], wukT[64 * i2:64 * i2 + 64, q4, :], qaT[64 * i2:64 * i2 + 64, q4, :], True, True, [B["wukT"], L["qaT"]], [PB[pb]])
                            P.op("dve", lambda e, pb=pb, hh=hh: e.tensor_copy(out=qlat[:, hh, :], in_=PS[pb][:]), reads=[PB[pb]], writes=[L["qlat"]])
                    elif nm == "small":
                        proj_fm(0, 128, 0)
                        P.op("act", lambda e: e.activation(out=craw, in_=PS[0][:], func=AF.Copy), reads=[PB[0]], writes=[L["craw"]])
                        P.op("act", lambda e: e.activation(out=csq, in_=PS[0][:], func=AF.Square), reads=[PB[0]], writes=[L["csq"]])
                        mm(PS[1][:], ones_b, csq, True, True, [cbB, L["csq"]], [PB[1]])
                        P.op("act", lambda e: e.activation(out=tA[0], in_=PS[1][:], func=AF.Ln, scale=1.0 / 128, bias=EPS), reads=[PB[1]], writes=[tAB[0]])
                        P.op("act", lambda e: e.activation(out=tA[0], in_=tA[0], func=AF.Exp, scale=-0.5), reads=[tAB[0]], writes=[tAB[0]])
                        P.op("dve", lambda e: e.scalar_tensor_tensor(out=cnT[:, tgs(g)], in0=craw, scalar=vecs[:, V_LAT:V_LAT + 1], in1=tA[0],
                                                                     op0=ALU.mult, op1=ALU.mult), reads=[L["craw"], vB, tAB[0]], writes=[B["cnT"]])
                        psb = PS[1][:].bitcast(BF16)
                        for j in range(4):
                            P.op("pe", lambda e, j=j: e.transpose(psb[:, j * 128:(j + 1) * 128], cnT[:, g * 512 + j * 128:g * 512 + (j + 1) * 128], ident),
                                 reads=[B["cnT"], cbB], writes=[PB[1]])
                        P.op("act", lambda e: e.activation(out=Ctok[:, 4 * g:4 * g + 4, :], in_=psb[:, 0:512].rearrange("p (j c) -> p j c", j=4), func=AF.Copy),
                             reads=[PB[1]], writes=[B["Ctok"]])
                        proj_fm(128, 128, 2)
                        P.op("act", lambda e: e.activation(out=kiT2[:, tgs(g)], in_=PS[2][:], func=AF.Copy), reads=[PB[2]], writes=[B["kiT2"]])
                        for j in range(4):
                            for c in range(NCH):
                                mm(PS[3][:, j * 8:(j + 1) * 8], hn[:, c, j * 128:(j + 1) * 128], W[:, c, 256:264], c == 0, c == NCH - 1, [WB, L["hn"]], [PB[3]])
                        P.op("act", lambda e: e.activation(out=wtok, in_=PS[3][:, 0:32].rearrange("p (j h) -> p j h", j=4), func=AF.Copy,
                                                           scale=0.125 * (8.0 ** -0.5)), reads=[PB[3]], writes=[L["wtok"]])
                    elif nm == "qi":
                        for q4 in range(4):
                            pb = q4 % 2
                            proj_fm(q4 * 128, 128, pb)
                            P.op("act", lambda e, pb=pb, q4=q4: e.activation(out=qiT[:, q4, :], in_=PS[pb][:], func=AF.Copy), reads=[PB[pb]], writes=[L["qiT"]])
                    elif nm == "f":
                        for hd in range(4):
                            pb = hd % 2
                            z = kq % 2
                            kq += 1
                            proj_fm(hd * 128, 128, pb)
                            P.op("act", lambda e, pb=pb, z=z: e.activation(out=tA[z], in_=PS[pb][:], func=AF.Sigmoid), reads=[PB[pb]], writes=[tAB[z]])
                            P.op("dve", lambda e, z=z, hd=hd: e.tensor_scalar(out=tA[z], in0=tA[z], scalar1=lbv[:, 4 + hd:5 + hd], scalar2=lbv[:, hd:hd + 1],
                                                                              op0=ALU.mult, op1=ALU.add), reads=[tAB[z], lbB], writes=[tAB[z]])
                            P.op("act", lambda e, z=z: e.activation(out=tB[z], in_=tA[z], func=AF.Ln), reads=[tAB[z]], writes=[tBB[z]])
                            for ch in range(8):
                                P.op("dve", lambda e, z=z, ch=ch: e.tensor_tensor_scan(out=tC[z][:, ch * 64:(ch + 1) * 64], data0=ones_f[:, 0:64],
                                                                                       data1=tB[z][:, ch * 64:(ch + 1) * 64], initial=0.0,
                                                                                       op0=ALU.mult, op1=ALU.add), reads=[tBB[z], cB], writes=[tCB[z]])
                            P.op("act", lambda e, z=z, hd=hd: e.activation(out=eb[:, hd, :], in_=tC[z], func=AF.Exp), reads=[tCB[z]], writes=[L["eb"]])
                            P.op("act", lambda e, z=z: e.activation(out=tB[z], in_=tC[z], func=AF.Exp, scale=-1.0), reads=[tCB[z], tBB[z]], writes=[tBB[z]])
                            P.op("dve", lambda e, z=z: e.tensor_scalar(out=tA[z], in0=tA[z], scalar1=-1.0, scalar2=1.0, op0=ALU.mult, op1=ALU.add),
                                 reads=[tAB[z]], writes=[tAB[z]])
                            P.op("dve", lambda e, z=z, hd=hd: e.tensor_tensor(out=Kt[:, hd, :], in0=tA[z], in1=tB[z], op=ALU.mult),
                                 reads=[tAB[z], tBB[z]], writes=[L["Kt"]])
                        for j in range(4):
                            pb = 2 + j % 2
                            psb = PS[pb][:].bitcast(BF16)
                            for hd in range(4):
                                P.op("pe", lambda e, j=j, hd=hd, psb=psb: e.transpose(psb[:, hd * 128:(hd + 1) * 128], Kt[:, hd, j * 128:(j + 1) * 128], ident),
                                     reads=[L["Kt"], cbB], writes=[PB[pb]])
                            P.op("act", lambda e, j=j, psb=psb: e.activation(out=Ktok[:, j], in_=psb[:, 0:512].rearrange("p (h d) -> p h d", h=4), func=AF.Copy),
                                 reads=[PB[pb]], writes=[L["Ktok"]])
                    elif nm == "q":
                        for hd in range(4):
                            pb = hd % 2
                            z = kq % 2
                            kq += 1
                            proj_fm(hd * 128, 128, pb)
                            P.op("act", lambda e, pb=pb, z=z: e.activation(out=tA[z], in_=PS[pb][:], func=AF.Silu), reads=[PB[pb]], writes=[tAB[z]])
                            P.op("dve", lambda e, z=z, hd=hd: e.tensor_tensor(out=Qt[:, hd, :], in0=tA[z], in1=eb[:, hd, :], op=ALU.mult),
                                 reads=[tAB[z], L["eb"]], writes=[L["Qt"]])
                    elif nm == "i":
                        for j in range(4):
                            pb = j % 2
                            for c in range(NCH):
                                mm(PS[pb][:], hn[:, c, j * 128:(j + 1) * 128], W[:, c, :], c == 0, c == NCH - 1, [WB, L["hn"]], [PB[pb]])
                            P.op("act", lambda e, pb=pb, j=j: e.activation(out=Vtok[:, j, :], in_=PS[pb][:], func=AF.Copy), reads=[PB[pb]], writes=[L["Vtok"]])
                    elif nm == "g":
                        for hd in range(4):
                            pb = hd % 2
                            proj_fm(hd * 128, 128, pb)
                            P.op("act", lambda e, pb=pb, hd=hd: e.activation(out=sgT[:, hd, :], in_=PS[pb][:], func=AF.Silu), reads=[PB[pb]], writes=[L["sgT"]])

                P.barrier()
                ar.reset(ph_off)
                Sm = [ar.bf(256) for _ in range(2)]
                SmB = [Buf("Sm0"), Buf("Sm1")]
                tS = ar.f32(512)
                tSB = Buf("tS")
                osq = ar.bf(512)
                osqB = Buf("osq")
                rst = ar.f32(512)
                rstB = Buf("rst")
                t1 = ar.f32(512)
                t1B = Buf("t1")
                first = (g == 0)
                for ch in (range(0) if 'hgrn' in KSKIP else range(8)):
                    j, p0 = ch // 2, 64 * (ch % 2)
                    cs = slice(ch * 64, (ch + 1) * 64)
                    sm = ch % 2
                    for hd in range(4):
                        mm(PS[4][p0:p0 + 64, hd * 64:(hd + 1) * 64], Kt[:, hd, cs], Qt[:, hd, cs], True, True, [L["Kt"], L["Qt"]], [PB[4]])
                    P.op("dve", lambda e, p0=p0, sm=sm: e.tensor_tensor(out=Sm[sm][p0:p0 + 64, :].rearrange("p (h t) -> p h t", h=4),
                                                                       in0=PS[4][p0:p0 + 64, 0:256].rearrange("p (h t) -> p h t", h=4),
                                                                       in1=triu_f[p0:p0 + 64, :].unsqueeze(1).to_broadcast([64, 4, 64]), op=ALU.mult),
                         reads=[PB[4], cB], writes=[SmB[sm]])
                    for hd in range(4):
                        noS = first and ch == 0
                        mm(PS[hd][:, cs], Vtok[p0:p0 + 64, j, hd * 128:(hd + 1) * 128], Sm[sm][p0:p0 + 64, hd * 64:(hd + 1) * 64], True, noS,
                           [L["Vtok"], SmB[sm]], [PB[hd]])
                        if not noS:
                            mm(PS[hd][:, cs], Sb[:, hd * 128:(hd + 1) * 128], Qt[:, hd, cs], False, True, [B["Sb"], L["Qt"]], [PB[hd]])
                    for hd in range(4):
                        mm(PS[5][:, hd * 128:(hd + 1) * 128], Ktok[p0:p0 + 64, j, hd, :], Vtok[p0:p0 + 64, j, hd * 128:(hd + 1) * 128], True, True,
                           [L["Ktok"], L["Vtok"]], [PB[5]])
                    P.op("dve", lambda e: e.tensor_tensor(out=tS, in0=Sf, in1=PS[5][:], op=ALU.add), reads=[B["Sf"], PB[5]], writes=[tSB])
                    aend = eb[:, :, ch * 64 + 63:ch * 64 + 64]
                    P.op("dve", lambda e, aend=aend: e.tensor_tensor(out=Sf.rearrange("p (h v) -> p h v", h=4), in0=tS.rearrange("p (h v) -> p h v", h=4),
                                                                     in1=aend.to_broadcast([128, 4, 128]), op=ALU.mult),
                         reads=[tSB, L["eb"]], writes=[B["Sf"]])
                    P.op("act", lambda e: e.activation(out=Sb, in_=Sf, func=AF.Copy), reads=[B["Sf"]], writes=[B["Sb"]])
                for hd in range(4):
                    P.op("act", lambda e, hd=hd: e.activation(out=osq, in_=PS[hd][:], func=AF.Square), reads=[PB[hd]], writes=[osqB])
                    mm(PS[6][:], ones_b, osq, True, True, [cbB, osqB], [PB[6]])
                    P.op("act", lambda e: e.activation(out=rst, in_=PS[6][:], func=AF.Ln, scale=1.0 / 128, bias=EPS), reads=[PB[6]], writes=[rstB])
                    P.op("act", lambda e: e.activation(out=rst, in_=rst, func=AF.Exp, scale=-0.5), reads=[rstB], writes=[rstB])
                    P.op("dve", lambda e, hd=hd: e.scalar_tensor_tensor(out=t1, in0=PS[hd][:], scalar=vecs[:, V_ON:V_ON + 1], in1=rst, op0=ALU.mult, op1=ALU.mult),
                         reads=[PB[hd], vB, rstB], writes=[t1B])
                    P.op("dve", lambda e, hd=hd: e.tensor_tensor(out=boT[:, hd, :], in0=t1, in1=sgT[:, hd, :], op=ALU.mult), reads=[t1B, L["sgT"]], writes=[L["boT"]])

                P.barrier()
                ar.reset(ph_off)
                score2 = [ar.f32(T) for _ in range(2)]
                Rr = [ar.bf(512) for _ in range(4)]
                rrow = ar.f32(512)
                msk = ar.bf(T)
                junk = msk
                maskT2 = [ar.bf(NT * 128).rearrange("p (i t) -> p i t", i=NT) for _ in range(2)]
                ET = [ar.bf(1024).rearrange("p (h t) -> p h t", h=8) for _ in range(3)]
                st_ = ar.f32(8 + 2 * (NIT + 1))
                rd = ar.f32(1024)
                olat = ar.bf(1024).rearrange("p (h t) -> p h t", h=8)
                wdiag = ar.bf(4 * 8 * 128).rearrange("p (j h t) -> p j h t", j=4, h=8)
                D3 = {n: Buf(n) for n in "msk st rd olat wdiag rrow".split()}
                D3["junk"] = D3["msk"]
                scB = [Buf("score0"), Buf("score1")]
                mTB = [Buf("maskT0"), Buf("maskT1")]
                RB = [Buf("R%d" % i) for i in range(4)]
                ETB = [Buf("ET0"), Buf("ET1"), Buf("ET2")]
                P7 = [Buf("ps7a"), Buf("ps7b")]
                P6 = [Buf("ps6a"), Buf("ps6b")]
                thr, cnt, tt, amax = st_[:, 0:1], st_[:, 1:2], st_[:, 2:3], st_[:, 3:4]
                dl = st_[:, 8:8 + NIT + 1]
                cnts = {"kr": 0, "ke": 0, "kf": 0}
                ident_f = cstf[:, C_ID:C_ID + 128]
                for jt in range(4):
                    P.op("dve", lambda e: e.tensor_tensor(out=wdiag[:, jt], in0=ident_f.unsqueeze(1).to_broadcast([128, 8, 128]),
                                                          in1=wtok[:, jt, :].unsqueeze(2).to_broadcast([128, 8, 128]), op=ALU.mult),
                         reads=[cB, L["wtok"]], writes=[D3["wdiag"]])

                def gen_I(jt):
                    J = 4 * g + jt
                    n2 = 128 * (J + 1)
                    tcol = slice(jt * 128, (jt + 1) * 128)
                    score = score2[jt % 2]
                    sB = scB[jt % 2]
                    nblk = (n2 + 511) // 512
                    steps = [(blk, hh) for blk in range(nblk) for hh in range(8)]
                    k0 = cnts["kr"]
                    cnts["kr"] += len(steps)
                    dbank = (5, 7)

                    def dots(k):
                        blk, hh = steps[k]
                        s0 = blk * 512
                        ns = min(512, n2 - s0)
                        pb = dbank[(k0 + k) % 2]
                        i2, q4 = hh % 2, hh // 2
                        mm(PS[pb][:, 0:ns], qiT[64 * i2:64 * i2 + 64, q4, tcol], kiT2[64 * i2:64 * i2 + 64, s0:s0 + ns], True, True,
                           [L["qiT"], B["kiT2"]], [PB[pb]])

                    dots(0)
                    for k, (blk, hh) in enumerate(steps):
                        s0 = blk * 512
                        ns = min(512, n2 - s0)
                        pb = dbank[(k0 + k) % 2]
                        r4 = (k0 + k) % 4
                        if k + 1 < len(steps):
                            dots(k + 1)
                        P.op("act", lambda e: e.activation(out=Rr[r4][:, 0:ns], in_=PS[pb][:, 0:ns], func=AF.Relu),
                             reads=[PB[pb]], writes=[RB[r4]])
                        mm(PS[6][:, 0:ns], wdiag[:, jt, hh, :], Rr[r4][:, 0:ns], hh == 0, hh == 7, [D3["wdiag"], RB[r4]], [PB[6]])
                        if hh == 7:
                            P.op("act", lambda e: e.activation(out=score[:, s0:s0 + ns], in_=PS[6][:, 0:ns], func=AF.Copy),
                                 reads=[PB[6]], writes=[sB])
                        yield "head"

                def gen_S(jt):
                    J = 4 * g + jt
                    n2 = 128 * (J + 1)
                    score = score2[jt % 2]
                    sB = scB[jt % 2]
                    maskT = maskT2[jt % 2]
                    mB = mTB[jt % 2]
                    if n2 > KTOP:
                        P.op("dve", lambda e: e.tensor_reduce(out=amax, in_=score[:, 0:n2], axis=AX.X, op=ALU.max, apply_absolute_value=True),
                             reads=[sB], writes=[D3["st"]])
                        P.op("dve", lambda e: e.tensor_scalar(out=dl, in0=pow_f, scalar1=amax, scalar2=None, op0=ALU.mult), reads=[D3["st"], cB], writes=[D3["st"]])
                        P.op("dve", lambda e: e.memset(thr, 0.0), reads=[D3["st"]], writes=[D3["st"]])
                    P.op("dve", lambda e: e.memset(score[0:64, n2 - 64:n2], NEG), reads=[sB], writes=[sB])
                    yield "it"
                    if n2 > KTOP:
                        for it in range(NIT):
                            P.op("dve", lambda e: e.tensor_scalar(out=junk[:, 0:n2], in0=score[:, 0:n2], scalar1=thr, scalar2=None, op0=ALU.is_ge, op1=ALU.add,
                                                                  accum_out=cnt), reads=[sB, D3["st"]], writes=[D3["junk"], D3["st"]])
                            P.op("dve", lambda e: e.tensor_scalar(out=tt, in0=cnt, scalar1=KTOP - 0.5, scalar2=dl[:, it:it + 1], op0=ALU.is_ge, op1=ALU.mult),
                                 reads=[D3["st"]], writes=[D3["st"]])
                            P.op("dve", lambda e: e.scalar_tensor_tensor(out=thr, in0=tt, scalar=dl[:, it + 1:it + 2], in1=thr, op0=ALU.subtract, op1=ALU.add),
                                 reads=[D3["st"]], writes=[D3["st"]])
                            yield "it"
                    else:
                        P.op("dve", lambda e: e.memset(thr, -1.0e29), reads=[D3["st"]], writes=[D3["st"]])
                    P.op("dve", lambda e: e.tensor_scalar(out=msk[:, 0:n2], in0=score[:, 0:n2], scalar1=thr, scalar2=-30000.0, op0=ALU.is_lt, op1=ALU.mult),
                         reads=[sB, D3["st"]], writes=[D3["msk"]])
                    yield "mask"
                    for i0 in range(0, J + 1, 8):
                        ni = min(8, J + 1 - i0)
                        psb = PS[7][:].bitcast(BF16)
                        for ii in range(ni):
                            P.op("pe", lambda e: e.transpose(psb[:, ii * 128:(ii + 1) * 128], msk[:, (i0 + ii) * 128:(i0 + ii + 1) * 128], ident),
                                 reads=[D3["msk"], cbB], writes=[PB[7]])
                        P.op("act", lambda e: e.activation(out=maskT[:, i0:i0 + ni, :], in_=psb[:, 0:ni * 128].rearrange("p (i t) -> p i t", i=ni),
                                                           func=AF.Copy), reads=[PB[7]], writes=[mB])
                        yield "tr"

                def gen_B(jt):
                    J = 4 * g + jt
                    tcol = slice(jt * 128, (jt + 1) * 128)
                    maskT = maskT2[jt % 2]
                    mB = mTB[jt % 2]
                    for i in range(J + 1):
                        e2 = cnts["ke"] % 3
                        cnts["ke"] += 1
                        near = i >= J - 1
                        kind = 0 if i == J else 1
                        for hf in range(2):
                            o3 = PS[hf][:].rearrange("p (h t) -> p h t", h=4)
                            mm(o3, cnT[:, i * 128:(i + 1) * 128], qlat[:, 4 * hf:4 * hf + 4, tcol], True, False, [B["cnT"], L["qlat"]], [PB[hf]])
                            mm(o3, ident, maskT[:, i:i + 1, :].to_broadcast([128, 4, 128]), False, not near, [cbB, mB], [PB[hf]])
                            if near:
                                mm(o3, ident, expb[:, kind, 4 * hf:4 * hf + 4, :], False, True, [cbB, B["expb"]], [PB[hf]])
                            P.op("act", lambda e: e.activation(out=ET[e2][:, 4 * hf:4 * hf + 4, :], in_=PS[hf][:].rearrange("p (h t) -> p h t", h=4),
                                                               func=AF.Exp, scale=0.125), reads=[PB[hf]], writes=[ETB[e2]])
                        for hf in range(2):
                            mm(PS[2 + hf][:].rearrange("p (h t) -> p h t", h=4), Ctok[:, i, :], ET[e2][:, 4 * hf:4 * hf + 4, :], i == 0, i == J,
                               [B["Ctok"], ETB[e2]], [PB[2 + hf]])
                        for hf in range(2):
                            mm(PS[4][32 * hf:32 * hf + 1, :].rearrange("p (h t) -> p h t", h=4), ones_b[:, 0:1], ET[e2][:, 4 * hf:4 * hf + 4, :], i == 0, i == J,
                               [cbB, ETB[e2]], [PB[4]])
                        yield "pair"
                    yield "pairs_done"
                    ones_f128 = cstf[:, C_ONE:C_ONE + 128]
                    for hf in range(2):
                        rr = rrow[32 * hf:32 * hf + 1, :]
                        P.op("act", lambda e: e.activation(out=rr, in_=PS[4][32 * hf:32 * hf + 1, :], func=AF.Ln), reads=[PB[4]], writes=[D3["rrow"]])
                        P.op("act", lambda e: e.activation(out=rr, in_=rr, func=AF.Exp, scale=-1.0), reads=[D3["rrow"]], writes=[D3["rrow"]])
                        mm(PS[hf][:], ones_f128[32 * hf:32 * hf + 1, :], rr, True, True, [cB, D3["rrow"]], [PB[hf]])
                        P.op("act", lambda e: e.activation(out=rd[:, hf * 512:(hf + 1) * 512], in_=PS[hf][:], func=AF.Copy), reads=[PB[hf]], writes=[D3["rd"]])
                        P.op("dve", lambda e: e.tensor_tensor(out=olat[:, 4 * hf:4 * hf + 4, :], in0=PS[2 + hf][:].rearrange("p (h t) -> p h t", h=4),
                                                              in1=rd[:, hf * 512:(hf + 1) * 512].rearrange("p (h t) -> p h t", h=4), op=ALU.mult),
                             reads=[PB[2 + hf], D3["rd"]], writes=[D3["olat"]])
                    yield
                    pb = 0
                    for hh in range(8):
                        i2, q4 = hh % 2, hh // 2
                        mm(PS[pb][64 * i2:64 * i2 + 64, q4 * 128:(q4 + 1) * 128], wuv[:, hh, :], olat[:, hh, :], True, True, [B["wuv"], D3["olat"]], [PB[pb]])
                    P.op("act", lambda e: e.activation(out=aoT[:, :, tcol], in_=PS[pb][:].rearrange("p (q t) -> p q t", q=4), func=AF.Copy),
                         reads=[PB[pb]], writes=[L["aoT"]])
                    yield

                if 'dsa' not in KSKIP:
                    def n_I(jt):
                        return ((128 * (4 * g + jt + 1) + 511) // 512) * 8
                    for step in range(6):
                        gi = gen_I(step) if step < 4 else None
                        gs = gen_S(step - 1) if 0 <= step - 1 < 4 else None
                        gb = gen_B(step - 2) if 0 <= step - 2 < 4 else None
                        nS = NIT + 2
                        rI = max(1, -(-n_I(step) // nS)) if gi is not None else 0
                        rB = max(1, -(-(4 * g + step - 2 + 2) // nS)) if gb is not None else 0
                        s_bisect_done = gs is None
                        b_parked = False
                        while gi is not None or gs is not None or (gb is not None):
                            if gs is not None:
                                try:
                                    if next(gs) == "mask":
                                        s_bisect_done = True
                                except StopIteration:
                                    gs = None
                                    s_bisect_done = True
                            if gb is not None:
                                for _ in range(rB):
                                    if b_parked and not s_bisect_done:
                                        break
                                    try:
                                        if next(gb) == "pairs_done":
                                            b_parked = True
                                    except StopIteration:
                                        gb = None
                                        break
                            if gi is not None:
                                for _ in range(rI):
                                    try:
                                        next(gi)
                                    except StopIteration:
                                        gi = None
                                        break
                P.barrier()
                ar.reset(ph_off)
                wo2 = ar.bf(NCH * D).rearrange("p (c n) -> p c n", c=NCH)
                wo2B = [Buf("wo2_%d" % i) for i in range(4)]
                for i4 in range(4):
                    P.dma("pool", wo2[:, :, i4 * 256:(i4 + 1) * 256], wview(e_w_out, i4 * 256, (i4 + 1) * 256), writes=[wo2B[i4]])
                for dc in range(NCH):
                    pb = dc % 2
                    for c in range(NCH):
                        src = aoT[:, c, :] if c < 4 else boT[:, c - 4, :]
                        mm(PS[pb][:], wo2[:, c, dc * 128:(dc + 1) * 128], src, c == 0, c == NCH - 1, [wo2B[dc // 2], L["aoT"], L["boT"]], [PB[pb]])
                    P.op("dve", lambda e, pb=pb, dc=dc, g=g: e.tensor_tensor(out=h[:, dc, tgs(g)], in0=h[:, dc, tgs(g)], in1=PS[pb][:], op=ALU.add),
                         reads=[PB[pb], hB[dc][g]], writes=[hB[dc][g]])
                P.barrier()

        def final_stage(b, do_norm):
            P.barrier()
            ar.reset()
            of = [ar.f32(NCH * 512).rearrange("p (c t) -> p c t", c=NCH) for _ in range(2)]
            ofB = [Buf("of0"), Buf("of1")]
            sq = ar.bf(NCH * 512).rearrange("p (c t) -> p c t", c=NCH)
            lnv = ar.f32(512)
            sqB, lnB = Buf("sq"), Buf("lnv")
            for g in range(NTG):
                q = g % 2
                if do_norm:
                    P.op("act", lambda e, g=g: e.activation(out=sq, in_=h[:, :, tgs(g)], func=AF.Square), reads=[hB[c][g] for c in range(NCH)], writes=[sqB])
                    for c in range(NCH):
                        mm(PS[0][:], ones_b, sq[:, c, :], c == 0, c == NCH - 1, [cbB, sqB], [PB[0]])
                    P.op("act", lambda e: e.activation(out=lnv, in_=PS[0][:], func=AF.Ln, scale=1.0 / D, bias=EPS), reads=[PB[0]], writes=[lnB])
                    P.op("act", lambda e: e.activation(out=lnv, in_=lnv, func=AF.Exp, scale=-0.5), reads=[lnB], writes=[lnB])
                    for c in range(NCH):
                        P.op("dve", lambda e, c=c, g=g, q=q: e.scalar_tensor_tensor(out=of[q][:, c, :], in0=h[:, c, tgs(g)], scalar=vecs[:, V_FIN + c:V_FIN + c + 1],
                                                                                  in1=lnv, op0=ALU.mult, op1=ALU.mult),
                             reads=[hB[c][g], vB, lnB], writes=[ofB[q]])
                    P.dma("sp", outT[b].rearrange("(c p) t -> p c t", p=128)[:, :, tgs(g)], of[q], reads=[ofB[q]])
                else:
                    P.dma("sp", outT[b].rearrange("(c p) t -> p c t", p=128)[:, :, tgs(g)], h[:, :, tgs(g)], reads=[hB[c][g] for c in range(NCH)])

        ones_f_t = sb("ones_f", [128, 64], F32)
        ones_f = ones_f_t[:]
        P.op("pool", lambda e: e.memset(ones_f, 1.0), writes=[cB])
        for b in range(NB):
            P.barrier()
            for g in range(NTG):
                P.dma("sp", h[:, :, tgs(g)], xT[b].rearrange("(c p) t -> p c t", p=128)[:, :, tgs(g)], writes=[hB[c][g] for c in range(NCH)])
            for s in stages:
                if s == "mix0":
                    mixer0_stage(b)
                elif s == "xa0":
                    xattn_stage(0, b)
                elif s == "ffn0":
                    ffn_stage(0)
                elif s == "mix1":
                    gmlp_stage()
                elif s == "xa1":
                    xattn_stage(1, b)
                elif s == "ffn1":
                    ffn_stage(1)
            final_stage(b, "fin" in stages)
        P.barrier()
        P.emit()
        nops = P.nops
    return nc, nops


def _t5_bucket(rel):
    nb = 16
    max_exact = 8
    ret = (rel > 0).astype(np.int64) * nb
    n = np.abs(rel)
    nf = np.maximum(n, 1).astype(np.float32)
    large = max_exact + (np.log(nf / max_exact) / math.log(128 / max_exact) * (nb - max_exact)).astype(np.int32)
    large = np.minimum(large, nb - 1)
    return ret + np.where(n < max_exact, n, large)


def _consts():
    c = np.zeros((128, NCST), np.float32)
    c[:, C_ID:C_ID + 128] = np.eye(128, dtype=np.float32)
    c[:, C_ONE:C_ONE + 128] = 1.0
    p = np.arange(128)
    c[:, C_TRIU:C_TRIU + 64] = ((p[:, None] % 64) <= np.arange(64)[None, :]).astype(np.float32)
    c[:, C_TRIL:C_TRIL + 128] = (p[:, None] <= np.arange(128)[None, :]).astype(np.float32)
    c[:, C_POW:C_POW + NIT + 1] = (0.5 ** np.arange(NIT + 1))[None, :].astype(np.float32)
    return c


def _fm(v):
    return np.ascontiguousarray(np.asarray(v, np.float32).reshape(8, 128).T)


def make_common(inp):
    f = lambda k: np.asarray(inp[k], np.float32)
    vec = np.zeros((128, NV), np.float32)
    vec[:, V_MIX0:V_MIX0 + 8] = _fm(f("mix_norm")[0])
    vec[:, V_MIX1:V_MIX1 + 8] = _fm(f("mix_norm")[1])
    vec[:, V_X0:V_X0 + 8] = _fm(f("x_norm")[0])
    vec[:, V_X1:V_X1 + 8] = _fm(f("x_norm")[1])
    vec[:, V_F0:V_F0 + 8] = _fm(f("f_norm")[0])
    vec[:, V_F1:V_F1 + 8] = _fm(f("f_norm")[1])
    vec[:, V_M0:V_M0 + 8] = _fm(f("mem_norm")[0])
    vec[:, V_M1:V_M1 + 8] = _fm(f("mem_norm")[1])
    vec[:, V_FIN:V_FIN + 8] = _fm(f("final_norm"))
    vec[:, V_LAT] = f("e_lat_norm")[0]
    vec[:, V_ON] = f("e_o_norm")[0]
    vec[:, V_LB:V_LB + 12] = f("hgrn_lb").reshape(3, 4, 128).transpose(2, 0, 1).reshape(128, 12)
    ps = np.arange(128)[:, None]
    pt = np.arange(128)[None, :]
    rb = f("rel_bias")
    tiles = np.zeros((128, 3, 8, 128), np.float32)
    for kind, off in enumerate((0, -128)):
        bk = _t5_bucket(ps - pt + off)
        tiles[:, kind] = rb[bk].transpose(0, 2, 1)
    tiles[:, 2] = rb[15][None, :, None]
    wuk = f("e_w_uk")[0]
    wukT = wuk.reshape(4, 2, 128, 64).transpose(1, 3, 0, 2).reshape(128, 4 * 128)
    wuv = f("e_w_uv")[0].transpose(1, 0, 2).reshape(128, 8 * 64)
    wsp = f("o_w_sp")[0].transpose(2, 0, 1).reshape(128, 8 * 128)
    return {
        "vecs": vec, "cst": _consts(), "biasT": np.ascontiguousarray(tiles.reshape(128, -1)),
        "e_w_in": np.ascontiguousarray(f("e_w_in")[0]), "e_w_ukT": np.ascontiguousarray(wukT), "e_w_uv": np.ascontiguousarray(wuv),
        "e_w_out": np.ascontiguousarray(f("e_w_out")[0]), "o_w_in": np.ascontiguousarray(f("o_w_in")[0]),
        "o_ln_g": f("o_ln_g").reshape(1, D), "o_ln_b": f("o_ln_b").reshape(1, D), "o_w_spT": np.ascontiguousarray(wsp),
        "o_b_sp": f("o_b_sp").reshape(1, 8 * 128), "o_w_out": np.ascontiguousarray(f("o_w_out")[0]),
        "x_wq": f("x_wq"), "x_wkv": f("x_wkv"), "x_wo": f("x_wo"), "f_w_gu": f("f_w_gu"), "f_w_down": f("f_w_down"),
    }


_CACHE = {}


def kernel(**inputs):
    x = np.asarray(inputs["x"], np.float32)
    mem = np.asarray(inputs["mem"], np.float32)
    Bn, T, _ = x.shape
    ncores = 8
    NB = Bn // ncores
    stages = tuple(os.environ.get("KSTAGES", "mix0,xa0,ffn0,mix1,xa1,ffn1,fin").split(","))
    key = (T, NB, stages)
    if key not in _CACHE:
        _CACHE[key] = build(T, NB, stages=stages)[0]
    nc = _CACHE[key]
    common = make_common(inputs)
    xT = np.ascontiguousarray(x.transpose(0, 2, 1))
    mT = np.ascontiguousarray(mem.transpose(0, 2, 1))
    in_maps = []
    for i in range(ncores):
        m = dict(common)
        m["xT"] = xT[i * NB:(i + 1) * NB]
        m["memT"] = mT[i * NB:(i + 1) * NB]
        in_maps.append(m)
    res = run_bass_kernel_spmd(nc, in_maps, core_ids=list(range(ncores)))
    outs = [r["outT"] for r in res.results]
    o = np.concatenate(outs, axis=0)
    return np.ascontiguousarray(o.transpose(0, 2, 1)).astype(np.float32)
```
